# Optimizing a Trainium2 kernel written in Bass

```python
import jax, jax.numpy as jnp
from jax import lax
import numpy as np

D_MODEL = 1024
BATCH = 8
SEQ = 4096
DEPTH = 1

D_RNN = 512
RNN_BLOCKS = 8
RNN_BLOCK_DIM = D_RNN // RNN_BLOCKS
CONV_WIDTH = 4
LRU_C = 8.0
N_HEADS = 8
N_KV = 2
HPG = N_HEADS // N_KV
HEAD_DIM = 64
D_ATTN = N_HEADS * HEAD_DIM
D_MIX = D_RNN + D_ATTN
KV_W = N_KV * HEAD_DIM
CMP_LEN = 32
CMP_STRIDE = 16
CMP_HIDDEN = 256
SLC_BLK = 64
SLC_TOPN = 16
WIN = 512
WIN_Q_BLK = 128
SLC_Q_BLK = 64
FORCE_SCORE = 1.0e4
ROPE_THETA = 500000.0
ROPE_DIM = HEAD_DIM // 4
D_FF = -(-8 * D_MODEL // (3 * 256)) * 256
EPS = 1e-6
IN_SIZES = (D_RNN, D_RNN, D_ATTN, KV_W, KV_W, KV_W, KV_W, KV_W, KV_W, 3 * N_HEADS)
D_IN = sum(IN_SIZES)
SPLIT_POINTS = tuple(int(v) for v in np.cumsum(IN_SIZES)[:-1])

kernel_name = "hymba_rglru_nsa_hybrid"


def rms_norm(x, w):
    xf = x.astype(jnp.float32)
    y = xf * lax.rsqrt(jnp.mean(xf * xf, axis=-1, keepdims=True) + EPS)
    return (y * w.astype(jnp.float32)).astype(x.dtype)


def partial_rope(x, pos):
    half = ROPE_DIM // 2
    inv_freq = jnp.power(ROPE_THETA, -jnp.arange(half, dtype=jnp.float32) * 2.0 / ROPE_DIM)
    ang = pos.astype(jnp.float32)[..., None] * inv_freq
    cos = jnp.cos(ang)[:, :, None, :]
    sin = jnp.sin(ang)[:, :, None, :]
    xf = x.astype(jnp.float32)
    x1 = xf[..., :half]
    x2 = xf[..., half:ROPE_DIM]
    out = jnp.concatenate([x1 * cos - x2 * sin, x2 * cos + x1 * sin, xf[..., ROPE_DIM:]], axis=-1)
    return out.astype(x.dtype)


def masked_softmax(s, mask):
    s = jnp.where(mask, s.astype(jnp.float32), -jnp.inf)
    m = jnp.max(s, axis=-1, keepdims=True)
    m = jnp.where(jnp.isfinite(m), m, 0.0)
    e = jnp.where(mask, jnp.exp(s - m), 0.0)
    return e / jnp.maximum(jnp.sum(e, axis=-1, keepdims=True), 1e-30)


def block_diag_linear(x, w, b):
    B, T, _ = x.shape
    xb = x.reshape(B, T, RNN_BLOCKS, RNN_BLOCK_DIM)
    return jnp.einsum('btnd,nde->btne', xb, w).reshape(B, T, D_RNN) + b


def lru_combine(c1, c2):
    a1, b1 = c1
    a2, b2 = c2
    return a1 * a2, a2 * b1 + b2


def rglru_mixer(xr, gr, conv_w, conv_b, gate_a_w, gate_a_b, gate_x_w, gate_x_b, lru_lambda):
    T = xr.shape[1]
    xp = jnp.pad(xr, ((0, 0), (CONV_WIDTH - 1, 0), (0, 0)))
    xc = conv_b
    for k in range(CONV_WIDTH):
        xc = xc + conv_w[k] * xp[:, k:k + T]
    r = jax.nn.sigmoid(block_diag_linear(xc, gate_a_w, gate_a_b)).astype(jnp.float32)
    i = jax.nn.sigmoid(block_diag_linear(xc, gate_x_w, gate_x_b))
    log_a = -LRU_C * r * jax.nn.softplus(-lru_lambda.astype(jnp.float32))
    a = jnp.exp(log_a)
    b = jnp.sqrt(-jnp.expm1(2.0 * log_a)) * (i * xc).astype(jnp.float32)
    _, h = lax.associative_scan(lru_combine, (a, b), axis=1)
    return jax.nn.gelu(gr) * h.astype(xr.dtype)


def nsa_mixer(q, kc_tok, vc_tok, ks_tok, vs_tok, kw_tok, vw_tok, gate_logits, positions,
              q_norm_w, k_norm_w, cmp_pos, cmp_k_w1, cmp_k_w2, cmp_v_w1, cmp_v_w2):
    B, T, _ = q.shape
    scale = HEAD_DIM ** -0.5
    t_idx = jnp.arange(T)

    def kv(t):
        return t.reshape(B, T, N_KV, HEAD_DIM)

    q = partial_rope(rms_norm(q.reshape(B, T, N_HEADS, HEAD_DIM), q_norm_w), positions)
    qg = q.reshape(B, T, N_KV, HPG, HEAD_DIM)

    n_cmp = (T - CMP_LEN) // CMP_STRIDE + 1
    cmp_start = jnp.arange(n_cmp) * CMP_STRIDE
    tok_idx = cmp_start[:, None] + jnp.arange(CMP_LEN)[None, :]
    cmp_end = cmp_start + CMP_LEN - 1

    def compress(t, w1, w2):
        blk = t[:, tok_idx] + cmp_pos[None, None, :, None, :]
        blk = blk.transpose(0, 1, 3, 2, 4).reshape(B, n_cmp, N_KV, CMP_LEN * HEAD_DIM)
        return jax.nn.gelu(blk @ w1) @ w2

    kc = partial_rope(rms_norm(compress(kv(kc_tok), cmp_k_w1, cmp_k_w2), k_norm_w), positions[:, cmp_end])
    vc = compress(kv(vc_tok), cmp_v_w1, cmp_v_w2)
    s_c = jnp.einsum('btghd,bcgd->bghtc', qg, kc).astype(jnp.float32) * scale
    p_c = masked_softmax(s_c, cmp_end[None, :] <= t_idx[:, None])
    o_cmp = jnp.einsum('bghtc,bcgd->btghd', p_c.astype(vc.dtype), vc)

    nb = T // SLC_BLK
    blk_start = jnp.arange(nb) * SLC_BLK
    lo = jnp.maximum(cmp_start[:, None], blk_start[None, :])
    hi = jnp.minimum(cmp_start[:, None] + CMP_LEN, blk_start[None, :] + SLC_BLK)
    cover = jnp.clip(hi - lo, 0).astype(jnp.float32) / CMP_LEN
    imp = jnp.einsum('bghtc,cj->bgtj', p_c, cover)
    cur = t_idx // SLC_BLK
    jb = jnp.arange(nb)
    valid_blk = blk_start[None, :] <= t_idx[:, None]
    forced = (jb[None, :] == 0) | (jb[None, :] == cur[:, None]) | (jb[None, :] == cur[:, None] - 1)
    score = jnp.where(forced, FORCE_SCORE, jnp.where(valid_blk, imp, -jnp.inf))
    n_top = min(SLC_TOPN, nb)
    _, sel_idx = lax.top_k(score, n_top)

    ks = partial_rope(rms_norm(kv(ks_tok), k_norm_w), positions)
    kb = ks.transpose(0, 2, 1, 3).reshape(B, N_KV, nb, SLC_BLK, HEAD_DIM)
    vb = kv(vs_tok).transpose(0, 2, 1, 3).reshape(B, N_KV, nb, SLC_BLK, HEAD_DIM)
    nqs = T // SLC_Q_BLK
    q_s = qg.reshape(B, nqs, SLC_Q_BLK, N_KV, HPG, HEAD_DIM).transpose(1, 0, 3, 4, 2, 5)
    i_s = sel_idx.reshape(B, N_KV, nqs, SLC_Q_BLK, n_top).transpose(2, 0, 1, 3, 4)
    t_s = t_idx.reshape(nqs, SLC_Q_BLK)
    b_ix = jnp.arange(B)[:, None, None, None]
    g_ix = jnp.arange(N_KV)[None, :, None, None]

    def sel_step(args):
        qb, ib, tb = args
        kg = kb[b_ix, g_ix, ib]
        vg = vb[b_ix, g_ix, ib]
        s = jnp.einsum('bghqd,bgqnkd->bghqnk', qb, kg).astype(jnp.float32) * scale
        key_pos = ib[..., None] * SLC_BLK + jnp.arange(SLC_BLK)
        mask = (key_pos <= tb[None, None, :, None, None])[:, :, None]
        p = masked_softmax(s.reshape(s.shape[:4] + (-1,)), mask.reshape(mask.shape[:4] + (-1,)))
        p = p.reshape(s.shape)
        return jnp.einsum('bghqnk,bgqnkd->bghqd', p.astype(vg.dtype), vg)

    o_s = lax.map(sel_step, (q_s, i_s, t_s))
    o_slc = o_s.transpose(1, 0, 4, 2, 3, 5).reshape(B, T, N_KV, HPG, HEAD_DIM)

    kw = partial_rope(rms_norm(kv(kw_tok), k_norm_w), positions)
    kwp = jnp.pad(kw.transpose(0, 2, 1, 3), ((0, 0), (0, 0), (WIN, 0), (0, 0)))
    vwp = jnp.pad(kv(vw_tok).transpose(0, 2, 1, 3), ((0, 0), (0, 0), (WIN, 0), (0, 0)))
    nqw = T // WIN_Q_BLK
    q_w = qg.reshape(B, nqw, WIN_Q_BLK, N_KV, HPG, HEAD_DIM).transpose(1, 0, 3, 4, 2, 5)
    starts = jnp.arange(nqw) * WIN_Q_BLK
    span = WIN + WIN_Q_BLK

    def win_step(args):
        qb, start = args
        kblk = lax.dynamic_slice_in_dim(kwp, start, span, axis=2)
        vblk = lax.dynamic_slice_in_dim(vwp, start, span, axis=2)
        s = jnp.einsum('bghqd,bgkd->bghqk', qb, kblk).astype(jnp.float32) * scale
        tq = start + jnp.arange(WIN_Q_BLK)
        sk = start - WIN + jnp.arange(span)
        mask = (sk[None, :] <= tq[:, None]) & (tq[:, None] - sk[None, :] < WIN) & (sk[None, :] >= 0)
        p = masked_softmax(s, mask)
        return jnp.einsum('bghqk,bgkd->bghqd', p.astype(vblk.dtype), vblk)

    o_w = lax.map(win_step, (q_w, starts))
    o_win = o_w.transpose(1, 0, 4, 2, 3, 5).reshape(B, T, N_KV, HPG, HEAD_DIM)

    g = jax.nn.sigmoid(gate_logits.astype(jnp.float32)).reshape(B, T, N_KV, HPG, 3).astype(q.dtype)
    o = g[..., 0:1] * o_cmp + g[..., 1:2] * o_slc + g[..., 2:3] * o_win
    return o.reshape(B, T, D_ATTN)


def setup_inputs(seed: int = 0) -> dict:
    key = jax.random.key(seed)
    ks = jax.random.split(key, 26)
    f32 = jnp.float32
    L = DEPTH

    def nrm(k, shape, scale):
        return jax.random.normal(k, shape, f32) * scale

    def gain(k, shape):
        return 1.0 + 0.01 * jax.random.normal(k, shape, f32)

    offset = jax.random.randint(ks[1], (BATCH, 1), 0, 1024, jnp.int32)
    positions = jnp.arange(SEQ, dtype=jnp.int32)[None, :] + offset
    u = jax.random.uniform(ks[10], (L, D_RNN), f32, 0.9, 0.999)
    s = u ** (1.0 / LRU_C)
    lru_lambda = jnp.log(s) - jnp.log1p(-s)
    return {
        "x": nrm(ks[0], (BATCH, SEQ, D_MODEL), 1.0),
        "positions": positions,
        "attn_norm_w": gain(ks[2], (L, D_MODEL)),
        "w_in": nrm(ks[3], (L, D_MODEL, D_IN), D_MODEL ** -0.5),
        "conv_w": nrm(ks[4], (L, CONV_WIDTH, D_RNN), CONV_WIDTH ** -0.5),
        "conv_b": nrm(ks[5], (L, D_RNN), 0.01),
        "gate_a_w": nrm(ks[6], (L, RNN_BLOCKS, RNN_BLOCK_DIM, RNN_BLOCK_DIM), RNN_BLOCK_DIM ** -0.5),
        "gate_a_b": nrm(ks[7], (L, D_RNN), 0.1),
        "gate_x_w": nrm(ks[8], (L, RNN_BLOCKS, RNN_BLOCK_DIM, RNN_BLOCK_DIM), RNN_BLOCK_DIM ** -0.5),
        "gate_x_b": nrm(ks[9], (L, D_RNN), 0.1),
        "lru_lambda": lru_lambda,
        "q_norm_w": gain(ks[11], (L, HEAD_DIM)),
        "k_norm_w": gain(ks[12], (L, HEAD_DIM)),
        "cmp_pos": nrm(ks[13], (L, CMP_LEN, HEAD_DIM), 0.1),
        "cmp_k_w1": nrm(ks[14], (L, CMP_LEN * HEAD_DIM, CMP_HIDDEN), (CMP_LEN * HEAD_DIM) ** -0.5),
        "cmp_k_w2": nrm(ks[15], (L, CMP_HIDDEN, HEAD_DIM), CMP_HIDDEN ** -0.5),
        "cmp_v_w1": nrm(ks[16], (L, CMP_LEN * HEAD_DIM, CMP_HIDDEN), (CMP_LEN * HEAD_DIM) ** -0.5),
        "cmp_v_w2": nrm(ks[17], (L, CMP_HIDDEN, HEAD_DIM), CMP_HIDDEN ** -0.5),
        "rnn_out_norm_w": gain(ks[18], (L, D_RNN)),
        "attn_out_norm_w": gain(ks[19], (L, D_ATTN)),
        "w_out": nrm(ks[20], (L, D_MIX, D_MODEL), D_MIX ** -0.5),
        "ffn_norm_w": gain(ks[21], (L, D_MODEL)),
        "w_gate": nrm(ks[22], (L, D_MODEL, D_FF), D_MODEL ** -0.5),
        "w_up": nrm(ks[23], (L, D_MODEL, D_FF), D_MODEL ** -0.5),
        "w_down": nrm(ks[24], (L, D_FF, D_MODEL), D_FF ** -0.5),
    }


def reference(x, positions, attn_norm_w, w_in, conv_w, conv_b, gate_a_w, gate_a_b, gate_x_w, gate_x_b,
              lru_lambda, q_norm_w, k_norm_w, cmp_pos, cmp_k_w1, cmp_k_w2, cmp_v_w1, cmp_v_w2,
              rnn_out_norm_w, attn_out_norm_w, w_out, ffn_norm_w, w_gate, w_up, w_down):
    for l in range(DEPTH):
        h = rms_norm(x, attn_norm_w[l])
        proj = h @ w_in[l]
        xr, gr, q, kc, vc, ks, vs, kw, vw, gl = jnp.split(proj, SPLIT_POINTS, axis=-1)
        y_rnn = rglru_mixer(xr, gr, conv_w[l], conv_b[l], gate_a_w[l], gate_a_b[l],
                            gate_x_w[l], gate_x_b[l], lru_lambda[l])
        y_att = nsa_mixer(q, kc, vc, ks, vs, kw, vw, gl, positions, q_norm_w[l], k_norm_w[l],
                          cmp_pos[l], cmp_k_w1[l], cmp_k_w2[l], cmp_v_w1[l], cmp_v_w2[l])
        y = jnp.concatenate([rms_norm(y_rnn, rnn_out_norm_w[l]), rms_norm(y_att, attn_out_norm_w[l])], axis=-1)
        x = x + y @ w_out[l]
        h = rms_norm(x, ffn_norm_w[l])
        x = x + (jax.nn.silu(h @ w_gate[l]) * (h @ w_up[l])) @ w_down[l]
    return x
```

```python
import numpy as np
import ml_dtypes
from contextlib import ExitStack
import concourse.bass as bass
import concourse.mybir as mybir
from concourse.bass_utils import run_bass_kernel_spmd

F32, BF16, I32 = mybir.dt.float32, mybir.dt.bfloat16, mybir.dt.int32
AF = mybir.ActivationFunctionType
ALU = mybir.AluOpType
AX = mybir.AxisListType

T = 4096
NT = 32
D = 1024
DIN = 2328
DFF = 2816
NFF = 22
EPS = 1e-6
NEG = -30000.0
TWO_PI = 6.283185307179586
PI = 3.141592653589793


class Buf:
    def __init__(self, fw, t, name):
        self.fw, self.t, self.name = fw, t, name
        self.w = None
        self.r = {}
        self.dsem = None
        self.dkey = None
        self.dcnt = 0

    def __getitem__(self, k):
        return self.t[k]


class Eng:
    def __init__(self, fw, name, eng, sem, key, selfwait):
        self.fw, self.name, self.eng, self.sem, self.key = fw, name, eng, sem, key
        self.selfwait = selfwait
        self.cnt = 0
        self.waited = {}

    def sync(self, reads, writes):
        waits = {}

        def need(ev, raw):
            if ev is None:
                return
            key, sem, val = ev
            if key == self.key and not (self.selfwait and raw):
                return
            if self.waited.get(key, 0) >= val:
                return
            if key not in waits or waits[key][1] < val:
                waits[key] = (sem, val)

        for b in reads:
            need(b.w, True)
        for b in writes:
            need(b.w, False)
            for ev in b.r.values():
                need(ev, False)
        for key, (sem, val) in waits.items():
            self.eng.wait_ge(sem, val)
            self.waited[key] = val

    def wait_ev(self, ev):
        key, sem, val = ev
        if self.waited.get(key, 0) >= val:
            return
        self.eng.wait_ge(sem, val)
        self.waited[key] = val


class FW:
    def __init__(self, nc, es):
        self.nc, self.es = nc, es
        self.nkey = 0
        self.engs = {}
        for name, eng, sw in (("PE", nc.tensor, False), ("ACT", nc.scalar, True),
                              ("DVE", nc.vector, True), ("POOL", nc.gpsimd, True),
                              ("SP", nc.sync, False)):
            sem = es.enter_context(nc.semaphore("sem_" + name))
            self.engs[name] = Eng(self, name, eng, sem, self.newkey(), sw)
        self.PE, self.ACT, self.DVE, self.POOL, self.SP = (self.engs[n] for n in ("PE", "ACT", "DVE", "POOL", "SP"))
        self.carriers = []
        self.nbuf = 0

    def newkey(self):
        self.nkey += 1
        return self.nkey

    def sb(self, name, shape, dt, es=None):
        es = es or self.es
        self.nbuf += 1
        t = es.enter_context(self.nc.sbuf_tensor("%s_%d" % (name, self.nbuf), list(shape), dt))
        return Buf(self, t, name)

    def ps(self, name, shape, dt, es=None):
        es = es or self.es
        self.nbuf += 1
        t = es.enter_context(self.nc.psum_tensor("%s_%d" % (name, self.nbuf), list(shape), dt))
        return Buf(self, t, name)

    def dram(self, ap, name):
        return Buf(self, ap, name)

    def op(self, E, meth, reads, writes, *a, **kw):
        E.sync(reads, writes)
        ins = getattr(E.eng, meth)(*a, **kw)
        E.cnt += 1
        ins.then_inc(E.sem, 1)
        ev = (E.key, E.sem, E.cnt)
        for b in reads:
            b.r[E.key] = ev
        for b in writes:
            b.w = ev
            b.r = {}
        return ins

    def dma(self, Q, carrier, reads, writes, out, in_, **kw):
        Q.sync(reads, writes)
        if carrier.dsem is None:
            carrier.dsem = self.es.enter_context(self.nc.semaphore("dsem_%s_%d" % (carrier.name, len(self.carriers))))
            carrier.dkey = self.newkey()
            self.carriers.append(carrier)
        ins = Q.eng.dma_start(out=out, in_=in_, **kw)
        carrier.dcnt += 16
        ins.then_inc(carrier.dsem, 16)
        ev = (carrier.dkey, carrier.dsem, carrier.dcnt)
        for b in reads:
            b.r[carrier.dkey] = ev
        for b in writes:
            b.w = ev
            b.r = {}
        return ins

    def barrier(self):
        SP = self.SP
        for E in self.engs.values():
            if E is not SP and E.cnt > 0:
                SP.wait_ev((E.key, E.sem, E.cnt))
        for c in self.carriers:
            if c.dcnt > 0:
                SP.wait_ev((c.dkey, c.dsem, c.dcnt))
        ins = SP.eng.nop()
        SP.cnt += 1
        ins.then_inc(SP.sem, 1)
        ev = (SP.key, SP.sem, SP.cnt)
        for E in self.engs.values():
            if E is not SP:
                E.wait_ev(ev)


class Ring:
    def __init__(self, bufs):
        self.bufs, self.i = bufs, 0

    def get(self):
        b = self.bufs[self.i % len(self.bufs)]
        self.i += 1
        return b


def build(dbg=None, stop=None):
    nc = bass.Bass("TRN2", target_bir_lowering=False)

    def din(name, shape, dt=F32):
        return nc.dram_tensor(name, list(shape), dt, kind="ExternalInput").ap()

    x_d = din("x", [T, D])
    pos_d = din("pos", [128, NT], I32)
    posc_d = din("posc", [128, 2], I32)
    invf_d = din("invf", [128, 8])
    win_d = din("w_in", [D, DIN])
    anw_d = din("anw", [128, D])
    qkw_d = din("qkw", [128, 12 * 64])
    convw_d = din("convw", [128, 16])
    rnnp_d = din("rnnp", [128, 20])
    gaw_d = din("gaw", [128, 512])
    gxw_d = din("gxw", [128, 512])
    identf_d = din("identf", [128, 128])
    cw1_d = {"k": din("cw1k", [128, 32 * 256]), "v": din("cw1v", [128, 32 * 256])}
    cw2_d = {"k": din("cw2k", [128, 128]), "v": din("cw2v", [128, 128])}
    cposT_d = din("cposT", [128, 32])
    cover_d = din("cover", [128, 128])
    tri4_d = din("tri4", [128, 512], BF16)
    wlow4_d = din("wlow4", [128, 512], BF16)
    cmask_d = din("cmask", [128, 33 * 128], BF16)
    efull_d = din("efull", [128, T], BF16)
    vmbs_d = din("vmbs", [128, 256])
    wout_d = din("w_out", [D, D])
    aonw_d = din("aonw", [128, 512])
    fnw_d = din("fnw", [128, D])
    wg_d = din("w_gate", [D, DFF])
    wu_d = din("w_up", [D, DFF])
    wd_d = din("w_down", [DFF, D])
    y_d = nc.dram_tensor("y", [T, D], F32, kind="ExternalOutput").ap()
    dbg_d = {}
    if dbg:
        for nm, shp, dt in dbg:
            dbg_d[nm] = nc.dram_tensor("dbg_" + nm, list(shp), dt, kind="ExternalOutput").ap()

    with ExitStack() as es:
        fw = FW(nc, es)
        PE, ACT, DVE, POOL, SP = fw.PE, fw.ACT, fw.DVE, fw.POOL, fw.SP
        op, dma = fw.op, fw.dma
        att = ExitStack()
        tokst = ExitStack()

        identf = fw.sb("identf", [128, 128], F32)
        identb = fw.sb("identb", [128, 128], BF16)
        dma(SP, identf, [], [identf], out=identf[:], in_=identf_d[:, :])
        op(DVE, "tensor_copy", [identf], [identb], out=identb[:], in_=identf[:])
        ones_col = fw.sb("ones_col", [128, 1], F32)
        ones_row = fw.sb("ones_row", [1, 128], F32)
        neghalf = fw.sb("neghalf", [128, 512], F32)
        op(DVE, "memset", [], [ones_col], ones_col[:], 1.0)
        op(DVE, "memset", [], [ones_row], ones_row[:], 1.0)
        op(DVE, "memset", [], [neghalf], neghalf[:], -0.5)

        anw = fw.sb("anw", [128, D], F32, att)
        dma(SP, anw, [], [anw], out=anw[:], in_=anw_d[:, :])
        qkw = fw.sb("qkw", [128, 12, 64], F32, att)
        dma(SP, qkw, [], [qkw], out=qkw[:].rearrange("p h d -> p (h d)"), in_=qkw_d[:, :])
        convw = fw.sb("convw", [128, 4, 4], F32, att)
        dma(SP, convw, [], [convw], out=convw[:].rearrange("p c k -> p (c k)"), in_=convw_d[:, :])
        rnnp = fw.sb("rnnp", [128, 4, 5], F32, att)
        dma(SP, rnnp, [], [rnnp], out=rnnp[:].rearrange("p c k -> p (c k)"), in_=rnnp_d[:, :])
        gaw = fw.sb("gaw", [128, 4, 128], BF16, att)
        gxw = fw.sb("gxw", [128, 4, 128], BF16, att)
        dma(POOL, gaw, [], [gaw], out=gaw[:].rearrange("p c k -> p (c k)"), in_=gaw_d[:, :])
        dma(POOL, gxw, [], [gxw], out=gxw[:].rearrange("p c k -> p (c k)"), in_=gxw_d[:, :])

        def rope_tables(pos_ap, n, name):
            cosT_ = fw.sb(name + "cos", [128, n, 16], F32, att)
            sinT_ = fw.sb(name + "sin", [128, n, 16], F32, att)
            with ExitStack() as tmp:
                posi = fw.sb(name + "posi", [128, n], I32, tmp)
                dma(SP, posi, [], [posi], out=posi[:], in_=pos_ap)
                invf = fw.sb(name + "invf", [128, 8], F32, tmp)
                dma(SP, invf, [], [invf], out=invf[:], in_=invf_d[:, :])
                posf = fw.sb(name + "posf", [128, n], F32, tmp)
                op(DVE, "tensor_copy", [posi], [posf], out=posf[:], in_=posi[:])
                ang = fw.sb(name + "ang", [128, n, 8], F32, tmp)
                op(DVE, "tensor_tensor", [posf, invf], [ang], out=ang[:],
                   in0=posf[:].unsqueeze(2).broadcast_to([128, n, 8]),
                   in1=invf[:].unsqueeze(1).broadcast_to([128, n, 8]), op=ALU.mult)
                a2 = fw.sb(name + "a2", [128, n * 8], F32, tmp)
                ki = fw.sb(name + "ki", [128, n * 8], I32, tmp)
                kf = fw.sb(name + "kf", [128, n * 8], F32, tmp)
                m = fw.sb(name + "m", [128, n * 8], F32, tmp)
                for tab, shift in ((cosT_, PI / 2), (sinT_, 0.0)):
                    angf = ang[:].rearrange("p n f -> p (n f)")
                    op(DVE, "tensor_scalar", [ang], [a2], out=a2[:], in0=angf, scalar1=shift, scalar2=None, op0=ALU.add)
                    op(DVE, "tensor_scalar", [a2], [kf], out=kf[:], in0=a2[:], scalar1=1.0 / TWO_PI, scalar2=None, op0=ALU.mult)
                    op(DVE, "tensor_copy", [kf], [ki], out=ki[:], in_=kf[:])
                    op(DVE, "tensor_copy", [ki], [kf], out=kf[:], in_=ki[:])
                    op(DVE, "scalar_tensor_tensor", [kf, a2], [a2], out=a2[:], in0=kf[:], scalar=-TWO_PI, in1=a2[:], op0=ALU.mult, op1=ALU.add)
                    op(DVE, "tensor_scalar", [a2], [m], out=m[:], in0=a2[:], scalar1=PI, scalar2=None, op0=ALU.is_gt)
                    op(DVE, "scalar_tensor_tensor", [m, a2], [a2], out=a2[:], in0=m[:], scalar=-TWO_PI, in1=a2[:], op0=ALU.mult, op1=ALU.add)
                    op(DVE, "tensor_scalar", [a2], [m], out=m[:], in0=a2[:], scalar1=-PI, scalar2=None, op0=ALU.is_lt)
                    op(DVE, "scalar_tensor_tensor", [m, a2], [a2], out=a2[:], in0=m[:], scalar=TWO_PI, in1=a2[:], op0=ALU.mult, op1=ALU.add)
                    op(DVE, "tensor_scalar", [a2], [a2], out=a2[:], in0=a2[:], scalar1=PI, scalar2=-PI, op0=ALU.min, op1=ALU.max)
                    a3 = a2[:].rearrange("p (n f) -> p n f", f=8)
                    op(ACT, "activation", [a2], [tab], out=tab[:, :, 0:8], in_=a3, func=AF.Sin)
                    op(ACT, "activation", [a2], [tab], out=tab[:, :, 8:16], in_=a3, func=AF.Sin)
                fw.barrier()
            return cosT_, sinT_

        cosT, sinT = rope_tables(pos_d[:, :], NT, "rp")
        coscT, sincT = rope_tables(posc_d[:, :], 2, "rc")

        def rsqrt_small(src_ap, srcbufs, dst, dst_ap, n, scale, tmp):
            op(DVE, "tensor_scalar", srcbufs, [tmp], out=tmp[:, 0:n], in0=src_ap, scalar1=scale, scalar2=EPS, op0=ALU.mult, op1=ALU.add)
            op(POOL, "tensor_tensor", [tmp, neghalf], [dst], out=dst_ap, in0=tmp[:, 0:n], in1=neghalf[:, 0:n], op=ALU.pow)

        def norm_rope(qf, qb, nh, wt, cos_ap, sin_ap, csbufs, qn, st12, qw16, t12, t34, wo=0):
            op(DVE, "tensor_tensor", [qf], [qn], out=qn[:, 0:nh, :], in0=qf[:, 0:nh, :], in1=qf[:, 0:nh, :], op=ALU.mult)
            s12 = st12.get()
            op(DVE, "tensor_reduce", [qn], [s12], out=s12[:, 0:nh], in_=qn[:, 0:nh, :], axis=AX.X, op=ALU.add)
            r12 = st12.get()
            rsqrt_small(s12[:, 0:nh], [s12], r12, r12[:, 0:nh], nh, 1.0 / 64, st12.get())
            op(DVE, "tensor_tensor", [qf, r12], [qn], out=qn[:, 0:nh, :], in0=qf[:, 0:nh, :], in1=r12[:, 0:nh].unsqueeze(2).broadcast_to([128, nh, 64]), op=ALU.mult)
            op(DVE, "tensor_tensor", [qn, wt], [qb], out=qb[:, 0:nh, :], in0=qn[:, 0:nh, :], in1=wt[:, wo:wo + nh, :], op=ALU.mult)
            op(DVE, "tensor_tensor", [qn, wt], [qw16], out=qw16[:, 0:nh, :], in0=qn[:, 0:nh, 0:16], in1=wt[:, wo:wo + nh, 0:16], op=ALU.mult)
            op(DVE, "tensor_tensor", [qw16] + csbufs, [t12], out=t12[:, 0:nh, :], in0=qw16[:, 0:nh, :], in1=cos_ap.broadcast_to([128, nh, 16]), op=ALU.mult)
            op(DVE, "tensor_tensor", [qw16] + csbufs, [t34], out=t34[:, 0:nh, :], in0=qw16[:, 0:nh, :], in1=sin_ap.broadcast_to([128, nh, 16]), op=ALU.mult)
            op(DVE, "tensor_tensor", [t12, t34], [qb], out=qb[:, 0:nh, 0:8], in0=t12[:, 0:nh, 0:8], in1=t34[:, 0:nh, 8:16], op=ALU.subtract)
            op(DVE, "tensor_tensor", [t12, t34], [qb], out=qb[:, 0:nh, 8:16], in0=t12[:, 0:nh, 8:16], in1=t34[:, 0:nh, 0:8], op=ALU.add)

        def norm_transpose_block(tb, src_d, wbc, W, hT, srcbufs=None):
            xts = []
            for j in range(4):
                tt = tb * 4 + j
                xt = W["x"].get()
                xts.append(xt)
                dma(SP, xt, [srcbufs[tt]] if srcbufs else [], [xt], out=xt[:], in_=src_d[tt * 128:(tt + 1) * 128, :])
                st = W["stat"].get()
                hb = W["hb"].get()
                op(ACT, "activation", [xt], [hb, st], out=hb[:], in_=xt[:], func=AF.Square, accum_out=st[:, 0:1])
                rsqrt_small(st[:, 0:1], [st], st, st[:, 2:3], 1, 1.0 / D, W["stmp"].get())
                op(DVE, "scalar_tensor_tensor", [xt, st, wbc], [hb], out=hb[:], in0=xt[:], scalar=st[:, 2:3], in1=wbc[:], op0=ALU.mult, op1=ALU.mult)
                pT = W["psT"].get()
                for k in range(8):
                    op(PE, "transpose", [hb, identb], [pT], out=pT[:, k * 128:(k + 1) * 128], in_=hb[:, k * 128:(k + 1) * 128], identity=identb[:])
                op(ACT, "activation", [pT], [hT], out=hT[:, :, j * 128:(j + 1) * 128], in_=pT[:].rearrange("p (k t) -> p k t", k=8), func=AF.Copy)
            return xts

        def nt_work(es_, nx=2, nh=2):
            return {
                "x": Ring([fw.sb("xt", [128, D], F32, es_) for _ in range(nx)]),
                "stat": Ring([fw.sb("stat", [128, 4], F32, es_) for _ in range(4)]),
                "stmp": Ring([fw.sb("stmp", [128, 12], F32, es_) for _ in range(4)]),
                "hb": Ring([fw.sb("hb", [128, D], BF16, es_) for _ in range(2)]),
                "psT": Ring([fw.ps("psT", [128, D], BF16, es_) for _ in range(2)]),
                "hT": Ring([fw.sb("hT", [128, 8, 512], BF16, es_) for _ in range(nh)]),
            }

        yrnnT = fw.sb("yrnnT", [128, 4, T], BF16, att)
        kcT = fw.sb("kcT", [128, 256], BF16, att)
        VcA = fw.sb("VcA", [128, 2, 2, 128], BF16, att)
        kcTok = fw.sb("kcTok", [128, T], BF16, tokst)
        vcTok = fw.sb("vcTok", [128, T], BF16, tokst)
        ydr = [fw.dram(None, "ydr%d" % i) for i in range(NT)]

        with ExitStack() as p1:
            W = nt_work(p1)
            WinA = fw.sb("WinA", [128, 8, 1280], BF16, p1)
            for k in range(8):
                dma(POOL, WinA, [], [WinA], out=WinA[:, k, 0:1024], in_=win_d[k * 128:(k + 1) * 128, 0:1024])
                dma(POOL, WinA, [], [WinA], out=WinA[:, k, 1024:1280], in_=win_d[k * 128:(k + 1) * 128, 1536:1792])
            psM = Ring([fw.ps("psM", [128, 512], F32, p1) for _ in range(4)])
            psS = fw.ps("psS", [128, 512], F32, p1)
            xpad = [fw.sb("xpad", [128, 515], F32, p1) for _ in range(4)]
            gel = Ring([fw.sb("gel", [128, 512], F32, p1) for _ in range(2)])
            xc = Ring([fw.sb("xc", [128, 512], F32, p1) for _ in range(2)])
            xcb = Ring([fw.sb("xcb", [128, 512], BF16, p1) for _ in range(2)])
            rr = Ring([fw.sb("rr", [128, 512], F32, p1) for _ in range(2)])
            ii = Ring([fw.sb("ii", [128, 512], F32, p1) for _ in range(2)])
            aa = Ring([fw.sb("aa", [128, 512], F32, p1) for _ in range(2)])
            hring = Ring([fw.sb("hh", [128, 512], F32, p1) for _ in range(2)])
            hlast = fw.sb("hlast", [128, 4], F32, p1)
            y4 = fw.sb("y4", [128, 4, 512], F32, p1)
            ysq = Ring([fw.sb("ysq", [128, 512], F32, p1) for _ in range(2)])
            ssrow = fw.sb("ssrow", [1, 512], F32, p1)
            rrow = fw.sb("rrow", [1, 512], F32, p1)
            c1 = fw.sb("c1", [128, 4, 2], F32, p1)
            c1t = fw.sb("c1t", [128, 4], F32, p1)

            for c in range(4):
                op(DVE, "memset", [], [xpad[c]], xpad[c][:], 0.0)
            op(DVE, "memset", [], [hlast], hlast[:], 0.0)
            op(ACT, "activation", [rnnp], [c1t], out=c1t[:], in_=rnnp[:, :, 3], func=AF.Exp, scale=-1.0)
            op(ACT, "activation", [c1t], [c1t], out=c1t[:], in_=c1t[:], func=AF.Ln, bias=1.0)
            op(DVE, "tensor_scalar", [c1t], [c1], out=c1[:, :, 0], in0=c1t[:], scalar1=-8.0, scalar2=None, op0=ALU.mult)
            op(DVE, "tensor_scalar", [c1t], [c1], out=c1[:, :, 1], in0=c1t[:], scalar1=-16.0, scalar2=None, op0=ALU.mult)

            for tb in range(8):
                hT = W["hT"].get()
                norm_transpose_block(tb, x_d, anw, W, hT)
                for dst, c0 in ((kcTok, 1024), (vcTok, 1152)):
                    pF = psM.get()
                    for k in range(8):
                        op(PE, "matmul", [hT, WinA], [pF], pF[:], lhsT=WinA[:, k, c0:c0 + 128], rhs=hT[:, k, :], start=(k == 0), stop=(k == 7))
                    op(ACT, "activation", [pF], [dst], out=dst[:, tb * 512:(tb + 1) * 512], in_=pF[:], func=AF.Copy)
                for c in range(4):
                    pX = psM.get()
                    for k in range(8):
                        op(PE, "matmul", [hT, WinA], [pX], pX[:], lhsT=WinA[:, k, c * 128:(c + 1) * 128], rhs=hT[:, k, :], start=(k == 0), stop=(k == 7))
                    xp = xpad[c]
                    if tb > 0:
                        op(DVE, "tensor_copy", [xp], [xp], out=xp[:, 0:3], in_=xp[:, 512:515])
                    op(ACT, "activation", [pX], [xp], out=xp[:, 3:515], in_=pX[:], func=AF.Copy)
                    pG = psM.get()
                    for k in range(8):
                        op(PE, "matmul", [hT, WinA], [pG], pG[:], lhsT=WinA[:, k, 512 + c * 128:512 + (c + 1) * 128], rhs=hT[:, k, :], start=(k == 0), stop=(k == 7))
                    g_ = gel.get()
                    op(ACT, "activation", [pG], [g_], out=g_[:], in_=pG[:], func=AF.Gelu_apprx_tanh)
                    xc_ = xc.get()
                    op(DVE, "tensor_scalar", [xp, convw, rnnp], [xc_], out=xc_[:], in0=xp[:, 0:512], scalar1=convw[:, c, 0:1], scalar2=rnnp[:, c, 0:1], op0=ALU.mult, op1=ALU.add)
                    for k in range(1, 4):
                        op(DVE, "scalar_tensor_tensor", [xp, convw, xc_], [xc_], out=xc_[:], in0=xp[:, k:k + 512], scalar=convw[:, c, k:k + 1], in1=xc_[:], op0=ALU.mult, op1=ALU.add)
                    xcb_ = xcb.get()
                    op(POOL, "tensor_copy", [xc_], [xcb_], out=xcb_[:], in_=xc_[:])
                    pa = psM.get()
                    op(PE, "matmul", [gaw, xcb_], [pa], pa[:], lhsT=gaw[:, c, :], rhs=xcb_[:], start=True, stop=True)
                    px = psM.get()
                    op(PE, "matmul", [gxw, xcb_], [px], px[:], lhsT=gxw[:, c, :], rhs=xcb_[:], start=True, stop=True)
                    r_, i_, a_ = rr.get(), ii.get(), aa.get()
                    op(ACT, "activation", [pa, rnnp], [r_], out=r_[:], in_=pa[:], func=AF.Sigmoid, bias=rnnp[:, c, 1:2])
                    op(ACT, "activation", [px, rnnp], [i_], out=i_[:], in_=px[:], func=AF.Sigmoid, bias=rnnp[:, c, 2:3])
                    op(ACT, "activation", [r_, c1], [a_], out=a_[:], in_=r_[:], func=AF.Exp, scale=c1[:, c, 0:1])
                    op(ACT, "activation", [r_, c1], [r_], out=r_[:], in_=r_[:], func=AF.Exp, scale=c1[:, c, 1:2])
                    op(ACT, "activation", [r_], [r_], out=r_[:], in_=r_[:], func=AF.Sqrt, scale=-1.0, bias=1.0)
                    op(DVE, "tensor_tensor", [i_, xc_], [i_], out=i_[:], in0=i_[:], in1=xc_[:], op=ALU.mult)
                    op(DVE, "tensor_tensor", [i_, r_], [i_], out=i_[:], in0=i_[:], in1=r_[:], op=ALU.mult)
                    h_ = hring.get()
                    op(DVE, "tensor_tensor_scan", [a_, i_, hlast], [h_], out=h_[:], data0=a_[:], data1=i_[:], initial=hlast[:, c:c + 1], op0=ALU.mult, op1=ALU.add)
                    op(DVE, "tensor_copy", [h_], [hlast], out=hlast[:, c:c + 1], in_=h_[:, 511:512])
                    op(DVE, "tensor_tensor", [g_, h_], [y4], out=y4[:, c, :], in0=g_[:], in1=h_[:], op=ALU.mult)
                    ys_ = ysq.get()
                    op(POOL, "tensor_tensor", [y4], [ys_], out=ys_[:], in0=y4[:, c, :], in1=y4[:, c, :], op=ALU.mult)
                    op(PE, "matmul", [ones_col, ys_], [psS], psS[0:1, :], lhsT=ones_col[:, 0:1], rhs=ys_[:], start=(c == 0), stop=(c == 3))
                op(DVE, "tensor_scalar", [psS], [ssrow], out=ssrow[:], in0=psS[0:1, :], scalar1=1.0 / 512, scalar2=EPS, op0=ALU.mult, op1=ALU.add)
                op(POOL, "tensor_tensor", [ssrow, neghalf], [rrow], out=rrow[:], in0=ssrow[:], in1=neghalf[0:1, :], op=ALU.pow)
                pb_ = psM.get()
                op(PE, "matmul", [ones_row, rrow], [pb_], pb_[:], lhsT=ones_row[:], rhs=rrow[:], start=True, stop=True)
                for c in range(4):
                    op(DVE, "scalar_tensor_tensor", [y4, rnnp, pb_], [yrnnT], out=yrnnT[:, c, tb * 512:(tb + 1) * 512], in0=y4[:, c, :], scalar=rnnp[:, c, 4:5], in1=pb_[:], op0=ALU.mult, op1=ALU.mult)
            fw.barrier()

        with ExitStack() as p2:
            cw1 = {}
            cw2 = {}
            for kd in ("k", "v"):
                cw1[kd] = fw.sb("cw1" + kd, [128, 32, 256], BF16, p2)
                for l0 in range(0, 32, 8):
                    dma(POOL, cw1[kd], [], [cw1[kd]], out=cw1[kd][:, l0:l0 + 8, :].rearrange("p l n -> p (l n)"), in_=cw1_d[kd][:, l0 * 256:(l0 + 8) * 256])
                cw2[kd] = fw.sb("cw2" + kd, [128, 2, 64], BF16, p2)
                dma(POOL, cw2[kd], [], [cw2[kd]], out=cw2[kd][:].rearrange("p c d -> p (c d)"), in_=cw2_d[kd][:, :])
            cposT = fw.sb("cposT", [128, 32], BF16, p2)
            dma(POOL, cposT, [], [cposT], out=cposT[:], in_=cposT_d[:, :])
            covf = fw.sb("covf", [128, 2, 64], F32, p2)
            dma(SP, covf, [], [covf], out=covf[:].rearrange("p c j -> p (c j)"), in_=cover_d[:, :])
            for g in range(2):
                op(DVE, "tensor_copy", [covf], [VcA], out=VcA[:, :, g, 64:128], in_=covf[:])
            psM = Ring([fw.ps("psM2", [128, 512], F32, p2) for _ in range(4)])
            psQ = fw.ps("psQ2", [128, 2, 128], BF16, p2)
            hidT = Ring([fw.sb("hidT", [128, 2, 256], BF16, p2) for _ in range(2)])
            cbias = fw.sb("cbias", [128, 4], F32, p2)
            kcf = [fw.sb("kcf", [128, 2, 64], F32, p2) for _ in range(2)]
            kcb = [fw.sb("kcb", [128, 2, 64], BF16, p2) for _ in range(2)]
            qn2 = fw.sb("qn2", [128, 2, 64], F32, p2)
            st2 = Ring([fw.sb("st2", [128, 12], F32, p2) for _ in range(6)])
            qw2 = fw.sb("qw2", [128, 2, 16], F32, p2)
            t12b = fw.sb("t12b", [128, 2, 16], F32, p2)
            t34b = fw.sb("t34b", [128, 2, 16], F32, p2)
            for ct in range(2):
                op(DVE, "memset", [], [kcf[ct]], kcf[ct][:], 0.0)
            op(DVE, "memset", [], [kcT], kcT[:], 0.0)
            pbias = psM.get()
            for ki, kd in enumerate(("k", "v")):
                for n_ in range(2):
                    col = ki * 2 + n_
                    for l in range(32):
                        op(PE, "matmul", [cw1[kd], cposT], [pbias], pbias[:, col:col + 1], lhsT=cw1[kd][0:64, l, n_ * 128:(n_ + 1) * 128], rhs=cposT[0:64, l:l + 1], start=(l == 0), stop=(l == 31))
            op(DVE, "tensor_copy", [pbias], [cbias], out=cbias[:], in_=pbias[:, 0:4])
            for ki, (kd, tok) in enumerate((("k", kcTok), ("v", vcTok))):
                for g in range(2):
                    hid = hidT.get()
                    for n_ in range(2):
                        ph = psM.get()
                        for l in range(32):
                            op(PE, "matmul", [cw1[kd], tok], [ph], ph[:, 0:255], lhsT=cw1[kd][64 * g:64 * g + 64, l, n_ * 128:(n_ + 1) * 128],
                               rhs=tok[64 * g:64 * g + 64, l:l + 16 * 254 + 1:16], start=(l == 0), stop=(l == 31))
                        op(ACT, "activation", [ph, cbias], [hid], out=hid[:, n_, 0:255], in_=ph[:, 0:255], func=AF.Gelu_apprx_tanh, bias=cbias[:, ki * 2 + n_:ki * 2 + n_ + 1])
                    for ct in range(2):
                        ncs = 128 if ct == 0 else 127
                        po = psM.get()
                        for n_ in range(2):
                            op(PE, "matmul", [hid, cw2[kd]], [po], po[0:ncs, 0:64], lhsT=hid[:, n_, ct * 128:ct * 128 + ncs], rhs=cw2[kd][:, n_, :], start=(n_ == 0), stop=(n_ == 1))
                        if kd == "k":
                            op(ACT, "activation", [po], [kcf[ct]], out=kcf[ct][0:ncs, g, :], in_=po[0:ncs, 0:64], func=AF.Copy)
                        else:
                            op(ACT, "activation", [po], [VcA], out=VcA[0:ncs, ct, g, 0:64], in_=po[0:ncs, 0:64], func=AF.Copy)
            for ct in range(2):
                norm_rope(kcf[ct], kcb[ct], 2, qkw, coscT[:, ct:ct + 1, :], sincT[:, ct:ct + 1, :], [coscT, sincT], qn2, st2, qw2, t12b, t34b, wo=8)
                op(PE, "transpose", [kcb[ct], identb], [psQ], out=psQ[:, ct, :], in_=kcb[ct][:].rearrange("p h d -> p (h d)"), identity=identb[:])
            op(ACT, "activation", [psQ], [kcT], out=kcT[:, 0:255], in_=psQ[:].rearrange("p c t -> p (c t)")[:, 0:255], func=AF.Copy)
            fw.barrier()
        tokst.close()
        if stop == 2:
            att.close()
            return nc

        qT = fw.sb("qT", [128, 4, T], BF16, att)
        ksT = fw.sb("ksT", [128, T], BF16, att)
        kwT = fw.sb("kwT", [128, T], BF16, att)
        Vs = fw.sb("Vs", [128, NT, 2, 65], BF16, att)
        Vw = fw.sb("Vw", [128, NT, 2, 65], BF16, att)
        gates = fw.sb("gates", [128, NT, 24], F32, att)
        op(POOL, "memset", [], [Vs], Vs[:, :, :, 64:65], 1.0)
        op(POOL, "memset", [], [Vw], Vw[:, :, :, 64:65], 1.0)
        with ExitStack() as p1:
            W = nt_work(p1)
            WinB = fw.sb("WinB", [128, 8, 1048], BF16, p1)
            for k in range(8):
                dma(POOL, WinB, [], [WinB], out=WinB[:, k, 0:512], in_=win_d[k * 128:(k + 1) * 128, 1024:1536])
                dma(POOL, WinB, [], [WinB], out=WinB[:, k, 512:1048], in_=win_d[k * 128:(k + 1) * 128, 1792:2328])
            psM = Ring([fw.ps("psM", [128, 512], F32, p1) for _ in range(4)])
            psQ = Ring([fw.ps("psQ", [128, 6, 128], BF16, p1) for _ in range(2)])
            qkf = Ring([fw.sb("qkf", [128, 12, 64], F32, p1) for _ in range(2)])
            qn = fw.sb("qn", [128, 12, 64], F32, p1)
            qkb = Ring([fw.sb("qkb", [128, 12, 64], BF16, p1) for _ in range(2)])
            st12 = Ring([fw.sb("st12", [128, 12], F32, p1) for _ in range(4)])
            qw16 = fw.sb("qw16", [128, 12, 16], F32, p1)
            t12 = fw.sb("t12", [128, 12, 16], F32, p1)
            t34 = fw.sb("t34", [128, 12, 16], F32, p1)
            for tb in range(8):
                hT = W["hT"].get()
                norm_transpose_block(tb, x_d, anw, W, hT)
                for j in range(4):
                    tt = tb * 4 + j
                    qf = qkf.get()
                    pA = psM.get()
                    for k in range(8):
                        op(PE, "matmul", [hT, WinB], [pA], pA[:, 0:512], lhsT=hT[:, k, j * 128:(j + 1) * 128], rhs=WinB[:, k, 0:512], start=(k == 0), stop=(k == 7))
                    op(ACT, "activation", [pA], [qf], out=qf[:, 0:8, :].rearrange("p (i g) d -> p i g d", g=2),
                       in_=pA[:, 0:512].rearrange("p (g i d) -> p i g d", g=2, i=4), func=AF.Copy)
                    pB = psM.get()
                    for k in range(8):
                        op(PE, "matmul", [hT, WinB], [pB], pB[:, 0:256], lhsT=hT[:, k, j * 128:(j + 1) * 128], rhs=WinB[:, k, 512:768], start=(k == 0), stop=(k == 7))
                    op(ACT, "activation", [pB], [qf], out=qf[:, 8:10, :].rearrange("p h d -> p (h d)"), in_=pB[:, 0:128], func=AF.Copy)
                    op(ACT, "activation", [pB], [Vs], out=Vs[:, tt, :, 0:64], in_=pB[:, 128:256].rearrange("p (g d) -> p g d", g=2), func=AF.Copy)
                    pC = psM.get()
                    for k in range(8):
                        op(PE, "matmul", [hT, WinB], [pC], pC[:, 0:280], lhsT=hT[:, k, j * 128:(j + 1) * 128], rhs=WinB[:, k, 768:1048], start=(k == 0), stop=(k == 7))
                    op(ACT, "activation", [pC], [qf], out=qf[:, 10:12, :].rearrange("p h d -> p (h d)"), in_=pC[:, 0:128], func=AF.Copy)
                    op(ACT, "activation", [pC], [Vw], out=Vw[:, tt, :, 0:64], in_=pC[:, 128:256].rearrange("p (g d) -> p g d", g=2), func=AF.Copy)
                    op(ACT, "activation", [pC], [gates], out=gates[:, tt, :], in_=pC[:, 256:280], func=AF.Sigmoid)
                    qb = qkb.get()
                    norm_rope(qf, qb, 12, qkw, cosT[:, tt:tt + 1, :], sinT[:, tt:tt + 1, :], [cosT, sinT], qn, st12, qw16, t12, t34)
                    pQ = psQ.get()
                    for i in range(6):
                        op(PE, "transpose", [qb, identb], [pQ], out=pQ[:, i, :], in_=qb[:, 2 * i:2 * i + 2, :].rearrange("p h d -> p (h d)"), identity=identb[:])
                    op(ACT, "activation", [pQ], [qT], out=qT[:, :, tt * 128:(tt + 1) * 128], in_=pQ[:, 0:4, :], func=AF.Copy)
                    op(ACT, "activation", [pQ], [ksT], out=ksT[:, tt * 128:(tt + 1) * 128], in_=pQ[:, 4, :], func=AF.Copy)
                    op(ACT, "activation", [pQ], [kwT], out=kwT[:, tt * 128:(tt + 1) * 128], in_=pQ[:, 5, :], func=AF.Copy)
            fw.barrier()

        if stop == "B":
            att.close()
            return nc
        with ExitStack() as p3:
            tri4 = fw.sb("tri4", [128, 4, 128], BF16, p3)
            wlow4 = fw.sb("wlow4", [128, 4, 128], BF16, p3)
            cmask = fw.sb("cmask", [128, 33, 128], BF16, p3)
            efull = fw.sb("efull", [128, T], BF16, p3)
            vmbs = fw.sb("vmbs", [128, 2, 128], F32, p3)
            wout = fw.sb("wout", [128, 8, D], BF16, p3)
            aonw = fw.sb("aonw", [128, 512], F32, p3)
            dma(SP, tri4, [], [tri4], out=tri4[:].rearrange("p h t -> p (h t)"), in_=tri4_d[:, :])
            dma(SP, wlow4, [], [wlow4], out=wlow4[:].rearrange("p h t -> p (h t)"), in_=wlow4_d[:, :])
            dma(SP, cmask, [], [cmask], out=cmask[:].rearrange("p m t -> p (m t)"), in_=cmask_d[:, :])
            dma(SP, efull, [], [efull], out=efull[:, :], in_=efull_d[:, :])
            dma(SP, vmbs, [], [vmbs], out=vmbs[:].rearrange("p a t -> p (a t)"), in_=vmbs_d[:, :])
            dma(SP, aonw, [], [aonw], out=aonw[:], in_=aonw_d[:, :])
            for k in range(8):
                dma(POOL, wout, [], [wout], out=wout[:, k, :], in_=wout_d[k * 128:(k + 1) * 128, :])
            psS = Ring([fw.ps("psS3", [128, 4, 128], F32, p3) for _ in range(3)])
            psC = fw.ps("psC", [128, 4, 128], F32, p3)
            psSel = fw.ps("psSel", [128, 4, 128], F32, p3)
            psWin = fw.ps("psWin", [128, 4, 128], F32, p3)
            psX = Ring([fw.ps("psX", [128, 512], F32, p3) for _ in range(1)])
            psTb = fw.ps("psTb", [128, 1024], BF16, p3)
            Er = Ring([fw.sb("E", [128, 4, 128], BF16, p3) for _ in range(4)])
            xr3 = Ring([fw.sb("xt3", [128, D], F32, p3) for _ in range(2)])
            ocmp = [fw.sb("ocmp", [128, 4, 64], F32, p3) for _ in range(2)]
            negT4 = [fw.sb("negT4", [128, 4, 128], BF16, p3) for _ in range(2)]
            sm = Ring([fw.sb("sm", [128, 16], F32, p3) for _ in range(12)])
            imp = fw.sb("imp", [128, 64], F32, p3)
            score = fw.sb("score", [128, 64], F32, p3)
            score2 = fw.sb("score2", [128, 64], F32, p3)
            negm = fw.sb("negm", [128, 64], BF16, p3)
            yatt = fw.sb("yatt", [128, 8, 64], F32, p3)
            ytmp = fw.sb("ytmp", [128, 4, 64], F32, p3)
            yb = fw.sb("yb", [128, 512], BF16, p3)
            yjunk = fw.sb("yjunk", [128, 512], BF16, p3)
            yattT = fw.sb("yattT", [128, 4, 128], BF16, p3)

            for g in range(2):
                op(POOL, "memset", [], [negT4[g]], negT4[g][:], 0.0)

            def qslice(g, qt):
                return qT[64 * g:64 * g + 64, :, qt * 128:(qt + 1) * 128]

            import os as _os
            _nqt = int(_os.environ.get('KQT', NT))
            _parts = _os.environ.get('KPARTS', 'ctuwseo')
            for qt in range(_nqt):
                xt = xr3.get()
                dma(SP, xt, [], [xt], out=xt[:], in_=x_d[qt * 128:(qt + 1) * 128, :])
                for g in range(2):
                    cts = [0] if qt < 16 else [0, 1]
                    for ct in (cts if 'c' in _parts else []):
                        ncs = 128 if ct == 0 else 127
                        mi = (qt if qt <= 16 else None) if ct == 0 else 17 + qt - 16
                        pS = psS.get()
                        op(PE, "matmul", [kcT, qT], [pS], pS[0:ncs, :, :], lhsT=kcT[64 * g:64 * g + 64, ct * 128:ct * 128 + ncs], rhs=qslice(g, qt), start=True, stop=(mi is None))
                        if mi is not None:
                            for h in range(4):
                                op(PE, "matmul", [identb, cmask], [pS], pS[0:ncs, h, :], lhsT=identb[0:ncs, 0:ncs], rhs=cmask[0:ncs, mi, :], start=False, stop=True)
                        E = Er.get()
                        op(ACT, "activation", [pS], [E], out=E[0:ncs], in_=pS[0:ncs], func=AF.Exp, scale=0.125)
                        for h in range(4):
                            op(PE, "matmul", [E, VcA], [psC], psC[:, h, :], lhsT=E[0:ncs, h, :], rhs=VcA[0:ncs, ct, g, :], start=(ct == cts[0] and h == 0), stop=(ct == cts[-1]), skip_group_check=True)
                    if 't' not in _parts:
                        continue
                    sums = sm.get()
                    op(DVE, "tensor_reduce", [psC], [sums], out=sums[:, 0:4], in_=psC[:, :, 64:128], axis=AX.X, op=ALU.add)
                    op(DVE, "tensor_scalar", [sums], [sums], out=sums[:, 4:8], in0=sums[:, 0:4], scalar1=1e-30, scalar2=None, op0=ALU.max)
                    op(DVE, "reciprocal", [sums], [sums], out=sums[:, 8:12], in_=sums[:, 4:8])
                    op(DVE, "tensor_tensor", [psC, sums], [ocmp[g]], out=ocmp[g][:], in0=psC[:, :, 0:64], in1=sums[:, 8:12].unsqueeze(2).broadcast_to([128, 4, 64]), op=ALU.mult)
                    op(DVE, "tensor_scalar", [psC, sums], [imp], out=imp[:], in0=psC[:, 0, 64:128], scalar1=sums[:, 8:9], scalar2=None, op0=ALU.mult)
                    for h in range(1, 4):
                        op(DVE, "scalar_tensor_tensor", [psC, sums, imp], [imp], out=imp[:], in0=psC[:, h, 64:128], scalar=sums[:, 8 + h:9 + h], in1=imp[:], op0=ALU.mult, op1=ALU.add)
                    lo = 64 - 2 * qt
                    op(DVE, "tensor_tensor", [imp, vmbs], [score], out=score[:], in0=imp[:], in1=vmbs[:, 0, lo:lo + 64], op=ALU.mult)
                    op(DVE, "tensor_tensor", [score, vmbs], [score], out=score[:], in0=score[:], in1=vmbs[:, 1, lo:lo + 64], op=ALU.add)
                    op(DVE, "memset", [], [score], score[:, 0:1], 1.0e4)
                    m8 = sm.get()
                    op(DVE, "max", [score], [m8], out=m8[:, 0:8], in_=score[:])
                    op(DVE, "match_replace", [m8, score], [score2], out=score2[:], in_to_replace=m8[:, 0:8], in_values=score[:], imm_value=-3.0e38)
                    op(DVE, "max", [score2], [m8], out=m8[:, 8:16], in_=score2[:])
                    op(DVE, "tensor_scalar", [score, m8], [negm], out=negm[:], in0=score[:], scalar1=m8[:, 15:16], scalar2=NEG, op0=ALU.is_lt, op1=ALU.mult)
                    if dbg and qt == 8 and g == 0:
                        for nm_, b_ in (("score", score), ("negm", negm), ("imp", imp), ("m8", m8), ("score2", score2)):
                            if nm_ in dbg_d:
                                dma(SP, b_, [b_], [], out=dbg_d[nm_], in_=b_[:])
                    if 'u' not in _parts:
                        continue
                    pN = psTb
                    pNb = psTb[:, 512:1024]
                    op(PE, "transpose", [negm, identb], [pN], out=pNb[0:64, 0:128], in_=negm[:], identity=identb[:])
                    op(DVE, "tensor_copy", [pN], [negT4[g]], out=negT4[g][0:64, 0, :], in_=pNb[0:64, 0:128])
                    op(POOL, "tensor_copy", [negT4[g]], [negT4[g]], out=negT4[g][0:64, 1:4, :], in_=negT4[g][0:64, 0:1, :].broadcast_to([64, 3, 128]))
                for g in range(2):
                    for (kT_, V_, pO, kts, issel) in ((kwT, Vw, psWin, list(range(max(0, qt - 4), qt + 1)), False), (ksT, Vs, psSel, list(range(qt + 1)), True)):
                        if ('s' if issel else 'w') not in _parts:
                            continue
                        for kt in kts:
                            extra = []
                            if issel and 'e' in _parts:
                                extra.append((efull[:, kt * 128:(kt + 1) * 128], negT4[g][:], [efull, negT4[g]]))
                            if kt == qt:
                                extra.append((identb[:], tri4[:], [identb, tri4]))
                            if (not issel) and kt == qt - 4:
                                extra.append((identb[:], wlow4[:], [identb, wlow4]))
                            pS = psS.get()
                            op(PE, "matmul", [kT_, qT], [pS], pS[:], lhsT=kT_[64 * g:64 * g + 64, kt * 128:(kt + 1) * 128], rhs=qslice(g, qt), start=True, stop=(not extra))
                            for ei, (l_, r_, bufs_) in enumerate(extra):
                                op(PE, "matmul", bufs_, [pS], pS[:], lhsT=l_, rhs=r_, start=False, stop=(ei == len(extra) - 1))
                            E = Er.get()
                            op(ACT, "activation", [pS], [E], out=E[:], in_=pS[:], func=AF.Exp, scale=0.125)
                            for h in range(4):
                                op(PE, "matmul", [E, V_], [pO], pO[:, h, 0:65], lhsT=E[:, h, :], rhs=V_[:, kt, g, :], start=(kt == kts[0] and h == 0), stop=(kt == kts[-1]), skip_group_check=True)
                    if 'o' not in _parts:
                        continue
                    cf = sm.get()
                    gv = gates[:, qt, g * 12:(g + 1) * 12].rearrange("p (h b) -> p b h", b=3)
                    op(DVE, "tensor_scalar", [psSel], [cf], out=cf[:, 0:4], in0=psSel[:, :, 64], scalar1=1e-30, scalar2=None, op0=ALU.max)
                    op(DVE, "tensor_scalar", [psWin], [cf], out=cf[:, 4:8], in0=psWin[:, :, 64], scalar1=1e-30, scalar2=None, op0=ALU.max)
                    op(DVE, "reciprocal", [cf], [cf], out=cf[:, 8:16], in_=cf[:, 0:8])
                    op(DVE, "tensor_tensor", [cf, gates], [cf], out=cf[:, 0:8].rearrange("p (b h) -> p b h", b=2), in0=cf[:, 8:16].rearrange("p (b h) -> p b h", b=2), in1=gv[:, 1:3, :], op=ALU.mult)
                    if dbg and qt == 8 and g == 0:
                        for nm_, b_ in (("psSel", psSel), ("psWin", psWin), ("ocmp", ocmp[0]), ("negT4", negT4[0])):
                            if nm_ in dbg_d:
                                tmpb = fw.sb("dbgtmp" + nm_, [128, 4, 128], F32, p3)
                                op(DVE, "tensor_copy", [b_], [tmpb], out=tmpb[:, :, 0:b_.t.shape[2]], in_=b_[:])
                                dma(SP, tmpb, [tmpb], [], out=dbg_d[nm_], in_=tmpb[:])
                    yg = yatt[:, g * 4:(g + 1) * 4, :]
                    op(DVE, "tensor_tensor", [ocmp[g], gates], [yatt], out=yg, in0=ocmp[g][:], in1=gv[:, 0, :].unsqueeze(2).broadcast_to([128, 4, 64]), op=ALU.mult)
                    op(DVE, "tensor_tensor", [psSel, cf], [ytmp], out=ytmp[:], in0=psSel[:, :, 0:64], in1=cf[:, 0:4].unsqueeze(2).broadcast_to([128, 4, 64]), op=ALU.mult)
                    op(DVE, "tensor_tensor", [yatt, ytmp], [yatt], out=yg, in0=yg, in1=ytmp[:], op=ALU.add)
                    op(DVE, "tensor_tensor", [psWin, cf], [ytmp], out=ytmp[:], in0=psWin[:, :, 0:64], in1=cf[:, 4:8].unsqueeze(2).broadcast_to([128, 4, 64]), op=ALU.mult)
                    op(DVE, "tensor_tensor", [yatt, ytmp], [yatt], out=yg, in0=yg, in1=ytmp[:], op=ALU.add)
                if 'o' not in _parts:
                    continue
                if dbg and qt == 8 and "yatt" in dbg_d:
                    dma(SP, yatt, [yatt], [], out=dbg_d["yatt"], in_=yatt[:])
                yf = yatt[:].rearrange("p h d -> p (h d)")
                st = sm.get()
                op(ACT, "activation", [yatt], [yjunk, st], out=yjunk[:], in_=yf, func=AF.Square, accum_out=st[:, 0:1])
                rsqrt_small(st[:, 0:1], [st], st, st[:, 2:3], 1, 1.0 / 512, sm.get())
                op(DVE, "scalar_tensor_tensor", [yatt, st, aonw], [yb], out=yb[:], in0=yf, scalar=st[:, 2:3], in1=aonw[:], op0=ALU.mult, op1=ALU.mult)
                pY = psTb
                pYb = psTb[:, 0:512]
                for c in range(4):
                    op(PE, "transpose", [yb, identb], [pY], out=pYb[:, c * 128:(c + 1) * 128], in_=yb[:, c * 128:(c + 1) * 128], identity=identb[:])
                op(ACT, "activation", [pY], [yattT], out=yattT[:].rearrange("p c t -> p (c t)"), in_=pYb[:, 0:512], func=AF.Copy)
                for half in range(2):
                    pO_ = psX.get()
                    for c in range(4):
                        op(PE, "matmul", [yrnnT, wout], [pO_], pO_[:], lhsT=yrnnT[:, c, qt * 128:(qt + 1) * 128], rhs=wout[:, c, half * 512:(half + 1) * 512], start=(c == 0), stop=False)
                    for c in range(4):
                        op(PE, "matmul", [yattT, wout], [pO_], pO_[:], lhsT=yattT[:, c, :], rhs=wout[:, 4 + c, half * 512:(half + 1) * 512], start=False, stop=(c == 3))
                    op(DVE, "tensor_tensor", [xt, pO_], [xt], out=xt[:, half * 512:(half + 1) * 512], in0=xt[:, half * 512:(half + 1) * 512], in1=pO_[:], op=ALU.add)
                dma(SP, xt, [xt], [ydr[qt]], out=y_d[qt * 128:(qt + 1) * 128, :], in_=xt[:])
            fw.barrier()
        att.close()
        if stop == 3:
            return nc

        with ExitStack() as p4:
            Wg = fw.sb("Wg", [128, 8, DFF], BF16, p4)
            Wu = fw.sb("Wu", [128, 8, DFF], BF16, p4)
            Wd = fw.sb("Wd", [128, NFF, D], BF16, p4)
            fnw = fw.sb("fnw", [128, D], F32, p4)
            dma(SP, fnw, [], [fnw], out=fnw[:], in_=fnw_d[:, :])
            for k in range(8):
                dma(POOL, Wg, [], [Wg], out=Wg[:, k, :], in_=wg_d[k * 128:(k + 1) * 128, :])
                dma(POOL, Wu, [], [Wu], out=Wu[:, k, :], in_=wu_d[k * 128:(k + 1) * 128, :])
            for j in range(NFF):
                dma(POOL, Wd, [], [Wd], out=Wd[:, j, :], in_=wd_d[j * 128:(j + 1) * 128, :])
            W = nt_work(p4, nx=5, nh=1)
            psG = Ring([fw.ps("psG", [128, 512], F32, p4) for _ in range(2)])
            psU = Ring([fw.ps("psU", [128, 512], F32, p4) for _ in range(2)])
            psD = Ring([fw.ps("psD", [128, 512], F32, p4) for _ in range(2)])
            sg = Ring([fw.sb("sg", [128, 512], F32, p4) for _ in range(2)])
            aT = fw.sb("aT", [128, NFF, 512], BF16, p4)
            for tb in range(8):
                hT = W["hT"].get()
                xts = norm_transpose_block(tb, y_d, fnw, W, hT, srcbufs=ydr)
                for j in range(NFF):
                    pg, pu = psG.get(), psU.get()
                    for k in range(8):
                        op(PE, "matmul", [Wg, hT], [pg], pg[:], lhsT=Wg[:, k, j * 128:(j + 1) * 128], rhs=hT[:, k, :], start=(k == 0), stop=(k == 7))
                    for k in range(8):
                        op(PE, "matmul", [Wu, hT], [pu], pu[:], lhsT=Wu[:, k, j * 128:(j + 1) * 128], rhs=hT[:, k, :], start=(k == 0), stop=(k == 7))
                    sg_ = sg.get()
                    op(ACT, "activation", [pg], [sg_], out=sg_[:], in_=pg[:], func=AF.Silu)
                    op(DVE, "tensor_tensor", [sg_, pu], [aT], out=aT[:, j, :], in0=sg_[:], in1=pu[:], op=ALU.mult)
                for i in range(4):
                    tt = tb * 4 + i
                    xt = xts[i]
                    for half in range(2):
                        pd = psD.get()
                        for j in range(NFF):
                            op(PE, "matmul", [aT, Wd], [pd], pd[:], lhsT=aT[:, j, i * 128:(i + 1) * 128], rhs=Wd[:, j, half * 512:(half + 1) * 512], start=(j == 0), stop=(j == NFF - 1))
                        op(DVE, "tensor_tensor", [xt, pd], [xt], out=xt[:, half * 512:(half + 1) * 512], in0=xt[:, half * 512:(half + 1) * 512], in1=pd[:], op=ALU.add)
                    dma(SP, xt, [xt], [ydr[tt]], out=y_d[tt * 128:(tt + 1) * 128, :], in_=xt[:])
            fw.barrier()
    return nc


def host_inputs(inp, b):
    f32 = np.float32
    m = {}
    m["x"] = np.ascontiguousarray(inp["x"][b], dtype=f32)
    pos = np.asarray(inp["positions"][b], dtype=np.int32)
    m["pos"] = np.ascontiguousarray(pos.reshape(NT, 128).T)
    pc = np.zeros(256, np.int32)
    pc[:255] = pos[np.arange(255) * 16 + 31]
    m["posc"] = np.ascontiguousarray(pc.reshape(2, 128).T)
    invf = (500000.0 ** (-np.arange(8, dtype=np.float32) * 2.0 / 16)).astype(f32)
    m["invf"] = np.ascontiguousarray(np.broadcast_to(invf, (128, 8)), dtype=f32)
    m["w_in"] = np.ascontiguousarray(inp["w_in"][0], dtype=f32)
    m["anw"] = np.ascontiguousarray(np.broadcast_to(inp["attn_norm_w"][0], (128, D)), dtype=f32)
    qkw = np.concatenate([np.tile(inp["q_norm_w"][0], 8), np.tile(inp["k_norm_w"][0], 4)])
    m["qkw"] = np.ascontiguousarray(np.broadcast_to(qkw, (128, 768)), dtype=f32)
    cw = inp["conv_w"][0].reshape(4, 4, 128)
    m["convw"] = np.ascontiguousarray(cw.transpose(2, 1, 0).reshape(128, 16), dtype=f32)
    rp = np.stack([inp["conv_b"][0], inp["gate_a_b"][0], inp["gate_x_b"][0], inp["lru_lambda"][0], inp["rnn_out_norm_w"][0]], 0)
    m["rnnp"] = np.ascontiguousarray(rp.reshape(5, 4, 128).transpose(2, 1, 0).reshape(128, 20), dtype=f32)
    for nm, key in (("gaw", "gate_a_w"), ("gxw", "gate_x_w")):
        w = inp[key][0]
        bd = np.zeros((128, 4, 128), f32)
        for c in range(4):
            for i in range(2):
                bd[64 * i:64 * i + 64, c, 64 * i:64 * i + 64] = w[2 * c + i]
        m[nm] = bd.reshape(128, 512)
    m["identf"] = np.eye(128, dtype=f32)
    for kd in ("k", "v"):
        w1 = inp["cmp_%s_w1" % kd][0].reshape(32, 64, 256).transpose(1, 0, 2).reshape(64, 32 * 256)
        m["cw1" + kd] = np.ascontiguousarray(np.concatenate([w1, w1], 0), dtype=f32)
        w2 = inp["cmp_%s_w2" % kd][0].reshape(2, 128, 64).transpose(1, 0, 2).reshape(128, 128)
        m["cw2" + kd] = np.ascontiguousarray(w2, dtype=f32)
    cp = inp["cmp_pos"][0].T
    m["cposT"] = np.ascontiguousarray(np.concatenate([cp, cp], 0), dtype=f32)
    m.update(CONSTS)
    m["w_out"] = np.ascontiguousarray(inp["w_out"][0], dtype=f32)
    m["aonw"] = np.ascontiguousarray(np.broadcast_to(inp["attn_out_norm_w"][0], (128, 512)), dtype=f32)
    m["fnw"] = np.ascontiguousarray(np.broadcast_to(inp["ffn_norm_w"][0], (128, D)), dtype=f32)
    m["w_gate"] = np.ascontiguousarray(inp["w_gate"][0], dtype=f32)
    m["w_up"] = np.ascontiguousarray(inp["w_up"][0], dtype=f32)
    m["w_down"] = np.ascontiguousarray(inp["w_down"][0], dtype=f32)
    return m


def _consts():
    bf = ml_dtypes.bfloat16
    f32 = np.float32
    c = {}
    kl = np.arange(128)[:, None]
    ql = np.arange(128)[None, :]
    tri = np.where(kl <= ql, 0.0, NEG).astype(f32)
    c["tri4"] = np.ascontiguousarray(np.tile(tri, (1, 4))).astype(bf)
    wl = np.where(kl > ql, 0.0, NEG).astype(f32)
    c["wlow4"] = np.ascontiguousarray(np.tile(wl, (1, 4))).astype(bf)
    cm = np.zeros((128, 33, 128), f32)
    for mi in range(33):
        ct, qt = (0, mi) if mi <= 16 else (1, 16 + mi - 17)
        dv = 128 * qt - 2048 * ct - 31
        cm[:, mi, :] = np.where(16 * kl - ql <= dv, 0.0, NEG)
    c["cmask"] = np.ascontiguousarray(cm.reshape(128, 33 * 128)).astype(bf)
    ef = (np.arange(T)[None, :] // 64 == np.arange(128)[:, None]).astype(f32)
    c["efull"] = np.ascontiguousarray(ef).astype(bf)
    cs = np.arange(255)[:, None] * 16
    bs = np.arange(64)[None, :] * 64
    cov = np.clip(np.minimum(cs + 32, bs + 64) - np.maximum(cs, bs), 0, None).astype(f32) / 32.0
    covp = np.zeros((256, 64), f32)
    covp[:255] = cov
    c["cover"] = np.ascontiguousarray(covp.reshape(2, 128, 64).transpose(1, 0, 2).reshape(128, 128))
    tl = np.arange(128)[:, None]
    jp = np.arange(128)[None, :] - 64
    cc = (tl >= 64).astype(np.int64)
    forced = (jp == cc) | (jp == cc - 1)
    invalid = jp > cc
    vm = np.where(forced | invalid, 0.0, 1.0).astype(f32)
    bsb = np.where(forced, 1.0e4, np.where(invalid, -1.0e30, 0.0)).astype(f32)
    c["vmbs"] = np.ascontiguousarray(np.concatenate([vm, bsb], 1))
    return c


CONSTS = _consts()


def kernel(**inputs):
    inp = {k: np.asarray(v) for k, v in inputs.items()}
    nc = build()
    in_maps = [host_inputs(inp, b) for b in range(8)]
    res = run_bass_kernel_spmd(nc, in_maps, core_ids=list(range(8)))
    return np.stack([r["y"] for r in res.results], 0).astype(np.float32)
```

```python
import numpy as np
import ml_dtypes
from contextlib import ExitStack
import concourse.bass as bass
import concourse.mybir as mybir
from concourse.bass_utils import run_bass_kernel_spmd

F32, BF16, I32 = mybir.dt.float32, mybir.dt.bfloat16, mybir.dt.int32
AF = mybir.ActivationFunctionType
ALU = mybir.AluOpType
AX = mybir.AxisListType

T = 4096
NT = 32
D = 1024
DIN = 2328
DFF = 2816
NFF = 22
EPS = 1e-6
NEG = -30000.0
TWO_PI = 6.283185307179586
PI = 3.141592653589793


class Buf:
    def __init__(self, fw, t, name):
        self.fw, self.t, self.name = fw, t, name
        self.w = None
        self.r = {}
        self.dsem = None
        self.dkey = None
        self.dcnt = 0

    def __getitem__(self, k):
        return self.t[k]


class Eng:
    def __init__(self, fw, name, eng, sem, key, selfwait):
        self.fw, self.name, self.eng, self.sem, self.key = fw, name, eng, sem, key
        self.selfwait = selfwait
        self.cnt = 0
        self.waited = {}

    def sync(self, reads, writes):
        waits = {}

        def need(ev, raw):
            if ev is None:
                return
            key, sem, val = ev
            if key == self.key and not (self.selfwait and raw):
                return
            if self.waited.get(key, 0) >= val:
                return
            if key not in waits or waits[key][1] < val:
                waits[key] = (sem, val)

        for b in reads:
            need(b.w, True)
        for b in writes:
            need(b.w, False)
            for ev in b.r.values():
                need(ev, False)
        for key, (sem, val) in waits.items():
            self.eng.wait_ge(sem, val)
            self.waited[key] = val

    def wait_ev(self, ev):
        key, sem, val = ev
        if self.waited.get(key, 0) >= val:
            return
        self.eng.wait_ge(sem, val)
        self.waited[key] = val


class FW:
    def __init__(self, nc, es):
        self.nc, self.es = nc, es
        self.nkey = 0
        self.engs = {}
        for name, eng, sw in (("PE", nc.tensor, False), ("ACT", nc.scalar, True),
                              ("DVE", nc.vector, True), ("POOL", nc.gpsimd, True),
                              ("SP", nc.sync, False)):
            sem = es.enter_context(nc.semaphore("sem_" + name))
            self.engs[name] = Eng(self, name, eng, sem, self.newkey(), sw)
        self.PE, self.ACT, self.DVE, self.POOL, self.SP = (self.engs[n] for n in ("PE", "ACT", "DVE", "POOL", "SP"))
        self.carriers = []
        self.nbuf = 0

    def newkey(self):
        self.nkey += 1
        return self.nkey

    def sb(self, name, shape, dt, es=None):
        es = es or self.es
        self.nbuf += 1
        t = es.enter_context(self.nc.sbuf_tensor("%s_%d" % (name, self.nbuf), list(shape), dt))
        return Buf(self, t, name)

    def ps(self, name, shape, dt, es=None):
        es = es or self.es
        self.nbuf += 1
        t = es.enter_context(self.nc.psum_tensor("%s_%d" % (name, self.nbuf), list(shape), dt))
        return Buf(self, t, name)

    def dram(self, ap, name):
        return Buf(self, ap, name)

    def op(self, E, meth, reads, writes, *a, **kw):
        E.sync(reads, writes)
        ins = getattr(E.eng, meth)(*a, **kw)
        E.cnt += 1
        ins.then_inc(E.sem, 1)
        ev = (E.key, E.sem, E.cnt)
        for b in reads:
            b.r[E.key] = ev
        for b in writes:
            b.w = ev
            b.r = {}
        return ins

    def dma(self, Q, carrier, reads, writes, out, in_, **kw):
        Q.sync(reads, writes)
        if carrier.dsem is None:
            carrier.dsem = self.es.enter_context(self.nc.semaphore("dsem_%s_%d" % (carrier.name, len(self.carriers))))
            carrier.dkey = self.newkey()
            self.carriers.append(carrier)
        ins = Q.eng.dma_start(out=out, in_=in_, **kw)
        carrier.dcnt += 16
        ins.then_inc(carrier.dsem, 16)
        ev = (carrier.dkey, carrier.dsem, carrier.dcnt)
        for b in reads:
            b.r[carrier.dkey] = ev
        for b in writes:
            b.w = ev
            b.r = {}
        return ins

    def barrier(self):
        SP = self.SP
        for E in self.engs.values():
            if E is not SP and E.cnt > 0:
                SP.wait_ev((E.key, E.sem, E.cnt))
        for c in self.carriers:
            if c.dcnt > 0:
                SP.wait_ev((c.dkey, c.dsem, c.dcnt))
        ins = SP.eng.nop()
        SP.cnt += 1
        ins.then_inc(SP.sem, 1)
        ev = (SP.key, SP.sem, SP.cnt)
        for E in self.engs.values():
            if E is not SP:
                E.wait_ev(ev)


class Ring:
    def __init__(self, bufs):
        self.bufs, self.i = bufs, 0

    def get(self):
        b = self.bufs[self.i % len(self.bufs)]
        self.i += 1
        return b


def build(dbg=None, stop=None):
    nc = bass.Bass("TRN2", target_bir_lowering=False)

    def din(name, shape, dt=F32):
        return nc.dram_tensor(name, list(shape), dt, kind="ExternalInput").ap()

    x_d = din("x", [T, D])
    pos_d = din("pos", [128, NT], I32)
    posc_d = din("posc", [128, 2], I32)
    invf_d = din("invf", [128, 8])
    win_d = din("w_in", [D, DIN])
    anw_d = din("anw", [128, D])
    qkw_d = din("qkw", [128, 12 * 64])
    convw_d = din("convw", [128, 16])
    rnnp_d = din("rnnp", [128, 20])
    gaw_d = din("gaw", [128, 512])
    gxw_d = din("gxw", [128, 512])
    identf_d = din("identf", [128, 128])
    cw1_d = {"k": din("cw1k", [128, 32 * 256]), "v": din("cw1v", [128, 32 * 256])}
    cw2_d = {"k": din("cw2k", [128, 128]), "v": din("cw2v", [128, 128])}
    cposT_d = din("cposT", [128, 32])
    cover_d = din("cover", [128, 128])
    tri4_d = din("tri4", [128, 512], BF16)
    wlow4_d = din("wlow4", [128, 512], BF16)
    cmask_d = din("cmask", [128, 33 * 128], BF16)
    efull_d = din("efull", [128, T], BF16)
    vmbs_d = din("vmbs", [128, 256])
    wout_d = din("w_out", [D, D])
    aonw_d = din("aonw", [128, 512])
    fnw_d = din("fnw", [128, D])
    wg_d = din("w_gate", [D, DFF])
    wu_d = din("w_up", [D, DFF])
    wd_d = din("w_down", [DFF, D])
    y_d = nc.dram_tensor("y", [T, D], F32, kind="ExternalOutput").ap()
    dbg_d = {}
    if dbg:
        for nm, shp, dt in dbg:
            dbg_d[nm] = nc.dram_tensor("dbg_" + nm, list(shp), dt, kind="ExternalOutput").ap()

    with ExitStack() as es:
        fw = FW(nc, es)
        PE, ACT, DVE, POOL, SP = fw.PE, fw.ACT, fw.DVE, fw.POOL, fw.SP
        op, dma = fw.op, fw.dma
        att = ExitStack()
        tokst = ExitStack()

        identf = fw.sb("identf", [128, 128], F32)
        identb = fw.sb("identb", [128, 128], BF16)
        dma(SP, identf, [], [identf], out=identf[:], in_=identf_d[:, :])
        op(DVE, "tensor_copy", [identf], [identb], out=identb[:], in_=identf[:])
        ones_col = fw.sb("ones_col", [128, 1], F32)
        ones_row = fw.sb("ones_row", [1, 128], F32)
        neghalf = fw.sb("neghalf", [128, 512], F32)
        op(DVE, "memset", [], [ones_col], ones_col[:], 1.0)
        op(DVE, "memset", [], [ones_row], ones_row[:], 1.0)
        op(DVE, "memset", [], [neghalf], neghalf[:], -0.5)

        anw = fw.sb("anw", [128, D], F32, att)
        dma(SP, anw, [], [anw], out=anw[:], in_=anw_d[:, :])
        qkw = fw.sb("qkw", [128, 12, 64], F32, att)
        dma(SP, qkw, [], [qkw], out=qkw[:].rearrange("p h d -> p (h d)"), in_=qkw_d[:, :])
        convw = fw.sb("convw", [128, 4, 4], F32, att)
        dma(SP, convw, [], [convw], out=convw[:].rearrange("p c k -> p (c k)"), in_=convw_d[:, :])
        rnnp = fw.sb("rnnp", [128, 4, 5], F32, att)
        dma(SP, rnnp, [], [rnnp], out=rnnp[:].rearrange("p c k -> p (c k)"), in_=rnnp_d[:, :])
        gaw = fw.sb("gaw", [128, 4, 128], BF16, att)
        gxw = fw.sb("gxw", [128, 4, 128], BF16, att)
        dma(POOL, gaw, [], [gaw], out=gaw[:].rearrange("p c k -> p (c k)"), in_=gaw_d[:, :])
        dma(POOL, gxw, [], [gxw], out=gxw[:].rearrange("p c k -> p (c k)"), in_=gxw_d[:, :])

        def rope_tables(pos_ap, n, name):
            cosT_ = fw.sb(name + "cos", [128, n, 16], F32, att)
            sinT_ = fw.sb(name + "sin", [128, n, 16], F32, att)
            with ExitStack() as tmp:
                posi = fw.sb(name + "posi", [128, n], I32, tmp)
                dma(SP, posi, [], [posi], out=posi[:], in_=pos_ap)
                invf = fw.sb(name + "invf", [128, 8], F32, tmp)
                dma(SP, invf, [], [invf], out=invf[:], in_=invf_d[:, :])
                posf = fw.sb(name + "posf", [128, n], F32, tmp)
                op(DVE, "tensor_copy", [posi], [posf], out=posf[:], in_=posi[:])
                ang = fw.sb(name + "ang", [128, n, 8], F32, tmp)
                op(DVE, "tensor_tensor", [posf, invf], [ang], out=ang[:],
                   in0=posf[:].unsqueeze(2).broadcast_to([128, n, 8]),
                   in1=invf[:].unsqueeze(1).broadcast_to([128, n, 8]), op=ALU.mult)
                a2 = fw.sb(name + "a2", [128, n * 8], F32, tmp)
                ki = fw.sb(name + "ki", [128, n * 8], I32, tmp)
                kf = fw.sb(name + "kf", [128, n * 8], F32, tmp)
                m = fw.sb(name + "m", [128, n * 8], F32, tmp)
                for tab, shift in ((cosT_, PI / 2), (sinT_, 0.0)):
                    angf = ang[:].rearrange("p n f -> p (n f)")
                    op(DVE, "tensor_scalar", [ang], [a2], out=a2[:], in0=angf, scalar1=shift, scalar2=None, op0=ALU.add)
                    op(DVE, "tensor_scalar", [a2], [kf], out=kf[:], in0=a2[:], scalar1=1.0 / TWO_PI, scalar2=None, op0=ALU.mult)
                    op(DVE, "tensor_copy", [kf], [ki], out=ki[:], in_=kf[:])
                    op(DVE, "tensor_copy", [ki], [kf], out=kf[:], in_=ki[:])
                    op(DVE, "scalar_tensor_tensor", [kf, a2], [a2], out=a2[:], in0=kf[:], scalar=-TWO_PI, in1=a2[:], op0=ALU.mult, op1=ALU.add)
                    op(DVE, "tensor_scalar", [a2], [m], out=m[:], in0=a2[:], scalar1=PI, scalar2=None, op0=ALU.is_gt)
                    op(DVE, "scalar_tensor_tensor", [m, a2], [a2], out=a2[:], in0=m[:], scalar=-TWO_PI, in1=a2[:], op0=ALU.mult, op1=ALU.add)
                    op(DVE, "tensor_scalar", [a2], [m], out=m[:], in0=a2[:], scalar1=-PI, scalar2=None, op0=ALU.is_lt)
                    op(DVE, "scalar_tensor_tensor", [m, a2], [a2], out=a2[:], in0=m[:], scalar=TWO_PI, in1=a2[:], op0=ALU.mult, op1=ALU.add)
                    op(DVE, "tensor_scalar", [a2], [a2], out=a2[:], in0=a2[:], scalar1=PI, scalar2=-PI, op0=ALU.min, op1=ALU.max)
                    a3 = a2[:].rearrange("p (n f) -> p n f", f=8)
                    op(ACT, "activation", [a2], [tab], out=tab[:, :, 0:8], in_=a3, func=AF.Sin)
                    op(ACT, "activation", [a2], [tab], out=tab[:, :, 8:16], in_=a3, func=AF.Sin)
                fw.barrier()
            return cosT_, sinT_

        cosT, sinT = rope_tables(pos_d[:, :], NT, "rp")
        coscT, sincT = rope_tables(posc_d[:, :], 2, "rc")

        def rsqrt_small(src_ap, srcbufs, dst, dst_ap, n, scale, tmp):
            op(DVE, "tensor_scalar", srcbufs, [tmp], out=tmp[:, 0:n], in0=src_ap, scalar1=scale, scalar2=EPS, op0=ALU.mult, op1=ALU.add)
            op(POOL, "tensor_tensor", [tmp, neghalf], [dst], out=dst_ap, in0=tmp[:, 0:n], in1=neghalf[:, 0:n], op=ALU.pow)

        def norm_rope(qf, qb, nh, wt, cos_ap, sin_ap, csbufs, qn, st12, qw16, t12, t34, wo=0):
            op(DVE, "tensor_tensor", [qf], [qn], out=qn[:, 0:nh, :], in0=qf[:, 0:nh, :], in1=qf[:, 0:nh, :], op=ALU.mult)
            s12 = st12.get()
            op(DVE, "tensor_reduce", [qn], [s12], out=s12[:, 0:nh], in_=qn[:, 0:nh, :], axis=AX.X, op=ALU.add)
            r12 = st12.get()
            rsqrt_small(s12[:, 0:nh], [s12], r12, r12[:, 0:nh], nh, 1.0 / 64, st12.get())
            op(DVE, "tensor_tensor", [qf, r12], [qn], out=qn[:, 0:nh, :], in0=qf[:, 0:nh, :], in1=r12[:, 0:nh].unsqueeze(2).broadcast_to([128, nh, 64]), op=ALU.mult)
            op(DVE, "tensor_tensor", [qn, wt], [qb], out=qb[:, 0:nh, :], in0=qn[:, 0:nh, :], in1=wt[:, wo:wo + nh, :], op=ALU.mult)
            op(DVE, "tensor_tensor", [qn, wt], [qw16], out=qw16[:, 0:nh, :], in0=qn[:, 0:nh, 0:16], in1=wt[:, wo:wo + nh, 0:16], op=ALU.mult)
            op(DVE, "tensor_tensor", [qw16] + csbufs, [t12], out=t12[:, 0:nh, :], in0=qw16[:, 0:nh, :], in1=cos_ap.broadcast_to([128, nh, 16]), op=ALU.mult)
            op(DVE, "tensor_tensor", [qw16] + csbufs, [t34], out=t34[:, 0:nh, :], in0=qw16[:, 0:nh, :], in1=sin_ap.broadcast_to([128, nh, 16]), op=ALU.mult)
            op(DVE, "tensor_tensor", [t12, t34], [qb], out=qb[:, 0:nh, 0:8], in0=t12[:, 0:nh, 0:8], in1=t34[:, 0:nh, 8:16], op=ALU.subtract)
            op(DVE, "tensor_tensor", [t12, t34], [qb], out=qb[:, 0:nh, 8:16], in0=t12[:, 0:nh, 8:16], in1=t34[:, 0:nh, 0:8], op=ALU.add)

        def norm_transpose_block(tb, src_d, wbc, W, hT, srcbufs=None):
            xts = []
            for j in range(4):
                tt = tb * 4 + j
                xt = W["x"].get()
                xts.append(xt)
                dma(SP, xt, [srcbufs[tt]] if srcbufs else [], [xt], out=xt[:], in_=src_d[tt * 128:(tt + 1) * 128, :])
                st = W["stat"].get()
                hb = W["hb"].get()
                op(ACT, "activation", [xt], [hb, st], out=hb[:], in_=xt[:], func=AF.Square, accum_out=st[:, 0:1])
                rsqrt_small(st[:, 0:1], [st], st, st[:, 2:3], 1, 1.0 / D, W["stmp"].get())
                op(DVE, "scalar_tensor_tensor", [xt, st, wbc], [hb], out=hb[:], in0=xt[:], scalar=st[:, 2:3], in1=wbc[:], op0=ALU.mult, op1=ALU.mult)
                pT = W["psT"].get()
                for k in range(8):
                    op(PE, "transpose", [hb, identb], [pT], out=pT[:, k * 128:(k + 1) * 128], in_=hb[:, k * 128:(k + 1) * 128], identity=identb[:])
                op(ACT, "activation", [pT], [hT], out=hT[:, :, j * 128:(j + 1) * 128], in_=pT[:].rearrange("p (k t) -> p k t", k=8), func=AF.Copy)
            return xts

        def nt_work(es_, nx=2, nh=2):
            return {
                "x": Ring([fw.sb("xt", [128, D], F32, es_) for _ in range(nx)]),
                "stat": Ring([fw.sb("stat", [128, 4], F32, es_) for _ in range(4)]),
                "stmp": Ring([fw.sb("stmp", [128, 12], F32, es_) for _ in range(4)]),
                "hb": Ring([fw.sb("hb", [128, D], BF16, es_) for _ in range(2)]),
                "psT": Ring([fw.ps("psT", [128, D], BF16, es_) for _ in range(2)]),
                "hT": Ring([fw.sb("hT", [128, 8, 512], BF16, es_) for _ in range(nh)]),
            }

        yrnnT = fw.sb("yrnnT", [128, 4, T], BF16, att)
        kcT = fw.sb("kcT", [128, 256], BF16, att)
        VcA = fw.sb("VcA", [128, 2, 2, 128], BF16, att)
        kcTok = fw.sb("kcTok", [128, T], BF16, tokst)
        vcTok = fw.sb("vcTok", [128, T], BF16, tokst)
        ydr = [fw.dram(None, "ydr%d" % i) for i in range(NT)]

        with ExitStack() as p1:
            W = nt_work(p1)
            WinA = fw.sb("WinA", [128, 8, 1280], BF16, p1)
            for k in range(8):
                dma(POOL, WinA, [], [WinA], out=WinA[:, k, 0:1024], in_=win_d[k * 128:(k + 1) * 128, 0:1024])
                dma(POOL, WinA, [], [WinA], out=WinA[:, k, 1024:1280], in_=win_d[k * 128:(k + 1) * 128, 1536:1792])
            psM = Ring([fw.ps("psM", [128, 512], F32, p1) for _ in range(4)])
            psS = fw.ps("psS", [128, 512], F32, p1)
            xpad = [fw.sb("xpad", [128, 515], F32, p1) for _ in range(4)]
            gel = Ring([fw.sb("gel", [128, 512], F32, p1) for _ in range(2)])
            xc = Ring([fw.sb("xc", [128, 512], F32, p1) for _ in range(2)])
            xcb = Ring([fw.sb("xcb", [128, 512], BF16, p1) for _ in range(2)])
            rr = Ring([fw.sb("rr", [128, 512], F32, p1) for _ in range(2)])
            ii = Ring([fw.sb("ii", [128, 512], F32, p1) for _ in range(2)])
            aa = Ring([fw.sb("aa", [128, 512], F32, p1) for _ in range(2)])
            hring = Ring([fw.sb("hh", [128, 512], F32, p1) for _ in range(2)])
            hlast = fw.sb("hlast", [128, 4], F32, p1)
            y4 = fw.sb("y4", [128, 4, 512], F32, p1)
            ysq = Ring([fw.sb("ysq", [128, 512], F32, p1) for _ in range(2)])
            ssrow = fw.sb("ssrow", [1, 512], F32, p1)
            rrow = fw.sb("rrow", [1, 512], F32, p1)
            c1 = fw.sb("c1", [128, 4, 2], F32, p1)
            c1t = fw.sb("c1t", [128, 4], F32, p1)

            for c in range(4):
                op(DVE, "memset", [], [xpad[c]], xpad[c][:], 0.0)
            op(DVE, "memset", [], [hlast], hlast[:], 0.0)
            op(ACT, "activation", [rnnp], [c1t], out=c1t[:], in_=rnnp[:, :, 3], func=AF.Exp, scale=-1.0)
            op(ACT, "activation", [c1t], [c1t], out=c1t[:], in_=c1t[:], func=AF.Ln, bias=1.0)
            op(DVE, "tensor_scalar", [c1t], [c1], out=c1[:, :, 0], in0=c1t[:], scalar1=-8.0, scalar2=None, op0=ALU.mult)
            op(DVE, "tensor_scalar", [c1t], [c1], out=c1[:, :, 1], in0=c1t[:], scalar1=-16.0, scalar2=None, op0=ALU.mult)

            for tb in range(8):
                hT = W["hT"].get()
                norm_transpose_block(tb, x_d, anw, W, hT)
                for dst, c0 in ((kcTok, 1024), (vcTok, 1152)):
                    pF = psM.get()
                    for k in range(8):
                        op(PE, "matmul", [hT, WinA], [pF], pF[:], lhsT=WinA[:, k, c0:c0 + 128], rhs=hT[:, k, :], start=(k == 0), stop=(k == 7))
                    op(ACT, "activation", [pF], [dst], out=dst[:, tb * 512:(tb + 1) * 512], in_=pF[:], func=AF.Copy)
                for c in range(4):
                    pX = psM.get()
                    for k in range(8):
                        op(PE, "matmul", [hT, WinA], [pX], pX[:], lhsT=WinA[:, k, c * 128:(c + 1) * 128], rhs=hT[:, k, :], start=(k == 0), stop=(k == 7))
                    xp = xpad[c]
                    if tb > 0:
                        op(DVE, "tensor_copy", [xp], [xp], out=xp[:, 0:3], in_=xp[:, 512:515])
                    op(ACT, "activation", [pX], [xp], out=xp[:, 3:515], in_=pX[:], func=AF.Copy)
                    pG = psM.get()
                    for k in range(8):
                        op(PE, "matmul", [hT, WinA], [pG], pG[:], lhsT=WinA[:, k, 512 + c * 128:512 + (c + 1) * 128], rhs=hT[:, k, :], start=(k == 0), stop=(k == 7))
                    g_ = gel.get()
                    op(ACT, "activation", [pG], [g_], out=g_[:], in_=pG[:], func=AF.Gelu_apprx_tanh)
                    xc_ = xc.get()
                    op(DVE, "tensor_scalar", [xp, convw, rnnp], [xc_], out=xc_[:], in0=xp[:, 0:512], scalar1=convw[:, c, 0:1], scalar2=rnnp[:, c, 0:1], op0=ALU.mult, op1=ALU.add)
                    for k in range(1, 4):
                        op(DVE, "scalar_tensor_tensor", [xp, convw, xc_], [xc_], out=xc_[:], in0=xp[:, k:k + 512], scalar=convw[:, c, k:k + 1], in1=xc_[:], op0=ALU.mult, op1=ALU.add)
                    xcb_ = xcb.get()
                    op(POOL, "tensor_copy", [xc_], [xcb_], out=xcb_[:], in_=xc_[:])
                    pa = psM.get()
                    op(PE, "matmul", [gaw, xcb_], [pa], pa[:], lhsT=gaw[:, c, :], rhs=xcb_[:], start=True, stop=True)
                    px = psM.get()
                    op(PE, "matmul", [gxw, xcb_], [px], px[:], lhsT=gxw[:, c, :], rhs=xcb_[:], start=True, stop=True)
                    r_, i_, a_ = rr.get(), ii.get(), aa.get()
                    op(ACT, "activation", [pa, rnnp], [r_], out=r_[:], in_=pa[:], func=AF.Sigmoid, bias=rnnp[:, c, 1:2])
                    op(ACT, "activation", [px, rnnp], [i_], out=i_[:], in_=px[:], func=AF.Sigmoid, bias=rnnp[:, c, 2:3])
                    op(ACT, "activation", [r_, c1], [a_], out=a_[:], in_=r_[:], func=AF.Exp, scale=c1[:, c, 0:1])
                    op(ACT, "activation", [r_, c1], [r_], out=r_[:], in_=r_[:], func=AF.Exp, scale=c1[:, c, 1:2])
                    op(ACT, "activation", [r_], [r_], out=r_[:], in_=r_[:], func=AF.Sqrt, scale=-1.0, bias=1.0)
                    op(DVE, "tensor_tensor", [i_, xc_], [i_], out=i_[:], in0=i_[:], in1=xc_[:], op=ALU.mult)
                    op(DVE, "tensor_tensor", [i_, r_], [i_], out=i_[:], in0=i_[:], in1=r_[:], op=ALU.mult)
                    h_ = hring.get()
                    op(DVE, "tensor_tensor_scan", [a_, i_, hlast], [h_], out=h_[:], data0=a_[:], data1=i_[:], initial=hlast[:, c:c + 1], op0=ALU.mult, op1=ALU.add)
                    op(DVE, "tensor_copy", [h_], [hlast], out=hlast[:, c:c + 1], in_=h_[:, 511:512])
                    op(DVE, "tensor_tensor", [g_, h_], [y4], out=y4[:, c, :], in0=g_[:], in1=h_[:], op=ALU.mult)
                    ys_ = ysq.get()
                    op(POOL, "tensor_tensor", [y4], [ys_], out=ys_[:], in0=y4[:, c, :], in1=y4[:, c, :], op=ALU.mult)
                    op(PE, "matmul", [ones_col, ys_], [psS], psS[0:1, :], lhsT=ones_col[:, 0:1], rhs=ys_[:], start=(c == 0), stop=(c == 3))
                op(DVE, "tensor_scalar", [psS], [ssrow], out=ssrow[:], in0=psS[0:1, :], scalar1=1.0 / 512, scalar2=EPS, op0=ALU.mult, op1=ALU.add)
                op(ACT, "activation", [ssrow], [ssrow], out=ssrow[:], in_=ssrow[:], func=AF.Sqrt)
                op(DVE, "reciprocal", [ssrow], [rrow], out=rrow[:], in_=ssrow[:])
                pb_ = psM.get()
                op(PE, "matmul", [ones_row, rrow], [pb_], pb_[:], lhsT=ones_row[:], rhs=rrow[:], start=True, stop=True)
                for c in range(4):
                    op(DVE, "scalar_tensor_tensor", [y4, rnnp, pb_], [yrnnT], out=yrnnT[:, c, tb * 512:(tb + 1) * 512], in0=y4[:, c, :], scalar=rnnp[:, c, 4:5], in1=pb_[:], op0=ALU.mult, op1=ALU.mult)
            fw.barrier()

        with ExitStack() as p2:
            cw1 = {}
            cw2 = {}
            for kd in ("k", "v"):
                cw1[kd] = fw.sb("cw1" + kd, [128, 32, 256], BF16, p2)
                for l0 in range(0, 32, 8):
                    dma(POOL, cw1[kd], [], [cw1[kd]], out=cw1[kd][:, l0:l0 + 8, :].rearrange("p l n -> p (l n)"), in_=cw1_d[kd][:, l0 * 256:(l0 + 8) * 256])
                cw2[kd] = fw.sb("cw2" + kd, [128, 2, 64], BF16, p2)
                dma(POOL, cw2[kd], [], [cw2[kd]], out=cw2[kd][:].rearrange("p c d -> p (c d)"), in_=cw2_d[kd][:, :])
            cposT = fw.sb("cposT", [128, 32], BF16, p2)
            dma(POOL, cposT, [], [cposT], out=cposT[:], in_=cposT_d[:, :])
            covf = fw.sb("covf", [128, 2, 64], F32, p2)
            dma(SP, covf, [], [covf], out=covf[:].rearrange("p c j -> p (c j)"), in_=cover_d[:, :])
            for g in range(2):
                op(DVE, "tensor_copy", [covf], [VcA], out=VcA[:, :, g, 64:128], in_=covf[:])
            psM = Ring([fw.ps("psM2", [128, 512], F32, p2) for _ in range(4)])
            psQ = fw.ps("psQ2", [128, 2, 128], BF16, p2)
            hidT = Ring([fw.sb("hidT", [128, 2, 256], BF16, p2) for _ in range(2)])
            cbias = fw.sb("cbias", [128, 4], F32, p2)
            kcf = [fw.sb("kcf", [128, 2, 64], F32, p2) for _ in range(2)]
            kcb = [fw.sb("kcb", [128, 2, 64], BF16, p2) for _ in range(2)]
            qn2 = fw.sb("qn2", [128, 2, 64], F32, p2)
            st2 = Ring([fw.sb("st2", [128, 12], F32, p2) for _ in range(6)])
            qw2 = fw.sb("qw2", [128, 2, 16], F32, p2)
            t12b = fw.sb("t12b", [128, 2, 16], F32, p2)
            t34b = fw.sb("t34b", [128, 2, 16], F32, p2)
            for ct in range(2):
                op(DVE, "memset", [], [kcf[ct]], kcf[ct][:], 0.0)
            op(DVE, "memset", [], [kcT], kcT[:], 0.0)
            pbias = psM.get()
            for ki, kd in enumerate(("k", "v")):
                for n_ in range(2):
                    col = ki * 2 + n_
                    for l in range(32):
                        op(PE, "matmul", [cw1[kd], cposT], [pbias], pbias[:, col:col + 1], lhsT=cw1[kd][0:64, l, n_ * 128:(n_ + 1) * 128], rhs=cposT[0:64, l:l + 1], start=(l == 0), stop=(l == 31))
            op(DVE, "tensor_copy", [pbias], [cbias], out=cbias[:], in_=pbias[:, 0:4])
            for ki, (kd, tok) in enumerate((("k", kcTok), ("v", vcTok))):
                for g in range(2):
                    hid = hidT.get()
                    for n_ in range(2):
                        ph = psM.get()
                        for l in range(32):
                            op(PE, "matmul", [cw1[kd], tok], [ph], ph[:, 0:255], lhsT=cw1[kd][64 * g:64 * g + 64, l, n_ * 128:(n_ + 1) * 128],
                               rhs=tok[64 * g:64 * g + 64, l:l + 16 * 254 + 1:16], start=(l == 0), stop=(l == 31))
                        op(ACT, "activation", [ph, cbias], [hid], out=hid[:, n_, 0:255], in_=ph[:, 0:255], func=AF.Gelu_apprx_tanh, bias=cbias[:, ki * 2 + n_:ki * 2 + n_ + 1])
                    for ct in range(2):
                        ncs = 128 if ct == 0 else 127
                        po = psM.get()
                        for n_ in range(2):
                            op(PE, "matmul", [hid, cw2[kd]], [po], po[0:ncs, 0:64], lhsT=hid[:, n_, ct * 128:ct * 128 + ncs], rhs=cw2[kd][:, n_, :], start=(n_ == 0), stop=(n_ == 1))
                        if kd == "k":
                            op(ACT, "activation", [po], [kcf[ct]], out=kcf[ct][0:ncs, g, :], in_=po[0:ncs, 0:64], func=AF.Copy)
                        else:
                            op(ACT, "activation", [po], [VcA], out=VcA[0:ncs, ct, g, 0:64], in_=po[0:ncs, 0:64], func=AF.Copy)
            for ct in range(2):
                norm_rope(kcf[ct], kcb[ct], 2, qkw, coscT[:, ct:ct + 1, :], sincT[:, ct:ct + 1, :], [coscT, sincT], qn2, st2, qw2, t12b, t34b, wo=8)
                op(PE, "transpose", [kcb[ct], identb], [psQ], out=psQ[:, ct, :], in_=kcb[ct][:].rearrange("p h d -> p (h d)"), identity=identb[:])
            op(ACT, "activation", [psQ], [kcT], out=kcT[:, 0:255], in_=psQ[:].rearrange("p c t -> p (c t)")[:, 0:255], func=AF.Copy)
            fw.barrier()
        tokst.close()
        if stop == 2:
            att.close()
            return nc

        qT = fw.sb("qT", [128, 4, T], BF16, att)
        ksT = fw.sb("ksT", [128, T], BF16, att)
        kwT = fw.sb("kwT", [128, T], BF16, att)
        Vs = fw.sb("Vs", [128, NT, 2, 65], BF16, att)
        Vw = fw.sb("Vw", [128, NT, 2, 65], BF16, att)
        gates = fw.sb("gates", [128, NT, 24], F32, att)
        op(POOL, "memset", [], [Vs], Vs[:, :, :, 64:65], 1.0)
        op(POOL, "memset", [], [Vw], Vw[:, :, :, 64:65], 1.0)
        with ExitStack() as p1:
            W = nt_work(p1)
            WinB = fw.sb("WinB", [128, 8, 1048], BF16, p1)
            for k in range(8):
                dma(POOL, WinB, [], [WinB], out=WinB[:, k, 0:512], in_=win_d[k * 128:(k + 1) * 128, 1024:1536])
                dma(POOL, WinB, [], [WinB], out=WinB[:, k, 512:1048], in_=win_d[k * 128:(k + 1) * 128, 1792:2328])
            psM = Ring([fw.ps("psM", [128, 512], F32, p1) for _ in range(4)])
            psQ = Ring([fw.ps("psQ", [128, 6, 128], BF16, p1) for _ in range(2)])
            qkf = Ring([fw.sb("qkf", [128, 12, 64], F32, p1) for _ in range(2)])
            qn = fw.sb("qn", [128, 12, 64], F32, p1)
            qkb = Ring([fw.sb("qkb", [128, 12, 64], BF16, p1) for _ in range(2)])
            st12 = Ring([fw.sb("st12", [128, 12], F32, p1) for _ in range(4)])
            qw16 = fw.sb("qw16", [128, 12, 16], F32, p1)
            t12 = fw.sb("t12", [128, 12, 16], F32, p1)
            t34 = fw.sb("t34", [128, 12, 16], F32, p1)
            for tb in range(8):
                hT = W["hT"].get()
                norm_transpose_block(tb, x_d, anw, W, hT)
                for j in range(4):
                    tt = tb * 4 + j
                    qf = qkf.get()
                    pA = psM.get()
                    for k in range(8):
                        op(PE, "matmul", [hT, WinB], [pA], pA[:, 0:512], lhsT=hT[:, k, j * 128:(j + 1) * 128], rhs=WinB[:, k, 0:512], start=(k == 0), stop=(k == 7))
                    op(ACT, "activation", [pA], [qf], out=qf[:, 0:8, :].rearrange("p (i g) d -> p i g d", g=2),
                       in_=pA[:, 0:512].rearrange("p (g i d) -> p i g d", g=2, i=4), func=AF.Copy)
                    pB = psM.get()
                    for k in range(8):
                        op(PE, "matmul", [hT, WinB], [pB], pB[:, 0:256], lhsT=hT[:, k, j * 128:(j + 1) * 128], rhs=WinB[:, k, 512:768], start=(k == 0), stop=(k == 7))
                    op(ACT, "activation", [pB], [qf], out=qf[:, 8:10, :].rearrange("p h d -> p (h d)"), in_=pB[:, 0:128], func=AF.Copy)
                    op(ACT, "activation", [pB], [Vs], out=Vs[:, tt, :, 0:64], in_=pB[:, 128:256].rearrange("p (g d) -> p g d", g=2), func=AF.Copy)
                    pC = psM.get()
                    for k in range(8):
                        op(PE, "matmul", [hT, WinB], [pC], pC[:, 0:280], lhsT=hT[:, k, j * 128:(j + 1) * 128], rhs=WinB[:, k, 768:1048], start=(k == 0), stop=(k == 7))
                    op(ACT, "activation", [pC], [qf], out=qf[:, 10:12, :].rearrange("p h d -> p (h d)"), in_=pC[:, 0:128], func=AF.Copy)
                    op(ACT, "activation", [pC], [Vw], out=Vw[:, tt, :, 0:64], in_=pC[:, 128:256].rearrange("p (g d) -> p g d", g=2), func=AF.Copy)
                    op(ACT, "activation", [pC], [gates], out=gates[:, tt, :], in_=pC[:, 256:280], func=AF.Sigmoid)
                    qb = qkb.get()
                    norm_rope(qf, qb, 12, qkw, cosT[:, tt:tt + 1, :], sinT[:, tt:tt + 1, :], [cosT, sinT], qn, st12, qw16, t12, t34)
                    pQ = psQ.get()
                    for i in range(6):
                        op(PE, "transpose", [qb, identb], [pQ], out=pQ[:, i, :], in_=qb[:, 2 * i:2 * i + 2, :].rearrange("p h d -> p (h d)"), identity=identb[:])
                    op(ACT, "activation", [pQ], [qT], out=qT[:, :, tt * 128:(tt + 1) * 128], in_=pQ[:, 0:4, :], func=AF.Copy)
                    op(ACT, "activation", [pQ], [ksT], out=ksT[:, tt * 128:(tt + 1) * 128], in_=pQ[:, 4, :], func=AF.Copy)
                    op(ACT, "activation", [pQ], [kwT], out=kwT[:, tt * 128:(tt + 1) * 128], in_=pQ[:, 5, :], func=AF.Copy)
            fw.barrier()

        if stop == "B":
            att.close()
            return nc
        with ExitStack() as p3:
            tri4 = fw.sb("tri4", [128, 4, 128], BF16, p3)
            wlow4 = fw.sb("wlow4", [128, 4, 128], BF16, p3)
            cmask = fw.sb("cmask", [128, 33, 128], BF16, p3)
            efull = fw.sb("efull", [128, T], BF16, p3)
            vmbs = fw.sb("vmbs", [128, 2, 128], F32, p3)
            wout = fw.sb("wout", [128, 8, D], BF16, p3)
            aonw = fw.sb("aonw", [128, 512], F32, p3)
            dma(SP, tri4, [], [tri4], out=tri4[:].rearrange("p h t -> p (h t)"), in_=tri4_d[:, :])
            dma(SP, wlow4, [], [wlow4], out=wlow4[:].rearrange("p h t -> p (h t)"), in_=wlow4_d[:, :])
            dma(SP, cmask, [], [cmask], out=cmask[:].rearrange("p m t -> p (m t)"), in_=cmask_d[:, :])
            dma(SP, efull, [], [efull], out=efull[:, :], in_=efull_d[:, :])
            dma(SP, vmbs, [], [vmbs], out=vmbs[:].rearrange("p a t -> p (a t)"), in_=vmbs_d[:, :])
            dma(SP, aonw, [], [aonw], out=aonw[:], in_=aonw_d[:, :])
            for k in range(8):
                dma(POOL, wout, [], [wout], out=wout[:, k, :], in_=wout_d[k * 128:(k + 1) * 128, :])
            psS = Ring([fw.ps("psS3", [128, 4, 128], F32, p3) for _ in range(3)])
            psC = fw.ps("psC", [128, 4, 128], F32, p3)
            psSel = fw.ps("psSel", [128, 4, 128], F32, p3)
            psWin = fw.ps("psWin", [128, 4, 128], F32, p3)
            psX = Ring([fw.ps("psX", [128, 512], F32, p3) for _ in range(1)])
            psTb = fw.ps("psTb", [128, 1024], BF16, p3)
            Er = Ring([fw.sb("E", [128, 4, 128], BF16, p3) for _ in range(4)])
            xr3 = Ring([fw.sb("xt3", [128, D], F32, p3) for _ in range(2)])
            ocmp = [fw.sb("ocmp", [128, 4, 64], F32, p3) for _ in range(2)]
            negT4 = [fw.sb("negT4", [128, 4, 128], BF16, p3) for _ in range(2)]
            sm = Ring([fw.sb("sm", [128, 16], F32, p3) for _ in range(12)])
            imp = fw.sb("imp", [128, 64], F32, p3)
            score = fw.sb("score", [128, 64], F32, p3)
            score2 = fw.sb("score2", [128, 64], F32, p3)
            negm = fw.sb("negm", [128, 64], BF16, p3)
            yatt = fw.sb("yatt", [128, 8, 64], F32, p3)
            ytmp = fw.sb("ytmp", [128, 4, 64], F32, p3)
            yb = fw.sb("yb", [128, 512], BF16, p3)
            yjunk = fw.sb("yjunk", [128, 512], BF16, p3)
            yattT = fw.sb("yattT", [128, 4, 128], BF16, p3)

            for g in range(2):
                op(POOL, "memset", [], [negT4[g]], negT4[g][:], 0.0)

            def qslice(g, qt):
                return qT[64 * g:64 * g + 64, :, qt * 128:(qt + 1) * 128]

            ytr = Ring([fw.sb("ytmp", [128, 4, 64], F32, p3) for _ in range(2)])
            xts3 = {}

            def gview(qt, g):
                return gates[:, qt, g * 12:(g + 1) * 12].rearrange("p (h b) -> p b h", b=3)

            def topk(qt, g):
                gv = gview(qt, g)
                sums = sm.get()
                op(DVE, "tensor_reduce", [psC], [sums], out=sums[:, 0:4], in_=psC[:, :, 64:128], axis=AX.X, op=ALU.add)
                op(DVE, "tensor_scalar", [sums], [sums], out=sums[:, 4:8], in0=sums[:, 0:4], scalar1=1e-30, scalar2=None, op0=ALU.max)
                op(DVE, "reciprocal", [sums], [sums], out=sums[:, 8:12], in_=sums[:, 4:8])
                op(DVE, "tensor_tensor", [sums, gates], [sums], out=sums[:, 12:16], in0=sums[:, 8:12], in1=gv[:, 0, :], op=ALU.mult)
                op(DVE, "tensor_tensor", [psC, sums], [yatt], out=yatt[:, g * 4:(g + 1) * 4, :], in0=psC[:, :, 0:64], in1=sums[:, 12:16].unsqueeze(2).broadcast_to([128, 4, 64]), op=ALU.mult)
                op(DVE, "tensor_scalar", [psC, sums], [imp], out=imp[:], in0=psC[:, 0, 64:128], scalar1=sums[:, 8:9], scalar2=None, op0=ALU.mult)
                for h in range(1, 4):
                    op(DVE, "scalar_tensor_tensor", [psC, sums, imp], [imp], out=imp[:], in0=psC[:, h, 64:128], scalar=sums[:, 8 + h:9 + h], in1=imp[:], op0=ALU.mult, op1=ALU.add)
                lo = 64 - 2 * qt
                op(DVE, "tensor_tensor", [imp, vmbs], [score], out=score[:], in0=imp[:], in1=vmbs[:, 0, lo:lo + 64], op=ALU.mult)
                op(DVE, "tensor_tensor", [score, vmbs], [score], out=score[:], in0=score[:], in1=vmbs[:, 1, lo:lo + 64], op=ALU.add)
                op(DVE, "memset", [], [score], score[:, 0:1], 1.0e4)
                m8 = sm.get()
                op(DVE, "max", [score], [m8], out=m8[:, 0:8], in_=score[:])
                op(DVE, "match_replace", [m8, score], [score2], out=score2[:], in_to_replace=m8[:, 0:8], in_values=score[:], imm_value=-3.0e38)
                op(DVE, "max", [score2], [m8], out=m8[:, 8:16], in_=score2[:])
                op(DVE, "tensor_scalar", [score, m8], [negm], out=negm[:], in0=score[:], scalar1=m8[:, 15:16], scalar2=NEG, op0=ALU.is_lt, op1=ALU.mult)
                pNb = psTb[:, 512:1024]
                op(PE, "transpose", [negm, identb], [psTb], out=pNb[0:64, 0:128], in_=negm[:], identity=identb[:])
                op(DVE, "tensor_copy", [psTb], [negT4[g]], out=negT4[g][0:64, 0, :], in_=pNb[0:64, 0:128])
                op(POOL, "tensor_copy", [negT4[g]], [negT4[g]], out=negT4[g][0:64, 1:4, :], in_=negT4[g][0:64, 0:1, :].broadcast_to([64, 3, 128]))

            def evac(qt, g, pO, b):
                gv = gview(qt, g)
                cf = sm.get()
                op(DVE, "tensor_scalar", [pO], [cf], out=cf[:, 0:4], in0=pO[:, :, 64], scalar1=1e-30, scalar2=None, op0=ALU.max)
                op(DVE, "reciprocal", [cf], [cf], out=cf[:, 4:8], in_=cf[:, 0:4])
                op(DVE, "tensor_tensor", [cf, gates], [cf], out=cf[:, 8:12], in0=cf[:, 4:8], in1=gv[:, b, :], op=ALU.mult)
                yt = ytr.get()
                yg = yatt[:, g * 4:(g + 1) * 4, :]
                op(DVE, "tensor_tensor", [pO, cf], [yt], out=yt[:], in0=pO[:, :, 0:64], in1=cf[:, 8:12].unsqueeze(2).broadcast_to([128, 4, 64]), op=ALU.mult)
                op(POOL, "tensor_tensor", [yatt, yt], [yatt], out=yg, in0=yg, in1=yt[:], op=ALU.add)

            def finish(qt):
                xt = xts3.pop(qt)
                yf = yatt[:].rearrange("p h d -> p (h d)")
                st = sm.get()
                op(DVE, "tensor_tensor", [yatt], [yjunk], out=yjunk[:], in0=yf, in1=yf, op=ALU.mult)
                op(DVE, "tensor_reduce", [yjunk], [st], out=st[:, 0:1], in_=yjunk[:], axis=AX.X, op=ALU.add)
                rsqrt_small(st[:, 0:1], [st], st, st[:, 2:3], 1, 1.0 / 512, sm.get())
                op(DVE, "scalar_tensor_tensor", [yatt, st, aonw], [yb], out=yb[:], in0=yf, scalar=st[:, 2:3], in1=aonw[:], op0=ALU.mult, op1=ALU.mult)
                pYb = psTb[:, 0:512]
                for c in range(4):
                    op(PE, "transpose", [yb, identb], [psTb], out=pYb[:, c * 128:(c + 1) * 128], in_=yb[:, c * 128:(c + 1) * 128], identity=identb[:])
                op(DVE, "tensor_copy", [psTb], [yattT], out=yattT[:].rearrange("p c t -> p (c t)"), in_=pYb[:, 0:512])
                for half in range(2):
                    pO_ = psX.get()
                    for c in range(4):
                        op(PE, "matmul", [yrnnT, wout], [pO_], pO_[:], lhsT=yrnnT[:, c, qt * 128:(qt + 1) * 128], rhs=wout[:, c, half * 512:(half + 1) * 512], start=(c == 0), stop=False)
                    for c in range(4):
                        op(PE, "matmul", [yattT, wout], [pO_], pO_[:], lhsT=yattT[:, c, :], rhs=wout[:, 4 + c, half * 512:(half + 1) * 512], start=False, stop=(c == 3))
                    op(DVE, "tensor_tensor", [xt, pO_], [xt], out=xt[:, half * 512:(half + 1) * 512], in0=xt[:, half * 512:(half + 1) * 512], in1=pO_[:], op=ALU.add)
                dma(SP, xt, [xt], [ydr[qt]], out=y_d[qt * 128:(qt + 1) * 128, :], in_=xt[:])

            def load_x(qt):
                xt = xr3.get()
                xts3[qt] = xt
                dma(SP, xt, [], [xt], out=xt[:], in_=x_d[qt * 128:(qt + 1) * 128, :])

            import os as _os
            _nqt = int(_os.environ.get('KQT', NT))
            jobs = []
            for qt in range(_nqt):
                first = True
                for g in range(2):
                    cts = [0] if qt < 16 else [0, 1]
                    for ct in cts:
                        ncs = 128 if ct == 0 else 127
                        mi = (qt if qt <= 16 else None) if ct == 0 else 17 + qt - 16
                        mm = [(kcT[64 * g:64 * g + 64, ct * 128:ct * 128 + ncs], qslice(g, qt), [kcT, qT], None)]
                        if mi is not None:
                            for h in range(4):
                                mm.append((identb[0:ncs, 0:ncs], cmask[0:ncs, mi, :], [identb, cmask], h))
                        jobs.append(dict(qt=qt, ncs=ncs, mm=mm, pO=psC, V=VcA[0:ncs, ct, g, :], Vb=VcA, ncol=128, first=(ct == cts[0]),
                                         before=(load_x if first else None),
                                         after=((lambda qt=qt, g=g: topk(qt, g)) if ct == cts[-1] else None)))
                        first = False
                for g in range(2):
                    kts = list(range(max(0, qt - 4), qt + 1))
                    for kt in kts:
                        mm = [(kwT[64 * g:64 * g + 64, kt * 128:(kt + 1) * 128], qslice(g, qt), [kwT, qT], None)]
                        if kt == qt:
                            mm.append((identb[:], tri4[:], [identb, tri4], None))
                        if kt == qt - 4:
                            mm.append((identb[:], wlow4[:], [identb, wlow4], None))
                        jobs.append(dict(qt=qt, ncs=128, mm=mm, pO=psWin, V=Vw[:, kt, g, :], Vb=Vw, ncol=65, first=(kt == kts[0]), before=None,
                                         after=((lambda qt=qt, g=g: evac(qt, g, psWin, 2)) if kt == kts[-1] else None)))
                for g in range(2):
                    kts = list(range(qt + 1))
                    for kt in kts:
                        mm = [(ksT[64 * g:64 * g + 64, kt * 128:(kt + 1) * 128], qslice(g, qt), [ksT, qT], None),
                              (efull[:, kt * 128:(kt + 1) * 128], negT4[g][:], [efull, negT4[g]], None)]
                        if kt == qt:
                            mm.append((identb[:], tri4[:], [identb, tri4], None))
                        if kt == kts[-1]:
                            if g == 0:
                                aft = (lambda qt=qt, g=g: evac(qt, g, psSel, 1))
                            else:
                                aft = (lambda qt=qt, g=g: (evac(qt, g, psSel, 1), finish(qt)))
                        else:
                            aft = None
                        jobs.append(dict(qt=qt, ncs=128, mm=mm, pO=psSel, V=Vs[:, kt, g, :], Vb=Vs, ncol=65, first=(kt == kts[0]), before=None, after=aft))

            def emit_score(job):
                if job["before"]:
                    job["before"](job["qt"])
                pS = psS.get()
                ncs = job["ncs"]
                n = len(job["mm"])
                for i, (l_, r_, bufs_, h) in enumerate(job["mm"]):
                    o_ = pS[0:ncs, :, :] if h is None else pS[0:ncs, h, :]
                    op(PE, "matmul", bufs_, [pS], o_, lhsT=l_, rhs=r_, start=(i == 0), stop=(i == n - 1), skip_group_check=True)
                return pS

            def emit_rest(job, pS):
                ncs = job["ncs"]
                E = Er.get()
                op(ACT, "activation", [pS], [E], out=E[0:ncs], in_=pS[0:ncs], func=AF.Exp, scale=0.125)
                pO = job["pO"]
                for h in range(4):
                    op(PE, "matmul", [E, job["Vb"]], [pO], pO[:, h, 0:job["ncol"]], lhsT=E[0:ncs, h, :], rhs=job["V"], start=(job["first"] and h == 0), stop=True, skip_group_check=True)
                if job["after"]:
                    job["after"]()

            LOOK = 2
            pend = []
            for job in jobs:
                pend.append((job, emit_score(job)))
                if len(pend) > LOOK:
                    emit_rest(*pend.pop(0))
            while pend:
                emit_rest(*pend.pop(0))
            fw.barrier()
        att.close()
        if stop == 3:
            return nc

        with ExitStack() as p4:
            Wg = fw.sb("Wg", [128, 8, DFF], BF16, p4)
            Wu = fw.sb("Wu", [128, 8, DFF], BF16, p4)
            Wd = fw.sb("Wd", [128, NFF, D], BF16, p4)
            fnw = fw.sb("fnw", [128, D], F32, p4)
            dma(SP, fnw, [], [fnw], out=fnw[:], in_=fnw_d[:, :])
            for k in range(8):
                dma(POOL, Wg, [], [Wg], out=Wg[:, k, :], in_=wg_d[k * 128:(k + 1) * 128, :])
                dma(POOL, Wu, [], [Wu], out=Wu[:, k, :], in_=wu_d[k * 128:(k + 1) * 128, :])
            for j in range(NFF):
                dma(POOL, Wd, [], [Wd], out=Wd[:, j, :], in_=wd_d[j * 128:(j + 1) * 128, :])
            W = nt_work(p4, nx=5, nh=1)
            psG = Ring([fw.ps("psG", [128, 512], F32, p4) for _ in range(2)])
            psU = Ring([fw.ps("psU", [128, 512], F32, p4) for _ in range(2)])
            psD = Ring([fw.ps("psD", [128, 512], F32, p4) for _ in range(2)])
            sg = Ring([fw.sb("sg", [128, 512], F32, p4) for _ in range(2)])
            aT = fw.sb("aT", [128, NFF, 512], BF16, p4)
            for tb in range(8):
                hT = W["hT"].get()
                xts = norm_transpose_block(tb, y_d, fnw, W, hT, srcbufs=ydr)
                for j in range(NFF):
                    pg, pu = psG.get(), psU.get()
                    for k in range(8):
                        op(PE, "matmul", [Wg, hT], [pg], pg[:], lhsT=Wg[:, k, j * 128:(j + 1) * 128], rhs=hT[:, k, :], start=(k == 0), stop=(k == 7))
                    for k in range(8):
                        op(PE, "matmul", [Wu, hT], [pu], pu[:], lhsT=Wu[:, k, j * 128:(j + 1) * 128], rhs=hT[:, k, :], start=(k == 0), stop=(k == 7))
                    sg_ = sg.get()
                    op(ACT, "activation", [pg], [sg_], out=sg_[:], in_=pg[:], func=AF.Silu)
                    op(DVE, "tensor_tensor", [sg_, pu], [aT], out=aT[:, j, :], in0=sg_[:], in1=pu[:], op=ALU.mult)
                for i in range(4):
                    tt = tb * 4 + i
                    xt = xts[i]
                    for half in range(2):
                        pd = psD.get()
                        for j in range(NFF):
                            op(PE, "matmul", [aT, Wd], [pd], pd[:], lhsT=aT[:, j, i * 128:(i + 1) * 128], rhs=Wd[:, j, half * 512:(half + 1) * 512], start=(j == 0), stop=(j == NFF - 1))
                        op(DVE, "tensor_tensor", [xt, pd], [xt], out=xt[:, half * 512:(half + 1) * 512], in0=xt[:, half * 512:(half + 1) * 512], in1=pd[:], op=ALU.add)
                    dma(SP, xt, [xt], [ydr[tt]], out=y_d[tt * 128:(tt + 1) * 128, :], in_=xt[:])
            fw.barrier()
    return nc


def host_inputs(inp, b):
    f32 = np.float32
    m = {}
    m["x"] = np.ascontiguousarray(inp["x"][b], dtype=f32)
    pos = np.asarray(inp["positions"][b], dtype=np.int32)
    m["pos"] = np.ascontiguousarray(pos.reshape(NT, 128).T)
    pc = np.zeros(256, np.int32)
    pc[:255] = pos[np.arange(255) * 16 + 31]
    m["posc"] = np.ascontiguousarray(pc.reshape(2, 128).T)
    invf = (500000.0 ** (-np.arange(8, dtype=np.float32) * 2.0 / 16)).astype(f32)
    m["invf"] = np.ascontiguousarray(np.broadcast_to(invf, (128, 8)), dtype=f32)
    m["w_in"] = np.ascontiguousarray(inp["w_in"][0], dtype=f32)
    m["anw"] = np.ascontiguousarray(np.broadcast_to(inp["attn_norm_w"][0], (128, D)), dtype=f32)
    qkw = np.concatenate([np.tile(inp["q_norm_w"][0], 8), np.tile(inp["k_norm_w"][0], 4)])
    m["qkw"] = np.ascontiguousarray(np.broadcast_to(qkw, (128, 768)), dtype=f32)
    cw = inp["conv_w"][0].reshape(4, 4, 128)
    m["convw"] = np.ascontiguousarray(cw.transpose(2, 1, 0).reshape(128, 16), dtype=f32)
    rp = np.stack([inp["conv_b"][0], inp["gate_a_b"][0], inp["gate_x_b"][0], inp["lru_lambda"][0], inp["rnn_out_norm_w"][0]], 0)
    m["rnnp"] = np.ascontiguousarray(rp.reshape(5, 4, 128).transpose(2, 1, 0).reshape(128, 20), dtype=f32)
    for nm, key in (("gaw", "gate_a_w"), ("gxw", "gate_x_w")):
        w = inp[key][0]
        bd = np.zeros((128, 4, 128), f32)
        for c in range(4):
            for i in range(2):
                bd[64 * i:64 * i + 64, c, 64 * i:64 * i + 64] = w[2 * c + i]
        m[nm] = bd.reshape(128, 512)
    m["identf"] = np.eye(128, dtype=f32)
    for kd in ("k", "v"):
        w1 = inp["cmp_%s_w1" % kd][0].reshape(32, 64, 256).transpose(1, 0, 2).reshape(64, 32 * 256)
        m["cw1" + kd] = np.ascontiguousarray(np.concatenate([w1, w1], 0), dtype=f32)
        w2 = inp["cmp_%s_w2" % kd][0].reshape(2, 128, 64).transpose(1, 0, 2).reshape(128, 128)
        m["cw2" + kd] = np.ascontiguousarray(w2, dtype=f32)
    cp = inp["cmp_pos"][0].T
    m["cposT"] = np.ascontiguousarray(np.concatenate([cp, cp], 0), dtype=f32)
    m.update(CONSTS)
    m["w_out"] = np.ascontiguousarray(inp["w_out"][0], dtype=f32)
    m["aonw"] = np.ascontiguousarray(np.broadcast_to(inp["attn_out_norm_w"][0], (128, 512)), dtype=f32)
    m["fnw"] = np.ascontiguousarray(np.broadcast_to(inp["ffn_norm_w"][0], (128, D)), dtype=f32)
    m["w_gate"] = np.ascontiguousarray(inp["w_gate"][0], dtype=f32)
    m["w_up"] = np.ascontiguousarray(inp["w_up"][0], dtype=f32)
    m["w_down"] = np.ascontiguousarray(inp["w_down"][0], dtype=f32)
    return m


def _consts():
    bf = ml_dtypes.bfloat16
    f32 = np.float32
    c = {}
    kl = np.arange(128)[:, None]
    ql = np.arange(128)[None, :]
    tri = np.where(kl <= ql, 0.0, NEG).astype(f32)
    c["tri4"] = np.ascontiguousarray(np.tile(tri, (1, 4))).astype(bf)
    wl = np.where(kl > ql, 0.0, NEG).astype(f32)
    c["wlow4"] = np.ascontiguousarray(np.tile(wl, (1, 4))).astype(bf)
    cm = np.zeros((128, 33, 128), f32)
    for mi in range(33):
        ct, qt = (0, mi) if mi <= 16 else (1, 16 + mi - 17)
        dv = 128 * qt - 2048 * ct - 31
        cm[:, mi, :] = np.where(16 * kl - ql <= dv, 0.0, NEG)
    c["cmask"] = np.ascontiguousarray(cm.reshape(128, 33 * 128)).astype(bf)
    ef = (np.arange(T)[None, :] // 64 == np.arange(128)[:, None]).astype(f32)
    c["efull"] = np.ascontiguousarray(ef).astype(bf)
    cs = np.arange(255)[:, None] * 16
    bs = np.arange(64)[None, :] * 64
    cov = np.clip(np.minimum(cs + 32, bs + 64) - np.maximum(cs, bs), 0, None).astype(f32) / 32.0
    covp = np.zeros((256, 64), f32)
    covp[:255] = cov
    c["cover"] = np.ascontiguousarray(covp.reshape(2, 128, 64).transpose(1, 0, 2).reshape(128, 128))
    tl = np.arange(128)[:, None]
    jp = np.arange(128)[None, :] - 64
    cc = (tl >= 64).astype(np.int64)
    forced = (jp == cc) | (jp == cc - 1)
    invalid = jp > cc
    vm = np.where(forced | invalid, 0.0, 1.0).astype(f32)
    bsb = np.where(forced, 1.0e4, np.where(invalid, -1.0e30, 0.0)).astype(f32)
    c["vmbs"] = np.ascontiguousarray(np.concatenate([vm, bsb], 1))
    return c


CONSTS = _consts()


def kernel(**inputs):
    inp = {k: np.asarray(v) for k, v in inputs.items()}
    nc = build()
    in_maps = [host_inputs(inp, b) for b in range(8)]
    res = run_bass_kernel_spmd(nc, in_maps, core_ids=list(range(8)))
    return np.stack([r["y"] for r in res.results], 0).astype(np.float32)
```

```python
import numpy as np
import ml_dtypes
from contextlib import ExitStack
import concourse.bass as bass
import concourse.mybir as mybir
from concourse.bass_utils import run_bass_kernel_spmd

F32, BF16, I32 = mybir.dt.float32, mybir.dt.bfloat16, mybir.dt.int32
AF = mybir.ActivationFunctionType
ALU = mybir.AluOpType
AX = mybir.AxisListType

T = 4096
NT = 32
D = 1024
DIN = 2328
DFF = 2816
NFF = 22
EPS = 1e-6
NEG = -30000.0
TWO_PI = 6.283185307179586
PI = 3.141592653589793


class Buf:
    def __init__(self, fw, t, name):
        self.fw, self.t, self.name = fw, t, name
        self.w = None
        self.r = {}
        self.dsem = None
        self.dkey = None
        self.dcnt = 0

    def __getitem__(self, k):
        return self.t[k]


class Eng:
    def __init__(self, fw, name, eng, sem, key, selfwait):
        self.fw, self.name, self.eng, self.sem, self.key = fw, name, eng, sem, key
        self.selfwait = selfwait
        self.cnt = 0
        self.waited = {}

    def sync(self, reads, writes):
        waits = {}

        def need(ev, raw):
            if ev is None:
                return
            key, sem, val = ev
            if key == self.key and not (self.selfwait and raw):
                return
            if self.waited.get(key, 0) >= val:
                return
            if key not in waits or waits[key][1] < val:
                waits[key] = (sem, val)

        for b in reads:
            need(b.w, True)
        for b in writes:
            need(b.w, False)
            for ev in b.r.values():
                need(ev, False)
        for key, (sem, val) in waits.items():
            self.eng.wait_ge(sem, val)
            self.waited[key] = val

    def wait_ev(self, ev):
        key, sem, val = ev
        if self.waited.get(key, 0) >= val:
            return
        self.eng.wait_ge(sem, val)
        self.waited[key] = val


class FW:
    def __init__(self, nc, es):
        self.nc, self.es = nc, es
        self.nkey = 0
        self.engs = {}
        for name, eng, sw in (("PE", nc.tensor, False), ("ACT", nc.scalar, True),
                              ("DVE", nc.vector, True), ("POOL", nc.gpsimd, True),
                              ("SP", nc.sync, False)):
            sem = es.enter_context(nc.semaphore("sem_" + name))
            self.engs[name] = Eng(self, name, eng, sem, self.newkey(), sw)
        self.PE, self.ACT, self.DVE, self.POOL, self.SP = (self.engs[n] for n in ("PE", "ACT", "DVE", "POOL", "SP"))
        self.carriers = []
        self.nbuf = 0

    def newkey(self):
        self.nkey += 1
        return self.nkey

    def sb(self, name, shape, dt, es=None):
        es = es or self.es
        self.nbuf += 1
        t = es.enter_context(self.nc.sbuf_tensor("%s_%d" % (name, self.nbuf), list(shape), dt))
        return Buf(self, t, name)

    def ps(self, name, shape, dt, es=None):
        es = es or self.es
        self.nbuf += 1
        t = es.enter_context(self.nc.psum_tensor("%s_%d" % (name, self.nbuf), list(shape), dt))
        return Buf(self, t, name)

    def dram(self, ap, name):
        return Buf(self, ap, name)

    def op(self, E, meth, reads, writes, *a, **kw):
        E.sync(reads, writes)
        ins = getattr(E.eng, meth)(*a, **kw)
        E.cnt += 1
        ins.then_inc(E.sem, 1)
        ev = (E.key, E.sem, E.cnt)
        for b in reads:
            b.r[E.key] = ev
        for b in writes:
            b.w = ev
            b.r = {}
        return ins

    def dma(self, Q, carrier, reads, writes, out, in_, **kw):
        Q.sync(reads, writes)
        if carrier.dsem is None:
            carrier.dsem = self.es.enter_context(self.nc.semaphore("dsem_%s_%d" % (carrier.name, len(self.carriers))))
            carrier.dkey = self.newkey()
            self.carriers.append(carrier)
        ins = Q.eng.dma_start(out=out, in_=in_, **kw)
        carrier.dcnt += 16
        ins.then_inc(carrier.dsem, 16)
        ev = (carrier.dkey, carrier.dsem, carrier.dcnt)
        for b in reads:
            b.r[carrier.dkey] = ev
        for b in writes:
            b.w = ev
            b.r = {}
        return ins

    def barrier(self):
        SP = self.SP
        for E in self.engs.values():
            if E is not SP and E.cnt > 0:
                SP.wait_ev((E.key, E.sem, E.cnt))
        for c in self.carriers:
            if c.dcnt > 0:
                SP.wait_ev((c.dkey, c.dsem, c.dcnt))
        ins = SP.eng.nop()
        SP.cnt += 1
        ins.then_inc(SP.sem, 1)
        ev = (SP.key, SP.sem, SP.cnt)
        for E in self.engs.values():
            if E is not SP:
                E.wait_ev(ev)


class Ring:
    def __init__(self, bufs):
        self.bufs, self.i = bufs, 0

    def get(self):
        b = self.bufs[self.i % len(self.bufs)]
        self.i += 1
        return b


def build(dbg=None, stop=None):
    nc = bass.Bass("TRN2", target_bir_lowering=False)

    def din(name, shape, dt=F32):
        return nc.dram_tensor(name, list(shape), dt, kind="ExternalInput").ap()

    x_d = din("x", [T, D])
    pos_d = din("pos", [128, NT], I32)
    posc_d = din("posc", [128, 2], I32)
    invf_d = din("invf", [128, 8])
    win_d = din("w_in", [D, DIN])
    anw_d = din("anw", [128, D])
    qkw_d = din("qkw", [128, 12 * 64])
    convw_d = din("convw", [128, 16])
    rnnp_d = din("rnnp", [128, 20])
    gaw_d = din("gaw", [128, 512])
    gxw_d = din("gxw", [128, 512])
    identf_d = din("identf", [128, 128])
    cw1_d = {"k": din("cw1k", [128, 32 * 256]), "v": din("cw1v", [128, 32 * 256])}
    cw2_d = {"k": din("cw2k", [128, 128]), "v": din("cw2v", [128, 128])}
    cposT_d = din("cposT", [128, 32])
    cover_d = din("cover", [128, 128])
    tri4_d = din("tri4", [128, 512], BF16)
    wlow4_d = din("wlow4", [128, 512], BF16)
    cmask_d = din("cmask", [128, 33 * 128], BF16)
    efull_d = din("efull", [64, T], BF16)
    vmbs_d = din("vmbs", [128, 256])
    wout_d = din("w_out", [D, D])
    aonw_d = din("aonw", [128, 512])
    fnw_d = din("fnw", [128, D])
    wg_d = din("w_gate", [D, DFF])
    wu_d = din("w_up", [D, DFF])
    wd_d = din("w_down", [DFF, D])
    y_d = nc.dram_tensor("y", [T, D], F32, kind="ExternalOutput").ap()
    dbg_d = {}
    if dbg:
        for nm, shp, dt in dbg:
            dbg_d[nm] = nc.dram_tensor("dbg_" + nm, list(shp), dt, kind="ExternalOutput").ap()

    with ExitStack() as es:
        fw = FW(nc, es)
        PE, ACT, DVE, POOL, SP = fw.PE, fw.ACT, fw.DVE, fw.POOL, fw.SP
        op, dma = fw.op, fw.dma
        att = ExitStack()
        tokst = ExitStack()

        identf = fw.sb("identf", [128, 128], F32)
        identb = fw.sb("identb", [128, 128], BF16)
        dma(SP, identf, [], [identf], out=identf[:], in_=identf_d[:, :])
        op(DVE, "tensor_copy", [identf], [identb], out=identb[:], in_=identf[:])
        ones_col = fw.sb("ones_col", [128, 1], F32)
        ones_row = fw.sb("ones_row", [1, 128], F32)
        neghalf = fw.sb("neghalf", [128, 512], F32)
        op(DVE, "memset", [], [ones_col], ones_col[:], 1.0)
        op(DVE, "memset", [], [ones_row], ones_row[:], 1.0)
        op(DVE, "memset", [], [neghalf], neghalf[:], -0.5)

        anw = fw.sb("anw", [128, D], F32, att)
        dma(SP, anw, [], [anw], out=anw[:], in_=anw_d[:, :])
        qkw = fw.sb("qkw", [128, 12, 64], F32, att)
        dma(SP, qkw, [], [qkw], out=qkw[:].rearrange("p h d -> p (h d)"), in_=qkw_d[:, :])
        convw = fw.sb("convw", [128, 4, 4], F32, att)
        dma(SP, convw, [], [convw], out=convw[:].rearrange("p c k -> p (c k)"), in_=convw_d[:, :])
        rnnp = fw.sb("rnnp", [128, 4, 5], F32, att)
        dma(SP, rnnp, [], [rnnp], out=rnnp[:].rearrange("p c k -> p (c k)"), in_=rnnp_d[:, :])
        gaw = fw.sb("gaw", [128, 4, 128], BF16, att)
        gxw = fw.sb("gxw", [128, 4, 128], BF16, att)
        dma(POOL, gaw, [], [gaw], out=gaw[:].rearrange("p c k -> p (c k)"), in_=gaw_d[:, :])
        dma(POOL, gxw, [], [gxw], out=gxw[:].rearrange("p c k -> p (c k)"), in_=gxw_d[:, :])

        def rope_tables(pos_ap, n, name):
            cosT_ = fw.sb(name + "cos", [128, n, 16], F32, att)
            sinT_ = fw.sb(name + "sin", [128, n, 16], F32, att)
            with ExitStack() as tmp:
                posi = fw.sb(name + "posi", [128, n], I32, tmp)
                dma(SP, posi, [], [posi], out=posi[:], in_=pos_ap)
                invf = fw.sb(name + "invf", [128, 8], F32, tmp)
                dma(SP, invf, [], [invf], out=invf[:], in_=invf_d[:, :])
                posf = fw.sb(name + "posf", [128, n], F32, tmp)
                op(DVE, "tensor_copy", [posi], [posf], out=posf[:], in_=posi[:])
                ang = fw.sb(name + "ang", [128, n, 8], F32, tmp)
                op(DVE, "tensor_tensor", [posf, invf], [ang], out=ang[:],
                   in0=posf[:].unsqueeze(2).broadcast_to([128, n, 8]),
                   in1=invf[:].unsqueeze(1).broadcast_to([128, n, 8]), op=ALU.mult)
                a2 = fw.sb(name + "a2", [128, n * 8], F32, tmp)
                ki = fw.sb(name + "ki", [128, n * 8], I32, tmp)
                kf = fw.sb(name + "kf", [128, n * 8], F32, tmp)
                m = fw.sb(name + "m", [128, n * 8], F32, tmp)
                for tab, shift in ((cosT_, PI / 2), (sinT_, 0.0)):
                    angf = ang[:].rearrange("p n f -> p (n f)")
                    op(DVE, "tensor_scalar", [ang], [a2], out=a2[:], in0=angf, scalar1=shift, scalar2=None, op0=ALU.add)
                    op(DVE, "tensor_scalar", [a2], [kf], out=kf[:], in0=a2[:], scalar1=1.0 / TWO_PI, scalar2=None, op0=ALU.mult)
                    op(DVE, "tensor_copy", [kf], [ki], out=ki[:], in_=kf[:])
                    op(DVE, "tensor_copy", [ki], [kf], out=kf[:], in_=ki[:])
                    op(DVE, "scalar_tensor_tensor", [kf, a2], [a2], out=a2[:], in0=kf[:], scalar=-TWO_PI, in1=a2[:], op0=ALU.mult, op1=ALU.add)
                    op(DVE, "tensor_scalar", [a2], [m], out=m[:], in0=a2[:], scalar1=PI, scalar2=None, op0=ALU.is_gt)
                    op(DVE, "scalar_tensor_tensor", [m, a2], [a2], out=a2[:], in0=m[:], scalar=-TWO_PI, in1=a2[:], op0=ALU.mult, op1=ALU.add)
                    op(DVE, "tensor_scalar", [a2], [m], out=m[:], in0=a2[:], scalar1=-PI, scalar2=None, op0=ALU.is_lt)
                    op(DVE, "scalar_tensor_tensor", [m, a2], [a2], out=a2[:], in0=m[:], scalar=TWO_PI, in1=a2[:], op0=ALU.mult, op1=ALU.add)
                    op(DVE, "tensor_scalar", [a2], [a2], out=a2[:], in0=a2[:], scalar1=PI, scalar2=-PI, op0=ALU.min, op1=ALU.max)
                    a3 = a2[:].rearrange("p (n f) -> p n f", f=8)
                    op(ACT, "activation", [a2], [tab], out=tab[:, :, 0:8], in_=a3, func=AF.Sin)
                    op(ACT, "activation", [a2], [tab], out=tab[:, :, 8:16], in_=a3, func=AF.Sin)
                fw.barrier()
            return cosT_, sinT_

        cosT, sinT = rope_tables(pos_d[:, :], NT, "rp")
        coscT, sincT = rope_tables(posc_d[:, :], 2, "rc")

        def rsqrt_small(src_ap, srcbufs, dst, dst_ap, n, scale, tmp):
            op(DVE, "tensor_scalar", srcbufs, [tmp], out=tmp[:, 0:n], in0=src_ap, scalar1=scale, scalar2=EPS, op0=ALU.mult, op1=ALU.add)
            op(POOL, "tensor_tensor", [tmp, neghalf], [dst], out=dst_ap, in0=tmp[:, 0:n], in1=neghalf[:, 0:n], op=ALU.pow)

        def norm_rope(qf, qb, nh, wt, cos_ap, sin_ap, csbufs, qn, st12, qw16, t12, t34, wo=0):
            op(DVE, "tensor_tensor", [qf], [qn], out=qn[:, 0:nh, :], in0=qf[:, 0:nh, :], in1=qf[:, 0:nh, :], op=ALU.mult)
            s12 = st12.get()
            op(DVE, "tensor_reduce", [qn], [s12], out=s12[:, 0:nh], in_=qn[:, 0:nh, :], axis=AX.X, op=ALU.add)
            r12 = st12.get()
            rsqrt_small(s12[:, 0:nh], [s12], r12, r12[:, 0:nh], nh, 1.0 / 64, st12.get())
            op(DVE, "tensor_tensor", [qf, r12], [qn], out=qn[:, 0:nh, :], in0=qf[:, 0:nh, :], in1=r12[:, 0:nh].unsqueeze(2).broadcast_to([128, nh, 64]), op=ALU.mult)
            op(DVE, "tensor_tensor", [qn, wt], [qb], out=qb[:, 0:nh, :], in0=qn[:, 0:nh, :], in1=wt[:, wo:wo + nh, :], op=ALU.mult)
            op(DVE, "tensor_tensor", [qn, wt], [qw16], out=qw16[:, 0:nh, :], in0=qn[:, 0:nh, 0:16], in1=wt[:, wo:wo + nh, 0:16], op=ALU.mult)
            op(DVE, "tensor_tensor", [qw16] + csbufs, [t12], out=t12[:, 0:nh, :], in0=qw16[:, 0:nh, :], in1=cos_ap.broadcast_to([128, nh, 16]), op=ALU.mult)
            op(DVE, "tensor_tensor", [qw16] + csbufs, [t34], out=t34[:, 0:nh, :], in0=qw16[:, 0:nh, :], in1=sin_ap.broadcast_to([128, nh, 16]), op=ALU.mult)
            op(DVE, "tensor_tensor", [t12, t34], [qb], out=qb[:, 0:nh, 0:8], in0=t12[:, 0:nh, 0:8], in1=t34[:, 0:nh, 8:16], op=ALU.subtract)
            op(DVE, "tensor_tensor", [t12, t34], [qb], out=qb[:, 0:nh, 8:16], in0=t12[:, 0:nh, 8:16], in1=t34[:, 0:nh, 0:8], op=ALU.add)

        def norm_transpose_block(tb, src_d, wbc, W, hT, srcbufs=None):
            xts = []
            for j in range(4):
                tt = tb * 4 + j
                xt = W["x"].get()
                xts.append(xt)
                dma(SP, xt, [srcbufs[tt]] if srcbufs else [], [xt], out=xt[:], in_=src_d[tt * 128:(tt + 1) * 128, :])
                st = W["stat"].get()
                hb = W["hb"].get()
                op(ACT, "activation", [xt], [hb, st], out=hb[:], in_=xt[:], func=AF.Square, accum_out=st[:, 0:1])
                rsqrt_small(st[:, 0:1], [st], st, st[:, 2:3], 1, 1.0 / D, W["stmp"].get())
                op(DVE, "scalar_tensor_tensor", [xt, st, wbc], [hb], out=hb[:], in0=xt[:], scalar=st[:, 2:3], in1=wbc[:], op0=ALU.mult, op1=ALU.mult)
                pT = W["psT"].get()
                for k in range(8):
                    op(PE, "transpose", [hb, identb], [pT], out=pT[:, k * 128:(k + 1) * 128], in_=hb[:, k * 128:(k + 1) * 128], identity=identb[:])
                op(ACT, "activation", [pT], [hT], out=hT[:, :, j * 128:(j + 1) * 128], in_=pT[:].rearrange("p (k t) -> p k t", k=8), func=AF.Copy)
            return xts

        def nt_work(es_, nx=2, nh=2):
            return {
                "x": Ring([fw.sb("xt", [128, D], F32, es_) for _ in range(nx)]),
                "stat": Ring([fw.sb("stat", [128, 4], F32, es_) for _ in range(4)]),
                "stmp": Ring([fw.sb("stmp", [128, 12], F32, es_) for _ in range(4)]),
                "hb": Ring([fw.sb("hb", [128, D], BF16, es_) for _ in range(2)]),
                "psT": Ring([fw.ps("psT", [128, D], BF16, es_) for _ in range(2)]),
                "hT": Ring([fw.sb("hT", [128, 8, 512], BF16, es_) for _ in range(nh)]),
            }

        yrnnT = fw.sb("yrnnT", [128, 4, T], BF16, att)
        kcT = fw.sb("kcT", [128, 256], BF16, att)
        VcA = fw.sb("VcA", [128, 2, 2, 128], BF16, att)
        kcTok = fw.sb("kcTok", [128, T], BF16, tokst)
        vcTok = fw.sb("vcTok", [128, T], BF16, tokst)
        ydr = [fw.dram(None, "ydr%d" % i) for i in range(NT)]

        with ExitStack() as p1:
            W = nt_work(p1)
            WinA = fw.sb("WinA", [128, 8, 1280], BF16, p1)
            for k in range(8):
                dma(POOL, WinA, [], [WinA], out=WinA[:, k, 0:1024], in_=win_d[k * 128:(k + 1) * 128, 0:1024])
                dma(POOL, WinA, [], [WinA], out=WinA[:, k, 1024:1280], in_=win_d[k * 128:(k + 1) * 128, 1536:1792])
            psM = Ring([fw.ps("psM", [128, 512], F32, p1) for _ in range(4)])
            psS = fw.ps("psS", [128, 512], F32, p1)
            xpad = [fw.sb("xpad", [128, 515], F32, p1) for _ in range(4)]
            gel = Ring([fw.sb("gel", [128, 512], F32, p1) for _ in range(2)])
            xc = Ring([fw.sb("xc", [128, 512], F32, p1) for _ in range(2)])
            xcb = Ring([fw.sb("xcb", [128, 512], BF16, p1) for _ in range(2)])
            rr = Ring([fw.sb("rr", [128, 512], F32, p1) for _ in range(2)])
            ii = Ring([fw.sb("ii", [128, 512], F32, p1) for _ in range(2)])
            aa = Ring([fw.sb("aa", [128, 512], F32, p1) for _ in range(2)])
            hring = Ring([fw.sb("hh", [128, 512], F32, p1) for _ in range(2)])
            hlast = fw.sb("hlast", [128, 4], F32, p1)
            y4 = fw.sb("y4", [128, 4, 512], F32, p1)
            ysq = Ring([fw.sb("ysq", [128, 512], F32, p1) for _ in range(2)])
            ssrow = fw.sb("ssrow", [1, 512], F32, p1)
            rrow = fw.sb("rrow", [1, 512], F32, p1)
            c1 = fw.sb("c1", [128, 4, 2], F32, p1)
            c1t = fw.sb("c1t", [128, 4], F32, p1)

            for c in range(4):
                op(DVE, "memset", [], [xpad[c]], xpad[c][:], 0.0)
            op(DVE, "memset", [], [hlast], hlast[:], 0.0)
            op(ACT, "activation", [rnnp], [c1t], out=c1t[:], in_=rnnp[:, :, 3], func=AF.Exp, scale=-1.0)
            op(ACT, "activation", [c1t], [c1t], out=c1t[:], in_=c1t[:], func=AF.Ln, bias=1.0)
            op(DVE, "tensor_scalar", [c1t], [c1], out=c1[:, :, 0], in0=c1t[:], scalar1=-8.0, scalar2=None, op0=ALU.mult)
            op(DVE, "tensor_scalar", [c1t], [c1], out=c1[:, :, 1], in0=c1t[:], scalar1=-16.0, scalar2=None, op0=ALU.mult)

            for tb in range(8):
                hT = W["hT"].get()
                norm_transpose_block(tb, x_d, anw, W, hT)
                for dst, c0 in ((kcTok, 1024), (vcTok, 1152)):
                    pF = psM.get()
                    for k in range(8):
                        op(PE, "matmul", [hT, WinA], [pF], pF[:], lhsT=WinA[:, k, c0:c0 + 128], rhs=hT[:, k, :], start=(k == 0), stop=(k == 7))
                    op(ACT, "activation", [pF], [dst], out=dst[:, tb * 512:(tb + 1) * 512], in_=pF[:], func=AF.Copy)
                for c in range(4):
                    pX = psM.get()
                    for k in range(8):
                        op(PE, "matmul", [hT, WinA], [pX], pX[:], lhsT=WinA[:, k, c * 128:(c + 1) * 128], rhs=hT[:, k, :], start=(k == 0), stop=(k == 7))
                    xp = xpad[c]
                    if tb > 0:
                        op(DVE, "tensor_copy", [xp], [xp], out=xp[:, 0:3], in_=xp[:, 512:515])
                    op(ACT, "activation", [pX], [xp], out=xp[:, 3:515], in_=pX[:], func=AF.Copy)
                    pG = psM.get()
                    for k in range(8):
                        op(PE, "matmul", [hT, WinA], [pG], pG[:], lhsT=WinA[:, k, 512 + c * 128:512 + (c + 1) * 128], rhs=hT[:, k, :], start=(k == 0), stop=(k == 7))
                    g_ = gel.get()
                    op(ACT, "activation", [pG], [g_], out=g_[:], in_=pG[:], func=AF.Gelu_apprx_tanh)
                    xc_ = xc.get()
                    op(DVE, "tensor_scalar", [xp, convw, rnnp], [xc_], out=xc_[:], in0=xp[:, 0:512], scalar1=convw[:, c, 0:1], scalar2=rnnp[:, c, 0:1], op0=ALU.mult, op1=ALU.add)
                    for k in range(1, 4):
                        op(DVE, "scalar_tensor_tensor", [xp, convw, xc_], [xc_], out=xc_[:], in0=xp[:, k:k + 512], scalar=convw[:, c, k:k + 1], in1=xc_[:], op0=ALU.mult, op1=ALU.add)
                    xcb_ = xcb.get()
                    op(POOL, "tensor_copy", [xc_], [xcb_], out=xcb_[:], in_=xc_[:])
                    pa = psM.get()
                    op(PE, "matmul", [gaw, xcb_], [pa], pa[:], lhsT=gaw[:, c, :], rhs=xcb_[:], start=True, stop=True)
                    px = psM.get()
                    op(PE, "matmul", [gxw, xcb_], [px], px[:], lhsT=gxw[:, c, :], rhs=xcb_[:], start=True, stop=True)
                    r_, i_, a_ = rr.get(), ii.get(), aa.get()
                    op(ACT, "activation", [pa, rnnp], [r_], out=r_[:], in_=pa[:], func=AF.Sigmoid, bias=rnnp[:, c, 1:2])
                    op(ACT, "activation", [px, rnnp], [i_], out=i_[:], in_=px[:], func=AF.Sigmoid, bias=rnnp[:, c, 2:3])
                    op(ACT, "activation", [r_, c1], [a_], out=a_[:], in_=r_[:], func=AF.Exp, scale=c1[:, c, 0:1])
                    op(ACT, "activation", [r_, c1], [r_], out=r_[:], in_=r_[:], func=AF.Exp, scale=c1[:, c, 1:2])
                    op(ACT, "activation", [r_], [r_], out=r_[:], in_=r_[:], func=AF.Sqrt, scale=-1.0, bias=1.0)
                    op(DVE, "tensor_tensor", [i_, xc_], [i_], out=i_[:], in0=i_[:], in1=xc_[:], op=ALU.mult)
                    op(DVE, "tensor_tensor", [i_, r_], [i_], out=i_[:], in0=i_[:], in1=r_[:], op=ALU.mult)
                    h_ = hring.get()
                    op(DVE, "tensor_tensor_scan", [a_, i_, hlast], [h_], out=h_[:], data0=a_[:], data1=i_[:], initial=hlast[:, c:c + 1], op0=ALU.mult, op1=ALU.add)
                    op(DVE, "tensor_copy", [h_], [hlast], out=hlast[:, c:c + 1], in_=h_[:, 511:512])
                    op(DVE, "tensor_tensor", [g_, h_], [y4], out=y4[:, c, :], in0=g_[:], in1=h_[:], op=ALU.mult)
                    ys_ = ysq.get()
                    op(POOL, "tensor_tensor", [y4], [ys_], out=ys_[:], in0=y4[:, c, :], in1=y4[:, c, :], op=ALU.mult)
                    op(PE, "matmul", [ones_col, ys_], [psS], psS[0:1, :], lhsT=ones_col[:, 0:1], rhs=ys_[:], start=(c == 0), stop=(c == 3))
                op(DVE, "tensor_scalar", [psS], [ssrow], out=ssrow[:], in0=psS[0:1, :], scalar1=1.0 / 512, scalar2=EPS, op0=ALU.mult, op1=ALU.add)
                op(ACT, "activation", [ssrow], [ssrow], out=ssrow[:], in_=ssrow[:], func=AF.Sqrt)
                op(DVE, "reciprocal", [ssrow], [rrow], out=rrow[:], in_=ssrow[:])
                pb_ = psM.get()
                op(PE, "matmul", [ones_row, rrow], [pb_], pb_[:], lhsT=ones_row[:], rhs=rrow[:], start=True, stop=True)
                for c in range(4):
                    op(DVE, "scalar_tensor_tensor", [y4, rnnp, pb_], [yrnnT], out=yrnnT[:, c, tb * 512:(tb + 1) * 512], in0=y4[:, c, :], scalar=rnnp[:, c, 4:5], in1=pb_[:], op0=ALU.mult, op1=ALU.mult)
            fw.barrier()

        with ExitStack() as p2:
            cw1 = {}
            cw2 = {}
            for kd in ("k", "v"):
                cw1[kd] = fw.sb("cw1" + kd, [128, 32, 256], BF16, p2)
                for l0 in range(0, 32, 8):
                    dma(POOL, cw1[kd], [], [cw1[kd]], out=cw1[kd][:, l0:l0 + 8, :].rearrange("p l n -> p (l n)"), in_=cw1_d[kd][:, l0 * 256:(l0 + 8) * 256])
                cw2[kd] = fw.sb("cw2" + kd, [128, 2, 64], BF16, p2)
                dma(POOL, cw2[kd], [], [cw2[kd]], out=cw2[kd][:].rearrange("p c d -> p (c d)"), in_=cw2_d[kd][:, :])
            cposT = fw.sb("cposT", [128, 32], BF16, p2)
            dma(POOL, cposT, [], [cposT], out=cposT[:], in_=cposT_d[:, :])
            covf = fw.sb("covf", [128, 2, 64], F32, p2)
            dma(SP, covf, [], [covf], out=covf[:].rearrange("p c j -> p (c j)"), in_=cover_d[:, :])
            for g in range(2):
                op(DVE, "tensor_copy", [covf], [VcA], out=VcA[:, :, g, 64:128], in_=covf[:])
            psM = Ring([fw.ps("psM2", [128, 512], F32, p2) for _ in range(4)])
            psQ = fw.ps("psQ2", [128, 2, 128], BF16, p2)
            hidT = Ring([fw.sb("hidT", [128, 2, 256], BF16, p2) for _ in range(2)])
            cbias = fw.sb("cbias", [128, 4], F32, p2)
            kcf = [fw.sb("kcf", [128, 2, 64], F32, p2) for _ in range(2)]
            kcb = [fw.sb("kcb", [128, 2, 64], BF16, p2) for _ in range(2)]
            qn2 = fw.sb("qn2", [128, 2, 64], F32, p2)
            st2 = Ring([fw.sb("st2", [128, 12], F32, p2) for _ in range(6)])
            qw2 = fw.sb("qw2", [128, 2, 16], F32, p2)
            t12b = fw.sb("t12b", [128, 2, 16], F32, p2)
            t34b = fw.sb("t34b", [128, 2, 16], F32, p2)
            for ct in range(2):
                op(DVE, "memset", [], [kcf[ct]], kcf[ct][:], 0.0)
            op(DVE, "memset", [], [kcT], kcT[:], 0.0)
            pbias = psM.get()
            for ki, kd in enumerate(("k", "v")):
                for n_ in range(2):
                    col = ki * 2 + n_
                    for l in range(32):
                        op(PE, "matmul", [cw1[kd], cposT], [pbias], pbias[:, col:col + 1], lhsT=cw1[kd][0:64, l, n_ * 128:(n_ + 1) * 128], rhs=cposT[0:64, l:l + 1], start=(l == 0), stop=(l == 31))
            op(DVE, "tensor_copy", [pbias], [cbias], out=cbias[:], in_=pbias[:, 0:4])
            for ki, (kd, tok) in enumerate((("k", kcTok), ("v", vcTok))):
                for g in range(2):
                    hid = hidT.get()
                    for n_ in range(2):
                        ph = psM.get()
                        for l in range(32):
                            op(PE, "matmul", [cw1[kd], tok], [ph], ph[:, 0:255], lhsT=cw1[kd][64 * g:64 * g + 64, l, n_ * 128:(n_ + 1) * 128],
                               rhs=tok[64 * g:64 * g + 64, l:l + 16 * 254 + 1:16], start=(l == 0), stop=(l == 31))
                        op(ACT, "activation", [ph, cbias], [hid], out=hid[:, n_, 0:255], in_=ph[:, 0:255], func=AF.Gelu_apprx_tanh, bias=cbias[:, ki * 2 + n_:ki * 2 + n_ + 1])
                    for ct in range(2):
                        ncs = 128 if ct == 0 else 127
                        po = psM.get()
                        for n_ in range(2):
                            op(PE, "matmul", [hid, cw2[kd]], [po], po[0:ncs, 0:64], lhsT=hid[:, n_, ct * 128:ct * 128 + ncs], rhs=cw2[kd][:, n_, :], start=(n_ == 0), stop=(n_ == 1))
                        if kd == "k":
                            op(ACT, "activation", [po], [kcf[ct]], out=kcf[ct][0:ncs, g, :], in_=po[0:ncs, 0:64], func=AF.Copy)
                        else:
                            op(ACT, "activation", [po], [VcA], out=VcA[0:ncs, ct, g, 0:64], in_=po[0:ncs, 0:64], func=AF.Copy)
            for ct in range(2):
                norm_rope(kcf[ct], kcb[ct], 2, qkw, coscT[:, ct:ct + 1, :], sincT[:, ct:ct + 1, :], [coscT, sincT], qn2, st2, qw2, t12b, t34b, wo=8)
                op(PE, "transpose", [kcb[ct], identb], [psQ], out=psQ[:, ct, :], in_=kcb[ct][:].rearrange("p h d -> p (h d)"), identity=identb[:])
            op(ACT, "activation", [psQ], [kcT], out=kcT[:, 0:255], in_=psQ[:].rearrange("p c t -> p (c t)")[:, 0:255], func=AF.Copy)
            fw.barrier()
        tokst.close()
        if stop == 2:
            att.close()
            return nc

        qT = fw.sb("qT", [128, 4, T], BF16, att)
        KA = [fw.sb("KA%d" % g_, [128, T], BF16, att) for g_ in range(2)]
        dma(SP, KA[0], [], [KA[0]], out=KA[0][64:128, :], in_=efull_d[:, :])
        dma(SP, KA[1], [], [KA[1]], out=KA[1][0:64, :], in_=efull_d[:, :])
        kwT = fw.sb("kwT", [128, T], BF16, att)
        Vs = fw.sb("Vs", [128, NT, 2, 65], BF16, att)
        Vw = fw.sb("Vw", [128, NT, 2, 65], BF16, att)
        gates = fw.sb("gates", [128, NT, 24], F32, att)
        op(POOL, "memset", [], [Vs], Vs[:, :, :, 64:65], 1.0)
        op(POOL, "memset", [], [Vw], Vw[:, :, :, 64:65], 1.0)
        with ExitStack() as p1:
            W = nt_work(p1)
            WinB = fw.sb("WinB", [128, 8, 1048], BF16, p1)
            for k in range(8):
                dma(POOL, WinB, [], [WinB], out=WinB[:, k, 0:512], in_=win_d[k * 128:(k + 1) * 128, 1024:1536])
                dma(POOL, WinB, [], [WinB], out=WinB[:, k, 512:1048], in_=win_d[k * 128:(k + 1) * 128, 1792:2328])
            psM = Ring([fw.ps("psM", [128, 512], F32, p1) for _ in range(4)])
            psQ = Ring([fw.ps("psQ", [128, 6, 128], BF16, p1) for _ in range(2)])
            qkf = Ring([fw.sb("qkf", [128, 12, 64], F32, p1) for _ in range(2)])
            qn = fw.sb("qn", [128, 12, 64], F32, p1)
            qkb = Ring([fw.sb("qkb", [128, 12, 64], BF16, p1) for _ in range(2)])
            st12 = Ring([fw.sb("st12", [128, 12], F32, p1) for _ in range(4)])
            qw16 = fw.sb("qw16", [128, 12, 16], F32, p1)
            t12 = fw.sb("t12", [128, 12, 16], F32, p1)
            t34 = fw.sb("t34", [128, 12, 16], F32, p1)
            for tb in range(8):
                hT = W["hT"].get()
                norm_transpose_block(tb, x_d, anw, W, hT)
                for j in range(4):
                    tt = tb * 4 + j
                    qf = qkf.get()
                    pA = psM.get()
                    for k in range(8):
                        op(PE, "matmul", [hT, WinB], [pA], pA[:, 0:512], lhsT=hT[:, k, j * 128:(j + 1) * 128], rhs=WinB[:, k, 0:512], start=(k == 0), stop=(k == 7))
                    op(ACT, "activation", [pA], [qf], out=qf[:, 0:8, :].rearrange("p (i g) d -> p i g d", g=2),
                       in_=pA[:, 0:512].rearrange("p (g i d) -> p i g d", g=2, i=4), func=AF.Copy)
                    pB = psM.get()
                    for k in range(8):
                        op(PE, "matmul", [hT, WinB], [pB], pB[:, 0:256], lhsT=hT[:, k, j * 128:(j + 1) * 128], rhs=WinB[:, k, 512:768], start=(k == 0), stop=(k == 7))
                    op(ACT, "activation", [pB], [qf], out=qf[:, 8:10, :].rearrange("p h d -> p (h d)"), in_=pB[:, 0:128], func=AF.Copy)
                    op(ACT, "activation", [pB], [Vs], out=Vs[:, tt, :, 0:64], in_=pB[:, 128:256].rearrange("p (g d) -> p g d", g=2), func=AF.Copy)
                    pC = psM.get()
                    for k in range(8):
                        op(PE, "matmul", [hT, WinB], [pC], pC[:, 0:280], lhsT=hT[:, k, j * 128:(j + 1) * 128], rhs=WinB[:, k, 768:1048], start=(k == 0), stop=(k == 7))
                    op(ACT, "activation", [pC], [qf], out=qf[:, 10:12, :].rearrange("p h d -> p (h d)"), in_=pC[:, 0:128], func=AF.Copy)
                    op(ACT, "activation", [pC], [Vw], out=Vw[:, tt, :, 0:64], in_=pC[:, 128:256].rearrange("p (g d) -> p g d", g=2), func=AF.Copy)
                    op(ACT, "activation", [pC], [gates], out=gates[:, tt, :], in_=pC[:, 256:280], func=AF.Sigmoid)
                    qb = qkb.get()
                    norm_rope(qf, qb, 12, qkw, cosT[:, tt:tt + 1, :], sinT[:, tt:tt + 1, :], [cosT, sinT], qn, st12, qw16, t12, t34)
                    pQ = psQ.get()
                    for i in range(6):
                        op(PE, "transpose", [qb, identb], [pQ], out=pQ[:, i, :], in_=qb[:, 2 * i:2 * i + 2, :].rearrange("p h d -> p (h d)"), identity=identb[:])
                    op(ACT, "activation", [pQ], [qT], out=qT[:, :, tt * 128:(tt + 1) * 128], in_=pQ[:, 0:4, :], func=AF.Copy)
                    op(ACT, "activation", [pQ], [KA[0]], out=KA[0][0:64, tt * 128:(tt + 1) * 128], in_=pQ[0:64, 4, :], func=AF.Copy)
                    op(ACT, "activation", [pQ], [KA[1]], out=KA[1][64:128, tt * 128:(tt + 1) * 128], in_=pQ[64:128, 4, :], func=AF.Copy)
                    op(ACT, "activation", [pQ], [kwT], out=kwT[:, tt * 128:(tt + 1) * 128], in_=pQ[:, 5, :], func=AF.Copy)
            fw.barrier()

        if stop == "B":
            att.close()
            return nc
        with ExitStack() as p3:
            tri4 = fw.sb("tri4", [128, 4, 128], BF16, p3)
            wlow4 = fw.sb("wlow4", [128, 4, 128], BF16, p3)
            cmask = fw.sb("cmask", [128, 33, 128], BF16, p3)
            vmbs = fw.sb("vmbs", [128, 2, 128], F32, p3)
            wout = fw.sb("wout", [128, 8, D], BF16, p3)
            aonw = fw.sb("aonw", [128, 512], F32, p3)
            dma(SP, tri4, [], [tri4], out=tri4[:].rearrange("p h t -> p (h t)"), in_=tri4_d[:, :])
            dma(SP, wlow4, [], [wlow4], out=wlow4[:].rearrange("p h t -> p (h t)"), in_=wlow4_d[:, :])
            dma(SP, cmask, [], [cmask], out=cmask[:].rearrange("p m t -> p (m t)"), in_=cmask_d[:, :])
            dma(SP, vmbs, [], [vmbs], out=vmbs[:].rearrange("p a t -> p (a t)"), in_=vmbs_d[:, :])
            dma(SP, aonw, [], [aonw], out=aonw[:], in_=aonw_d[:, :])
            for k in range(8):
                dma(POOL, wout, [], [wout], out=wout[:, k, :], in_=wout_d[k * 128:(k + 1) * 128, :])
            psS = Ring([fw.ps("psS3", [128, 4, 128], F32, p3) for _ in range(3)])
            psC = fw.ps("psC", [128, 4, 128], F32, p3)
            psSel = fw.ps("psSel", [128, 4, 128], F32, p3)
            psWin = fw.ps("psWin", [128, 4, 128], F32, p3)
            psX = Ring([fw.ps("psX", [128, 512], F32, p3) for _ in range(1)])
            psTb = fw.ps("psTb", [128, 1024], BF16, p3)
            Er = Ring([fw.sb("E", [128, 4, 128], BF16, p3) for _ in range(4)])
            xr3 = Ring([fw.sb("xt3", [128, D], F32, p3) for _ in range(2)])
            ocmp = [fw.sb("ocmp", [128, 4, 64], F32, p3) for _ in range(2)]
            RA = [[fw.sb("RA", [128, 4, 128], BF16, p3) for _ in range(2)] for _ in range(2)]
            negm2 = [fw.sb("negm2", [128, 128], BF16, p3) for _ in range(2)]
            sm = Ring([fw.sb("sm", [128, 16], F32, p3) for _ in range(12)])
            imp = fw.sb("imp", [128, 64], F32, p3)
            score = fw.sb("score", [128, 64], F32, p3)
            score2 = fw.sb("score2", [128, 64], F32, p3)
            negm = fw.sb("negm", [128, 64], BF16, p3)
            yatt = fw.sb("yatt", [128, 8, 64], F32, p3)
            ytmp = fw.sb("ytmp", [128, 4, 64], F32, p3)
            yb = fw.sb("yb", [128, 512], BF16, p3)
            yjunk = fw.sb("yjunk", [128, 512], BF16, p3)
            yattT = fw.sb("yattT", [128, 4, 128], BF16, p3)

            for g in range(2):
                op(POOL, "memset", [], [negm2[g]], negm2[g][:], 0.0)

            def qslice(g, qt):
                return qT[64 * g:64 * g + 64, :, qt * 128:(qt + 1) * 128]

            ytr = Ring([fw.sb("ytmp", [128, 4, 64], F32, p3) for _ in range(2)])
            xts3 = {}

            def gview(qt, g):
                return gates[:, qt, g * 12:(g + 1) * 12].rearrange("p (h b) -> p b h", b=3)

            def topk(qt, g):
                gv = gview(qt, g)
                sums = sm.get()
                op(DVE, "tensor_reduce", [psC], [sums], out=sums[:, 0:4], in_=psC[:, :, 64:128], axis=AX.X, op=ALU.add)
                op(DVE, "tensor_scalar", [sums], [sums], out=sums[:, 4:8], in0=sums[:, 0:4], scalar1=1e-30, scalar2=None, op0=ALU.max)
                op(DVE, "reciprocal", [sums], [sums], out=sums[:, 8:12], in_=sums[:, 4:8])
                op(DVE, "tensor_tensor", [sums, gates], [sums], out=sums[:, 12:16], in0=sums[:, 8:12], in1=gv[:, 0, :], op=ALU.mult)
                op(DVE, "tensor_tensor", [psC, sums], [yatt], out=yatt[:, g * 4:(g + 1) * 4, :], in0=psC[:, :, 0:64], in1=sums[:, 12:16].unsqueeze(2).broadcast_to([128, 4, 64]), op=ALU.mult)
                op(DVE, "tensor_scalar", [psC, sums], [imp], out=imp[:], in0=psC[:, 0, 64:128], scalar1=sums[:, 8:9], scalar2=None, op0=ALU.mult)
                for h in range(1, 4):
                    op(DVE, "scalar_tensor_tensor", [psC, sums, imp], [imp], out=imp[:], in0=psC[:, h, 64:128], scalar=sums[:, 8 + h:9 + h], in1=imp[:], op0=ALU.mult, op1=ALU.add)
                lo = 64 - 2 * qt
                op(DVE, "tensor_tensor", [imp, vmbs], [score], out=score[:], in0=imp[:], in1=vmbs[:, 0, lo:lo + 64], op=ALU.mult)
                op(DVE, "tensor_tensor", [score, vmbs], [score], out=score[:], in0=score[:], in1=vmbs[:, 1, lo:lo + 64], op=ALU.add)
                op(DVE, "memset", [], [score], score[:, 0:1], 1.0e4)
                m8 = sm.get()
                op(DVE, "max", [score], [m8], out=m8[:, 0:8], in_=score[:])
                op(DVE, "match_replace", [m8, score], [score2], out=score2[:], in_to_replace=m8[:, 0:8], in_values=score[:], imm_value=-3.0e38)
                op(DVE, "max", [score2], [m8], out=m8[:, 8:16], in_=score2[:])
                c0 = 64 if g == 0 else 0
                nm = negm2[g]
                op(DVE, "tensor_scalar", [score, m8], [nm], out=nm[:, c0:c0 + 64], in0=score[:], scalar1=m8[:, 15:16], scalar2=NEG, op0=ALU.is_lt, op1=ALU.mult)
                pNb = psTb[:, 512:1024]
                op(PE, "transpose", [nm, identb], [psTb], out=pNb[:, 0:128], in_=nm[:], identity=identb[:])
                ra = RA[g][qt % 2]
                r0 = 64 if g == 0 else 0
                q0 = 0 if g == 0 else 64
                op(DVE, "tensor_copy", [psTb], [ra], out=ra[r0:r0 + 64, 0, :], in_=pNb[r0:r0 + 64, 0:128])
                op(POOL, "tensor_copy", [ra], [ra], out=ra[r0:r0 + 64, 1:4, :], in_=ra[r0:r0 + 64, 0:1, :].broadcast_to([64, 3, 128]))
                op(POOL, "tensor_copy", [qT], [ra], out=ra[q0:q0 + 64, :, :], in_=qT[q0:q0 + 64, :, qt * 128:(qt + 1) * 128])

            def evac(qt, g, pO, b):
                gv = gview(qt, g)
                cf = sm.get()
                op(DVE, "tensor_scalar", [pO], [cf], out=cf[:, 0:4], in0=pO[:, :, 64], scalar1=1e-30, scalar2=None, op0=ALU.max)
                op(DVE, "reciprocal", [cf], [cf], out=cf[:, 4:8], in_=cf[:, 0:4])
                op(DVE, "tensor_tensor", [cf, gates], [cf], out=cf[:, 8:12], in0=cf[:, 4:8], in1=gv[:, b, :], op=ALU.mult)
                yt = ytr.get()
                yg = yatt[:, g * 4:(g + 1) * 4, :]
                op(DVE, "tensor_tensor", [pO, cf], [yt], out=yt[:], in0=pO[:, :, 0:64], in1=cf[:, 8:12].unsqueeze(2).broadcast_to([128, 4, 64]), op=ALU.mult)
                op(POOL, "tensor_tensor", [yatt, yt], [yatt], out=yg, in0=yg, in1=yt[:], op=ALU.add)

            def finish(qt):
                xt = xts3.pop(qt)
                yf = yatt[:].rearrange("p h d -> p (h d)")
                st = sm.get()
                op(DVE, "tensor_tensor", [yatt], [yjunk], out=yjunk[:], in0=yf, in1=yf, op=ALU.mult)
                op(DVE, "tensor_reduce", [yjunk], [st], out=st[:, 0:1], in_=yjunk[:], axis=AX.X, op=ALU.add)
                rsqrt_small(st[:, 0:1], [st], st, st[:, 2:3], 1, 1.0 / 512, sm.get())
                op(DVE, "scalar_tensor_tensor", [yatt, st, aonw], [yb], out=yb[:], in0=yf, scalar=st[:, 2:3], in1=aonw[:], op0=ALU.mult, op1=ALU.mult)
                pYb = psTb[:, 0:512]
                for c in range(4):
                    op(PE, "transpose", [yb, identb], [psTb], out=pYb[:, c * 128:(c + 1) * 128], in_=yb[:, c * 128:(c + 1) * 128], identity=identb[:])
                op(DVE, "tensor_copy", [psTb], [yattT], out=yattT[:].rearrange("p c t -> p (c t)"), in_=pYb[:, 0:512])
                for half in range(2):
                    pO_ = psX.get()
                    for c in range(4):
                        op(PE, "matmul", [yrnnT, wout], [pO_], pO_[:], lhsT=yrnnT[:, c, qt * 128:(qt + 1) * 128], rhs=wout[:, c, half * 512:(half + 1) * 512], start=(c == 0), stop=False)
                    for c in range(4):
                        op(PE, "matmul", [yattT, wout], [pO_], pO_[:], lhsT=yattT[:, c, :], rhs=wout[:, 4 + c, half * 512:(half + 1) * 512], start=False, stop=(c == 3))
                    op(DVE, "tensor_tensor", [xt, pO_], [xt], out=xt[:, half * 512:(half + 1) * 512], in0=xt[:, half * 512:(half + 1) * 512], in1=pO_[:], op=ALU.add)
                dma(SP, xt, [xt], [ydr[qt]], out=y_d[qt * 128:(qt + 1) * 128, :], in_=xt[:])

            def load_x(qt):
                xt = xr3.get()
                xts3[qt] = xt
                dma(SP, xt, [], [xt], out=xt[:], in_=x_d[qt * 128:(qt + 1) * 128, :])

            import os as _os
            _nqt = int(_os.environ.get('KQT', NT))
            jobs = []
            for qt in range(_nqt):
                first = True
                for g in range(2):
                    cts = [0] if qt < 16 else [0, 1]
                    for ct in cts:
                        ncs = 128 if ct == 0 else 127
                        mi = (qt if qt <= 16 else None) if ct == 0 else 17 + qt - 16
                        mm = [(kcT[64 * g:64 * g + 64, ct * 128:ct * 128 + ncs], qslice(g, qt), [kcT, qT], None)]
                        if mi is not None:
                            for h in range(4):
                                mm.append((identb[0:ncs, 0:ncs], cmask[0:ncs, mi, :], [identb, cmask], h))
                        jobs.append(dict(qt=qt, ncs=ncs, mm=mm, pO=psC, V=VcA[0:ncs, ct, g, :], Vb=VcA, ncol=128, first=(ct == cts[0]),
                                         before=(load_x if first else None),
                                         after=((lambda qt=qt, g=g: topk(qt, g)) if ct == cts[-1] else None)))
                        first = False
                for g in range(2):
                    kts = list(range(max(0, qt - 4), qt + 1))
                    for kt in kts:
                        mm = [(kwT[64 * g:64 * g + 64, kt * 128:(kt + 1) * 128], qslice(g, qt), [kwT, qT], None)]
                        if kt == qt:
                            mm.append((identb[:], tri4[:], [identb, tri4], None))
                        if kt == qt - 4:
                            mm.append((identb[:], wlow4[:], [identb, wlow4], None))
                        jobs.append(dict(qt=qt, ncs=128, mm=mm, pO=psWin, V=Vw[:, kt, g, :], Vb=Vw, ncol=65, first=(kt == kts[0]), before=None,
                                         after=((lambda qt=qt, g=g: evac(qt, g, psWin, 2)) if kt == kts[-1] else None)))
                for g in range(2):
                    kts = list(range(qt + 1))
                    for kt in kts:
                        mm = [(KA[g][:, kt * 128:(kt + 1) * 128], RA[g][qt % 2][:], [KA[g], RA[g][qt % 2]], None)]
                        if kt == qt:
                            mm.append((identb[:], tri4[:], [identb, tri4], None))
                        if kt == kts[-1]:
                            if g == 0:
                                aft = (lambda qt=qt, g=g: evac(qt, g, psSel, 1))
                            else:
                                aft = (lambda qt=qt, g=g: (evac(qt, g, psSel, 1), finish(qt)))
                        else:
                            aft = None
                        jobs.append(dict(qt=qt, ncs=128, mm=mm, pO=psSel, V=Vs[:, kt, g, :], Vb=Vs, ncol=65, first=(kt == kts[0]), before=None, after=aft))

            def emit_score(job):
                if job["before"]:
                    job["before"](job["qt"])
                pS = psS.get()
                ncs = job["ncs"]
                n = len(job["mm"])
                for i, (l_, r_, bufs_, h) in enumerate(job["mm"]):
                    o_ = pS[0:ncs, :, :] if h is None else pS[0:ncs, h, :]
                    op(PE, "matmul", bufs_, [pS], o_, lhsT=l_, rhs=r_, start=(i == 0), stop=(i == n - 1), skip_group_check=True)
                return pS

            def emit_rest(job, pS):
                ncs = job["ncs"]
                E = Er.get()
                op(ACT, "activation", [pS], [E], out=E[0:ncs], in_=pS[0:ncs], func=AF.Exp, scale=0.125)
                pO = job["pO"]
                for h in range(4):
                    op(PE, "matmul", [E, job["Vb"]], [pO], pO[:, h, 0:job["ncol"]], lhsT=E[0:ncs, h, :], rhs=job["V"], start=(job["first"] and h == 0), stop=True, skip_group_check=True)
                if job["after"]:
                    job["after"]()

            LOOK = 2
            pend = []
            for job in jobs:
                pend.append((job, emit_score(job)))
                if len(pend) > LOOK:
                    emit_rest(*pend.pop(0))
            while pend:
                emit_rest(*pend.pop(0))
            fw.barrier()
        att.close()
        if stop == 3:
            return nc

        with ExitStack() as p4:
            Wg = fw.sb("Wg", [128, 8, DFF], BF16, p4)
            Wu = fw.sb("Wu", [128, 8, DFF], BF16, p4)
            Wd = fw.sb("Wd", [128, NFF, D], BF16, p4)
            fnw = fw.sb("fnw", [128, D], F32, p4)
            dma(SP, fnw, [], [fnw], out=fnw[:], in_=fnw_d[:, :])
            for k in range(8):
                dma(POOL, Wg, [], [Wg], out=Wg[:, k, :], in_=wg_d[k * 128:(k + 1) * 128, :])
                dma(POOL, Wu, [], [Wu], out=Wu[:, k, :], in_=wu_d[k * 128:(k + 1) * 128, :])
            for j in range(NFF):
                dma(POOL, Wd, [], [Wd], out=Wd[:, j, :], in_=wd_d[j * 128:(j + 1) * 128, :])
            W = nt_work(p4, nx=5, nh=1)
            psG = Ring([fw.ps("psG", [128, 512], F32, p4) for _ in range(2)])
            psU = Ring([fw.ps("psU", [128, 512], F32, p4) for _ in range(2)])
            psD = Ring([fw.ps("psD", [128, 512], F32, p4) for _ in range(2)])
            sg = Ring([fw.sb("sg", [128, 512], F32, p4) for _ in range(2)])
            aT = fw.sb("aT", [128, NFF, 512], BF16, p4)
            for tb in range(8):
                hT = W["hT"].get()
                xts = norm_transpose_block(tb, y_d, fnw, W, hT, srcbufs=ydr)
                for j in range(NFF):
                    pg, pu = psG.get(), psU.get()
                    for k in range(8):
                        op(PE, "matmul", [Wg, hT], [pg], pg[:], lhsT=Wg[:, k, j * 128:(j + 1) * 128], rhs=hT[:, k, :], start=(k == 0), stop=(k == 7))
                    for k in range(8):
                        op(PE, "matmul", [Wu, hT], [pu], pu[:], lhsT=Wu[:, k, j * 128:(j + 1) * 128], rhs=hT[:, k, :], start=(k == 0), stop=(k == 7))
                    sg_ = sg.get()
                    op(ACT, "activation", [pg], [sg_], out=sg_[:], in_=pg[:], func=AF.Silu)
                    op(DVE, "tensor_tensor", [sg_, pu], [aT], out=aT[:, j, :], in0=sg_[:], in1=pu[:], op=ALU.mult)
                for i in range(4):
                    tt = tb * 4 + i
                    xt = xts[i]
                    for half in range(2):
                        pd = psD.get()
                        for j in range(NFF):
                            op(PE, "matmul", [aT, Wd], [pd], pd[:], lhsT=aT[:, j, i * 128:(i + 1) * 128], rhs=Wd[:, j, half * 512:(half + 1) * 512], start=(j == 0), stop=(j == NFF - 1))
                        op(DVE, "tensor_tensor", [xt, pd], [xt], out=xt[:, half * 512:(half + 1) * 512], in0=xt[:, half * 512:(half + 1) * 512], in1=pd[:], op=ALU.add)
                    dma(SP, xt, [xt], [ydr[tt]], out=y_d[tt * 128:(tt + 1) * 128, :], in_=xt[:])
            fw.barrier()
    return nc


def host_inputs(inp, b):
    f32 = np.float32
    m = {}
    m["x"] = np.ascontiguousarray(inp["x"][b], dtype=f32)
    pos = np.asarray(inp["positions"][b], dtype=np.int32)
    m["pos"] = np.ascontiguousarray(pos.reshape(NT, 128).T)
    pc = np.zeros(256, np.int32)
    pc[:255] = pos[np.arange(255) * 16 + 31]
    m["posc"] = np.ascontiguousarray(pc.reshape(2, 128).T)
    invf = (500000.0 ** (-np.arange(8, dtype=np.float32) * 2.0 / 16)).astype(f32)
    m["invf"] = np.ascontiguousarray(np.broadcast_to(invf, (128, 8)), dtype=f32)
    m["w_in"] = np.ascontiguousarray(inp["w_in"][0], dtype=f32)
    m["anw"] = np.ascontiguousarray(np.broadcast_to(inp["attn_norm_w"][0], (128, D)), dtype=f32)
    qkw = np.concatenate([np.tile(inp["q_norm_w"][0], 8), np.tile(inp["k_norm_w"][0], 4)])
    m["qkw"] = np.ascontiguousarray(np.broadcast_to(qkw, (128, 768)), dtype=f32)
    cw = inp["conv_w"][0].reshape(4, 4, 128)
    m["convw"] = np.ascontiguousarray(cw.transpose(2, 1, 0).reshape(128, 16), dtype=f32)
    rp = np.stack([inp["conv_b"][0], inp["gate_a_b"][0], inp["gate_x_b"][0], inp["lru_lambda"][0], inp["rnn_out_norm_w"][0]], 0)
    m["rnnp"] = np.ascontiguousarray(rp.reshape(5, 4, 128).transpose(2, 1, 0).reshape(128, 20), dtype=f32)
    for nm, key in (("gaw", "gate_a_w"), ("gxw", "gate_x_w")):
        w = inp[key][0]
        bd = np.zeros((128, 4, 128), f32)
        for c in range(4):
            for i in range(2):
                bd[64 * i:64 * i + 64, c, 64 * i:64 * i + 64] = w[2 * c + i]
        m[nm] = bd.reshape(128, 512)
    m["identf"] = np.eye(128, dtype=f32)
    for kd in ("k", "v"):
        w1 = inp["cmp_%s_w1" % kd][0].reshape(32, 64, 256).transpose(1, 0, 2).reshape(64, 32 * 256)
        m["cw1" + kd] = np.ascontiguousarray(np.concatenate([w1, w1], 0), dtype=f32)
        w2 = inp["cmp_%s_w2" % kd][0].reshape(2, 128, 64).transpose(1, 0, 2).reshape(128, 128)
        m["cw2" + kd] = np.ascontiguousarray(w2, dtype=f32)
    cp = inp["cmp_pos"][0].T
    m["cposT"] = np.ascontiguousarray(np.concatenate([cp, cp], 0), dtype=f32)
    m.update(CONSTS)
    m["w_out"] = np.ascontiguousarray(inp["w_out"][0], dtype=f32)
    m["aonw"] = np.ascontiguousarray(np.broadcast_to(inp["attn_out_norm_w"][0], (128, 512)), dtype=f32)
    m["fnw"] = np.ascontiguousarray(np.broadcast_to(inp["ffn_norm_w"][0], (128, D)), dtype=f32)
    m["w_gate"] = np.ascontiguousarray(inp["w_gate"][0], dtype=f32)
    m["w_up"] = np.ascontiguousarray(inp["w_up"][0], dtype=f32)
    m["w_down"] = np.ascontiguousarray(inp["w_down"][0], dtype=f32)
    return m


def _consts():
    bf = ml_dtypes.bfloat16
    f32 = np.float32
    c = {}
    kl = np.arange(128)[:, None]
    ql = np.arange(128)[None, :]
    tri = np.where(kl <= ql, 0.0, NEG).astype(f32)
    c["tri4"] = np.ascontiguousarray(np.tile(tri, (1, 4))).astype(bf)
    wl = np.where(kl > ql, 0.0, NEG).astype(f32)
    c["wlow4"] = np.ascontiguousarray(np.tile(wl, (1, 4))).astype(bf)
    cm = np.zeros((128, 33, 128), f32)
    for mi in range(33):
        ct, qt = (0, mi) if mi <= 16 else (1, 16 + mi - 17)
        dv = 128 * qt - 2048 * ct - 31
        cm[:, mi, :] = np.where(16 * kl - ql <= dv, 0.0, NEG)
    c["cmask"] = np.ascontiguousarray(cm.reshape(128, 33 * 128)).astype(bf)
    ef = (np.arange(T)[None, :] // 64 == np.arange(64)[:, None]).astype(f32)
    c["efull"] = np.ascontiguousarray(ef).astype(bf)
    cs = np.arange(255)[:, None] * 16
    bs = np.arange(64)[None, :] * 64
    cov = np.clip(np.minimum(cs + 32, bs + 64) - np.maximum(cs, bs), 0, None).astype(f32) / 32.0
    covp = np.zeros((256, 64), f32)
    covp[:255] = cov
    c["cover"] = np.ascontiguousarray(covp.reshape(2, 128, 64).transpose(1, 0, 2).reshape(128, 128))
    tl = np.arange(128)[:, None]
    jp = np.arange(128)[None, :] - 64
    cc = (tl >= 64).astype(np.int64)
    forced = (jp == cc) | (jp == cc - 1)
    invalid = jp > cc
    vm = np.where(forced | invalid, 0.0, 1.0).astype(f32)
    bsb = np.where(forced, 1.0e4, np.where(invalid, -1.0e30, 0.0)).astype(f32)
    c["vmbs"] = np.ascontiguousarray(np.concatenate([vm, bsb], 1))
    return c


CONSTS = _consts()


def kernel(**inputs):
    inp = {k: np.asarray(v) for k, v in inputs.items()}
    nc = build()
    in_maps = [host_inputs(inp, b) for b in range(8)]
    res = run_bass_kernel_spmd(nc, in_maps, core_ids=list(range(8)))
    return np.stack([r["y"] for r in res.results], 0).astype(np.float32)
```

```python
import numpy as np
import ml_dtypes
from contextlib import ExitStack
import concourse.bass as bass
import concourse.mybir as mybir
from concourse.bass_utils import run_bass_kernel_spmd

F32, BF16, I32 = mybir.dt.float32, mybir.dt.bfloat16, mybir.dt.int32
AF = mybir.ActivationFunctionType
ALU = mybir.AluOpType
AX = mybir.AxisListType

T = 4096
NT = 32
D = 1024
DIN = 2328
DFF = 2816
NFF = 22
EPS = 1e-6
NEG = -30000.0
TWO_PI = 6.283185307179586
PI = 3.141592653589793


class Buf:
    def __init__(self, fw, t, name):
        self.fw, self.t, self.name = fw, t, name
        self.w = None
        self.r = {}
        self.dsem = None
        self.dkey = None
        self.dcnt = 0

    def __getitem__(self, k):
        return self.t[k]


class Eng:
    def __init__(self, fw, name, eng, sem, key, selfwait):
        self.fw, self.name, self.eng, self.sem, self.key = fw, name, eng, sem, key
        self.selfwait = selfwait
        self.cnt = 0
        self.waited = {}

    def sync(self, reads, writes):
        waits = {}

        def need(ev, raw):
            if ev is None:
                return
            key, sem, val = ev
            if key == self.key and not (self.selfwait and raw):
                return
            if self.waited.get(key, 0) >= val:
                return
            if key not in waits or waits[key][1] < val:
                waits[key] = (sem, val)

        for b in reads:
            need(b.w, True)
        for b in writes:
            need(b.w, False)
            for ev in b.r.values():
                need(ev, False)
        for key, (sem, val) in waits.items():
            self.eng.wait_ge(sem, val)
            self.waited[key] = val

    def wait_ev(self, ev):
        key, sem, val = ev
        if self.waited.get(key, 0) >= val:
            return
        self.eng.wait_ge(sem, val)
        self.waited[key] = val


class FW:
    def __init__(self, nc, es):
        self.nc, self.es = nc, es
        self.nkey = 0
        self.engs = {}
        for name, eng, sw in (("PE", nc.tensor, False), ("ACT", nc.scalar, True),
                              ("DVE", nc.vector, True), ("POOL", nc.gpsimd, True),
                              ("SP", nc.sync, False)):
            sem = es.enter_context(nc.semaphore("sem_" + name))
            self.engs[name] = Eng(self, name, eng, sem, self.newkey(), sw)
        self.PE, self.ACT, self.DVE, self.POOL, self.SP = (self.engs[n] for n in ("PE", "ACT", "DVE", "POOL", "SP"))
        self.carriers = []
        self.nbuf = 0

    def newkey(self):
        self.nkey += 1
        return self.nkey

    def sb(self, name, shape, dt, es=None):
        es = es or self.es
        self.nbuf += 1
        t = es.enter_context(self.nc.sbuf_tensor("%s_%d" % (name, self.nbuf), list(shape), dt))
        return Buf(self, t, name)

    def ps(self, name, shape, dt, es=None):
        es = es or self.es
        self.nbuf += 1
        t = es.enter_context(self.nc.psum_tensor("%s_%d" % (name, self.nbuf), list(shape), dt))
        return Buf(self, t, name)

    def dram(self, ap, name):
        return Buf(self, ap, name)

    def op(self, E, meth, reads, writes, *a, **kw):
        E.sync(reads, writes)
        ins = getattr(E.eng, meth)(*a, **kw)
        E.cnt += 1
        ins.then_inc(E.sem, 1)
        ev = (E.key, E.sem, E.cnt)
        for b in reads:
            b.r[E.key] = ev
        for b in writes:
            b.w = ev
            b.r = {}
        return ins

    def dma(self, Q, carrier, reads, writes, out, in_, **kw):
        Q.sync(reads, writes)
        if carrier.dsem is None:
            carrier.dsem = self.es.enter_context(self.nc.semaphore("dsem_%s_%d" % (carrier.name, len(self.carriers))))
            carrier.dkey = self.newkey()
            self.carriers.append(carrier)
        ins = Q.eng.dma_start(out=out, in_=in_, **kw)
        carrier.dcnt += 16
        ins.then_inc(carrier.dsem, 16)
        ev = (carrier.dkey, carrier.dsem, carrier.dcnt)
        for b in reads:
            b.r[carrier.dkey] = ev
        for b in writes:
            b.w = ev
            b.r = {}
        return ins

    def barrier(self):
        SP = self.SP
        for E in self.engs.values():
            if E is not SP and E.cnt > 0:
                SP.wait_ev((E.key, E.sem, E.cnt))
        for c in self.carriers:
            if c.dcnt > 0:
                SP.wait_ev((c.dkey, c.dsem, c.dcnt))
        ins = SP.eng.nop()
        SP.cnt += 1
        ins.then_inc(SP.sem, 1)
        ev = (SP.key, SP.sem, SP.cnt)
        for E in self.engs.values():
            if E is not SP:
                E.wait_ev(ev)


class Ring:
    def __init__(self, bufs):
        self.bufs, self.i = bufs, 0

    def get(self):
        b = self.bufs[self.i % len(self.bufs)]
        self.i += 1
        return b


def build(dbg=None, stop=None):
    nc = bass.Bass("TRN2", target_bir_lowering=False)

    def din(name, shape, dt=F32):
        return nc.dram_tensor(name, list(shape), dt, kind="ExternalInput").ap()

    x_d = din("x", [T, D])
    pos_d = din("pos", [128, NT], I32)
    posc_d = din("posc", [128, 2], I32)
    invf_d = din("invf", [128, 8])
    win_d = din("w_in", [D, DIN])
    anw_d = din("anw", [128, D])
    qkw_d = din("qkw", [128, 12 * 64])
    convw_d = din("convw", [128, 16])
    rnnp_d = din("rnnp", [128, 20])
    gaw_d = din("gaw", [128, 512])
    gxw_d = din("gxw", [128, 512])
    identf_d = din("identf", [128, 128])
    cw1_d = {"k": din("cw1k", [128, 32 * 256]), "v": din("cw1v", [128, 32 * 256])}
    cw2_d = {"k": din("cw2k", [128, 128]), "v": din("cw2v", [128, 128])}
    cposT_d = din("cposT", [128, 32])
    cover_d = din("cover", [128, 128])
    tri4_d = din("tri4", [128, 512], BF16)
    wlow4_d = din("wlow4", [128, 512], BF16)
    cmask_d = din("cmask", [128, 33 * 128], BF16)
    efull_d = din("efull", [64, T], BF16)
    vmbs_d = din("vmbs", [128, 256])
    wout_d = din("w_out", [D, D])
    aonw_d = din("aonw", [128, 512])
    fnw_d = din("fnw", [128, D])
    wg_d = din("w_gate", [D, DFF])
    wu_d = din("w_up", [D, DFF])
    wd_d = din("w_down", [DFF, D])
    y_d = nc.dram_tensor("y", [T, D], F32, kind="ExternalOutput").ap()
    dbg_d = {}
    if dbg:
        for nm, shp, dt in dbg:
            dbg_d[nm] = nc.dram_tensor("dbg_" + nm, list(shp), dt, kind="ExternalOutput").ap()

    with ExitStack() as es:
        fw = FW(nc, es)
        PE, ACT, DVE, POOL, SP = fw.PE, fw.ACT, fw.DVE, fw.POOL, fw.SP
        op, dma = fw.op, fw.dma
        att = ExitStack()
        tokst = ExitStack()

        identf = fw.sb("identf", [128, 128], F32)
        identb = fw.sb("identb", [128, 128], BF16)
        dma(SP, identf, [], [identf], out=identf[:], in_=identf_d[:, :])
        op(DVE, "tensor_copy", [identf], [identb], out=identb[:], in_=identf[:])
        ones_col = fw.sb("ones_col", [128, 1], F32)
        ones_row = fw.sb("ones_row", [1, 128], F32)
        neghalf = fw.sb("neghalf", [128, 512], F32)
        op(DVE, "memset", [], [ones_col], ones_col[:], 1.0)
        op(DVE, "memset", [], [ones_row], ones_row[:], 1.0)
        op(DVE, "memset", [], [neghalf], neghalf[:], -0.5)

        anw = fw.sb("anw", [128, D], F32, att)
        dma(SP, anw, [], [anw], out=anw[:], in_=anw_d[:, :])
        qkw = fw.sb("qkw", [128, 12, 64], F32, att)
        dma(SP, qkw, [], [qkw], out=qkw[:].rearrange("p h d -> p (h d)"), in_=qkw_d[:, :])
        convw = fw.sb("convw", [128, 4, 4], F32, att)
        dma(SP, convw, [], [convw], out=convw[:].rearrange("p c k -> p (c k)"), in_=convw_d[:, :])
        rnnp = fw.sb("rnnp", [128, 4, 5], F32, att)
        dma(SP, rnnp, [], [rnnp], out=rnnp[:].rearrange("p c k -> p (c k)"), in_=rnnp_d[:, :])
        gaw = fw.sb("gaw", [128, 4, 128], BF16, att)
        gxw = fw.sb("gxw", [128, 4, 128], BF16, att)
        dma(POOL, gaw, [], [gaw], out=gaw[:].rearrange("p c k -> p (c k)"), in_=gaw_d[:, :])
        dma(POOL, gxw, [], [gxw], out=gxw[:].rearrange("p c k -> p (c k)"), in_=gxw_d[:, :])

        def rope_tables(pos_ap, n, name):
            cosT_ = fw.sb(name + "cos", [128, n, 16], F32, att)
            sinT_ = fw.sb(name + "sin", [128, n, 16], F32, att)
            with ExitStack() as tmp:
                posi = fw.sb(name + "posi", [128, n], I32, tmp)
                dma(SP, posi, [], [posi], out=posi[:], in_=pos_ap)
                invf = fw.sb(name + "invf", [128, 8], F32, tmp)
                dma(SP, invf, [], [invf], out=invf[:], in_=invf_d[:, :])
                posf = fw.sb(name + "posf", [128, n], F32, tmp)
                op(DVE, "tensor_copy", [posi], [posf], out=posf[:], in_=posi[:])
                ang = fw.sb(name + "ang", [128, n, 8], F32, tmp)
                op(DVE, "tensor_tensor", [posf, invf], [ang], out=ang[:],
                   in0=posf[:].unsqueeze(2).broadcast_to([128, n, 8]),
                   in1=invf[:].unsqueeze(1).broadcast_to([128, n, 8]), op=ALU.mult)
                a2 = fw.sb(name + "a2", [128, n * 8], F32, tmp)
                ki = fw.sb(name + "ki", [128, n * 8], I32, tmp)
                kf = fw.sb(name + "kf", [128, n * 8], F32, tmp)
                m = fw.sb(name + "m", [128, n * 8], F32, tmp)
                for tab, shift in ((cosT_, PI / 2), (sinT_, 0.0)):
                    angf = ang[:].rearrange("p n f -> p (n f)")
                    op(DVE, "tensor_scalar", [ang], [a2], out=a2[:], in0=angf, scalar1=shift, scalar2=None, op0=ALU.add)
                    op(DVE, "tensor_scalar", [a2], [kf], out=kf[:], in0=a2[:], scalar1=1.0 / TWO_PI, scalar2=None, op0=ALU.mult)
                    op(DVE, "tensor_copy", [kf], [ki], out=ki[:], in_=kf[:])
                    op(DVE, "tensor_copy", [ki], [kf], out=kf[:], in_=ki[:])
                    op(DVE, "scalar_tensor_tensor", [kf, a2], [a2], out=a2[:], in0=kf[:], scalar=-TWO_PI, in1=a2[:], op0=ALU.mult, op1=ALU.add)
                    op(DVE, "tensor_scalar", [a2], [m], out=m[:], in0=a2[:], scalar1=PI, scalar2=None, op0=ALU.is_gt)
                    op(DVE, "scalar_tensor_tensor", [m, a2], [a2], out=a2[:], in0=m[:], scalar=-TWO_PI, in1=a2[:], op0=ALU.mult, op1=ALU.add)
                    op(DVE, "tensor_scalar", [a2], [m], out=m[:], in0=a2[:], scalar1=-PI, scalar2=None, op0=ALU.is_lt)
                    op(DVE, "scalar_tensor_tensor", [m, a2], [a2], out=a2[:], in0=m[:], scalar=TWO_PI, in1=a2[:], op0=ALU.mult, op1=ALU.add)
                    op(DVE, "tensor_scalar", [a2], [a2], out=a2[:], in0=a2[:], scalar1=PI, scalar2=-PI, op0=ALU.min, op1=ALU.max)
                    a3 = a2[:].rearrange("p (n f) -> p n f", f=8)
                    op(ACT, "activation", [a2], [tab], out=tab[:, :, 0:8], in_=a3, func=AF.Sin)
                    op(ACT, "activation", [a2], [tab], out=tab[:, :, 8:16], in_=a3, func=AF.Sin)
                fw.barrier()
            return cosT_, sinT_

        cosT, sinT = rope_tables(pos_d[:, :], NT, "rp")
        coscT, sincT = rope_tables(posc_d[:, :], 2, "rc")

        def rsqrt_small(src_ap, srcbufs, dst, dst_ap, n, scale, tmp):
            op(DVE, "tensor_scalar", srcbufs, [tmp], out=tmp[:, 0:n], in0=src_ap, scalar1=scale, scalar2=EPS, op0=ALU.mult, op1=ALU.add)
            op(POOL, "tensor_tensor", [tmp, neghalf], [dst], out=dst_ap, in0=tmp[:, 0:n], in1=neghalf[:, 0:n], op=ALU.pow)

        def norm_rope(qf, qb, nh, wt, cos_ap, sin_ap, csbufs, qn, st12, qw16, t12, t34, wo=0):
            op(DVE, "tensor_tensor", [qf], [qn], out=qn[:, 0:nh, :], in0=qf[:, 0:nh, :], in1=qf[:, 0:nh, :], op=ALU.mult)
            s12 = st12.get()
            op(DVE, "tensor_reduce", [qn], [s12], out=s12[:, 0:nh], in_=qn[:, 0:nh, :], axis=AX.X, op=ALU.add)
            r12 = st12.get()
            rsqrt_small(s12[:, 0:nh], [s12], r12, r12[:, 0:nh], nh, 1.0 / 64, st12.get())
            op(DVE, "tensor_tensor", [qf, r12], [qn], out=qn[:, 0:nh, :], in0=qf[:, 0:nh, :], in1=r12[:, 0:nh].unsqueeze(2).broadcast_to([128, nh, 64]), op=ALU.mult)
            op(DVE, "tensor_tensor", [qn, wt], [qb], out=qb[:, 0:nh, :], in0=qn[:, 0:nh, :], in1=wt[:, wo:wo + nh, :], op=ALU.mult)
            op(DVE, "tensor_tensor", [qn, wt], [qw16], out=qw16[:, 0:nh, :], in0=qn[:, 0:nh, 0:16], in1=wt[:, wo:wo + nh, 0:16], op=ALU.mult)
            op(DVE, "tensor_tensor", [qw16] + csbufs, [t12], out=t12[:, 0:nh, :], in0=qw16[:, 0:nh, :], in1=cos_ap.broadcast_to([128, nh, 16]), op=ALU.mult)
            op(DVE, "tensor_tensor", [qw16] + csbufs, [t34], out=t34[:, 0:nh, :], in0=qw16[:, 0:nh, :], in1=sin_ap.broadcast_to([128, nh, 16]), op=ALU.mult)
            op(DVE, "tensor_tensor", [t12, t34], [qb], out=qb[:, 0:nh, 0:8], in0=t12[:, 0:nh, 0:8], in1=t34[:, 0:nh, 8:16], op=ALU.subtract)
            op(DVE, "tensor_tensor", [t12, t34], [qb], out=qb[:, 0:nh, 8:16], in0=t12[:, 0:nh, 8:16], in1=t34[:, 0:nh, 0:8], op=ALU.add)

        def norm_transpose_block(tb, src_d, wbc, W, hT, srcbufs=None):
            xts = []
            for j in range(4):
                tt = tb * 4 + j
                xt = W["x"].get()
                xts.append(xt)
                dma(SP, xt, [srcbufs[tt]] if srcbufs else [], [xt], out=xt[:], in_=src_d[tt * 128:(tt + 1) * 128, :])
                st = W["stat"].get()
                hb = W["hb"].get()
                op(ACT, "activation", [xt], [hb, st], out=hb[:], in_=xt[:], func=AF.Square, accum_out=st[:, 0:1])
                rsqrt_small(st[:, 0:1], [st], st, st[:, 2:3], 1, 1.0 / D, W["stmp"].get())
                op(DVE, "scalar_tensor_tensor", [xt, st, wbc], [hb], out=hb[:], in0=xt[:], scalar=st[:, 2:3], in1=wbc[:], op0=ALU.mult, op1=ALU.mult)
                pT = W["psT"].get()
                for k in range(8):
                    op(PE, "transpose", [hb, identb], [pT], out=pT[:, k * 128:(k + 1) * 128], in_=hb[:, k * 128:(k + 1) * 128], identity=identb[:])
                op(ACT, "activation", [pT], [hT], out=hT[:, :, j * 128:(j + 1) * 128], in_=pT[:].rearrange("p (k t) -> p k t", k=8), func=AF.Copy)
            return xts

        def nt_work(es_, nx=2, nh=2):
            return {
                "x": Ring([fw.sb("xt", [128, D], F32, es_) for _ in range(nx)]),
                "stat": Ring([fw.sb("stat", [128, 4], F32, es_) for _ in range(4)]),
                "stmp": Ring([fw.sb("stmp", [128, 12], F32, es_) for _ in range(4)]),
                "hb": Ring([fw.sb("hb", [128, D], BF16, es_) for _ in range(2)]),
                "psT": Ring([fw.ps("psT", [128, D], BF16, es_) for _ in range(2)]),
                "hT": Ring([fw.sb("hT", [128, 8, 512], BF16, es_) for _ in range(nh)]),
            }

        yrnnT = fw.sb("yrnnT", [128, 4, T], BF16, att)
        kcT = fw.sb("kcT", [128, 256], BF16, att)
        VcA = fw.sb("VcA", [128, 2, 2, 128], BF16, att)
        kcTok = fw.sb("kcTok", [128, T], BF16, tokst)
        vcTok = fw.sb("vcTok", [128, T], BF16, tokst)
        ydr = [fw.dram(None, "ydr%d" % i) for i in range(NT)]

        with ExitStack() as p1:
            W = nt_work(p1)
            WinA = fw.sb("WinA", [128, 8, 1280], BF16, p1)
            for k in range(8):
                dma(POOL, WinA, [], [WinA], out=WinA[:, k, 0:1024], in_=win_d[k * 128:(k + 1) * 128, 0:1024])
                dma(POOL, WinA, [], [WinA], out=WinA[:, k, 1024:1280], in_=win_d[k * 128:(k + 1) * 128, 1536:1792])
            psM = Ring([fw.ps("psM", [128, 512], F32, p1) for _ in range(4)])
            psS = fw.ps("psS", [128, 512], F32, p1)
            xpad = [fw.sb("xpad", [128, 515], F32, p1) for _ in range(4)]
            gel = Ring([fw.sb("gel", [128, 512], F32, p1) for _ in range(2)])
            xc = Ring([fw.sb("xc", [128, 512], F32, p1) for _ in range(2)])
            xcb = Ring([fw.sb("xcb", [128, 512], BF16, p1) for _ in range(2)])
            rr = Ring([fw.sb("rr", [128, 512], F32, p1) for _ in range(2)])
            ii = Ring([fw.sb("ii", [128, 512], F32, p1) for _ in range(2)])
            aa = Ring([fw.sb("aa", [128, 512], F32, p1) for _ in range(2)])
            hring = Ring([fw.sb("hh", [128, 512], F32, p1) for _ in range(2)])
            hlast = fw.sb("hlast", [128, 4], F32, p1)
            y4 = fw.sb("y4", [128, 4, 512], F32, p1)
            ysq = Ring([fw.sb("ysq", [128, 512], F32, p1) for _ in range(2)])
            ssrow = fw.sb("ssrow", [1, 512], F32, p1)
            rrow = fw.sb("rrow", [1, 512], F32, p1)
            c1 = fw.sb("c1", [128, 4, 2], F32, p1)
            c1t = fw.sb("c1t", [128, 4], F32, p1)

            for c in range(4):
                op(DVE, "memset", [], [xpad[c]], xpad[c][:], 0.0)
            op(DVE, "memset", [], [hlast], hlast[:], 0.0)
            op(ACT, "activation", [rnnp], [c1t], out=c1t[:], in_=rnnp[:, :, 3], func=AF.Exp, scale=-1.0)
            op(ACT, "activation", [c1t], [c1t], out=c1t[:], in_=c1t[:], func=AF.Ln, bias=1.0)
            op(DVE, "tensor_scalar", [c1t], [c1], out=c1[:, :, 0], in0=c1t[:], scalar1=-8.0, scalar2=None, op0=ALU.mult)
            op(DVE, "tensor_scalar", [c1t], [c1], out=c1[:, :, 1], in0=c1t[:], scalar1=-16.0, scalar2=None, op0=ALU.mult)

            for tb in range(8):
                hT = W["hT"].get()
                norm_transpose_block(tb, x_d, anw, W, hT)
                for dst, c0 in ((kcTok, 1024), (vcTok, 1152)):
                    pF = psM.get()
                    for k in range(8):
                        op(PE, "matmul", [hT, WinA], [pF], pF[:], lhsT=WinA[:, k, c0:c0 + 128], rhs=hT[:, k, :], start=(k == 0), stop=(k == 7))
                    op(ACT, "activation", [pF], [dst], out=dst[:, tb * 512:(tb + 1) * 512], in_=pF[:], func=AF.Copy)
                for c in range(4):
                    pX = psM.get()
                    for k in range(8):
                        op(PE, "matmul", [hT, WinA], [pX], pX[:], lhsT=WinA[:, k, c * 128:(c + 1) * 128], rhs=hT[:, k, :], start=(k == 0), stop=(k == 7))
                    xp = xpad[c]
                    if tb > 0:
                        op(DVE, "tensor_copy", [xp], [xp], out=xp[:, 0:3], in_=xp[:, 512:515])
                    op(ACT, "activation", [pX], [xp], out=xp[:, 3:515], in_=pX[:], func=AF.Copy)
                    pG = psM.get()
                    for k in range(8):
                        op(PE, "matmul", [hT, WinA], [pG], pG[:], lhsT=WinA[:, k, 512 + c * 128:512 + (c + 1) * 128], rhs=hT[:, k, :], start=(k == 0), stop=(k == 7))
                    g_ = gel.get()
                    op(ACT, "activation", [pG], [g_], out=g_[:], in_=pG[:], func=AF.Gelu_apprx_tanh)
                    xc_ = xc.get()
                    op(DVE, "tensor_scalar", [xp, convw, rnnp], [xc_], out=xc_[:], in0=xp[:, 0:512], scalar1=convw[:, c, 0:1], scalar2=rnnp[:, c, 0:1], op0=ALU.mult, op1=ALU.add)
                    for k in range(1, 4):
                        op(DVE, "scalar_tensor_tensor", [xp, convw, xc_], [xc_], out=xc_[:], in0=xp[:, k:k + 512], scalar=convw[:, c, k:k + 1], in1=xc_[:], op0=ALU.mult, op1=ALU.add)
                    xcb_ = xcb.get()
                    op(POOL, "tensor_copy", [xc_], [xcb_], out=xcb_[:], in_=xc_[:])
                    pa = psM.get()
                    op(PE, "matmul", [gaw, xcb_], [pa], pa[:], lhsT=gaw[:, c, :], rhs=xcb_[:], start=True, stop=True)
                    px = psM.get()
                    op(PE, "matmul", [gxw, xcb_], [px], px[:], lhsT=gxw[:, c, :], rhs=xcb_[:], start=True, stop=True)
                    r_, i_, a_ = rr.get(), ii.get(), aa.get()
                    op(ACT, "activation", [pa, rnnp], [r_], out=r_[:], in_=pa[:], func=AF.Sigmoid, bias=rnnp[:, c, 1:2])
                    op(ACT, "activation", [px, rnnp], [i_], out=i_[:], in_=px[:], func=AF.Sigmoid, bias=rnnp[:, c, 2:3])
                    op(ACT, "activation", [r_, c1], [a_], out=a_[:], in_=r_[:], func=AF.Exp, scale=c1[:, c, 0:1])
                    op(ACT, "activation", [r_, c1], [r_], out=r_[:], in_=r_[:], func=AF.Exp, scale=c1[:, c, 1:2])
                    op(ACT, "activation", [r_], [r_], out=r_[:], in_=r_[:], func=AF.Sqrt, scale=-1.0, bias=1.0)
                    op(DVE, "tensor_tensor", [i_, xc_], [i_], out=i_[:], in0=i_[:], in1=xc_[:], op=ALU.mult)
                    op(DVE, "tensor_tensor", [i_, r_], [i_], out=i_[:], in0=i_[:], in1=r_[:], op=ALU.mult)
                    h_ = hring.get()
                    op(DVE, "tensor_tensor_scan", [a_, i_, hlast], [h_], out=h_[:], data0=a_[:], data1=i_[:], initial=hlast[:, c:c + 1], op0=ALU.mult, op1=ALU.add)
                    op(DVE, "tensor_copy", [h_], [hlast], out=hlast[:, c:c + 1], in_=h_[:, 511:512])
                    op(DVE, "tensor_tensor", [g_, h_], [y4], out=y4[:, c, :], in0=g_[:], in1=h_[:], op=ALU.mult)
                    ys_ = ysq.get()
                    op(POOL, "tensor_tensor", [y4], [ys_], out=ys_[:], in0=y4[:, c, :], in1=y4[:, c, :], op=ALU.mult)
                    op(PE, "matmul", [ones_col, ys_], [psS], psS[0:1, :], lhsT=ones_col[:, 0:1], rhs=ys_[:], start=(c == 0), stop=(c == 3))
                op(DVE, "tensor_scalar", [psS], [ssrow], out=ssrow[:], in0=psS[0:1, :], scalar1=1.0 / 512, scalar2=EPS, op0=ALU.mult, op1=ALU.add)
                op(ACT, "activation", [ssrow], [ssrow], out=ssrow[:], in_=ssrow[:], func=AF.Sqrt)
                op(DVE, "reciprocal", [ssrow], [rrow], out=rrow[:], in_=ssrow[:])
                pb_ = psM.get()
                op(PE, "matmul", [ones_row, rrow], [pb_], pb_[:], lhsT=ones_row[:], rhs=rrow[:], start=True, stop=True)
                for c in range(4):
                    op(DVE, "scalar_tensor_tensor", [y4, rnnp, pb_], [yrnnT], out=yrnnT[:, c, tb * 512:(tb + 1) * 512], in0=y4[:, c, :], scalar=rnnp[:, c, 4:5], in1=pb_[:], op0=ALU.mult, op1=ALU.mult)
            fw.barrier()

        with ExitStack() as p2:
            cw1 = {}
            cw2 = {}
            for kd in ("k", "v"):
                cw1[kd] = fw.sb("cw1" + kd, [128, 32, 256], BF16, p2)
                for l0 in range(0, 32, 8):
                    dma(POOL, cw1[kd], [], [cw1[kd]], out=cw1[kd][:, l0:l0 + 8, :].rearrange("p l n -> p (l n)"), in_=cw1_d[kd][:, l0 * 256:(l0 + 8) * 256])
                cw2[kd] = fw.sb("cw2" + kd, [128, 2, 64], BF16, p2)
                dma(POOL, cw2[kd], [], [cw2[kd]], out=cw2[kd][:].rearrange("p c d -> p (c d)"), in_=cw2_d[kd][:, :])
            cposT = fw.sb("cposT", [128, 32], BF16, p2)
            dma(POOL, cposT, [], [cposT], out=cposT[:], in_=cposT_d[:, :])
            covf = fw.sb("covf", [128, 2, 64], F32, p2)
            dma(SP, covf, [], [covf], out=covf[:].rearrange("p c j -> p (c j)"), in_=cover_d[:, :])
            for g in range(2):
                op(DVE, "tensor_copy", [covf], [VcA], out=VcA[:, :, g, 64:128], in_=covf[:])
            psM = Ring([fw.ps("psM2", [128, 512], F32, p2) for _ in range(4)])
            psQ = fw.ps("psQ2", [128, 2, 128], BF16, p2)
            hidT = Ring([fw.sb("hidT", [128, 2, 256], BF16, p2) for _ in range(2)])
            cbias = fw.sb("cbias", [128, 4], F32, p2)
            kcf = [fw.sb("kcf", [128, 2, 64], F32, p2) for _ in range(2)]
            kcb = [fw.sb("kcb", [128, 2, 64], BF16, p2) for _ in range(2)]
            qn2 = fw.sb("qn2", [128, 2, 64], F32, p2)
            st2 = Ring([fw.sb("st2", [128, 12], F32, p2) for _ in range(6)])
            qw2 = fw.sb("qw2", [128, 2, 16], F32, p2)
            t12b = fw.sb("t12b", [128, 2, 16], F32, p2)
            t34b = fw.sb("t34b", [128, 2, 16], F32, p2)
            for ct in range(2):
                op(DVE, "memset", [], [kcf[ct]], kcf[ct][:], 0.0)
            op(DVE, "memset", [], [kcT], kcT[:], 0.0)
            pbias = psM.get()
            for ki, kd in enumerate(("k", "v")):
                for n_ in range(2):
                    col = ki * 2 + n_
                    for l in range(32):
                        op(PE, "matmul", [cw1[kd], cposT], [pbias], pbias[:, col:col + 1], lhsT=cw1[kd][0:64, l, n_ * 128:(n_ + 1) * 128], rhs=cposT[0:64, l:l + 1], start=(l == 0), stop=(l == 31))
            op(DVE, "tensor_copy", [pbias], [cbias], out=cbias[:], in_=pbias[:, 0:4])
            for ki, (kd, tok) in enumerate((("k", kcTok), ("v", vcTok))):
                for g in range(2):
                    hid = hidT.get()
                    for n_ in range(2):
                        ph = psM.get()
                        for l in range(32):
                            op(PE, "matmul", [cw1[kd], tok], [ph], ph[:, 0:255], lhsT=cw1[kd][64 * g:64 * g + 64, l, n_ * 128:(n_ + 1) * 128],
                               rhs=tok[64 * g:64 * g + 64, l:l + 16 * 254 + 1:16], start=(l == 0), stop=(l == 31))
                        op(ACT, "activation", [ph, cbias], [hid], out=hid[:, n_, 0:255], in_=ph[:, 0:255], func=AF.Gelu_apprx_tanh, bias=cbias[:, ki * 2 + n_:ki * 2 + n_ + 1])
                    for ct in range(2):
                        ncs = 128 if ct == 0 else 127
                        po = psM.get()
                        for n_ in range(2):
                            op(PE, "matmul", [hid, cw2[kd]], [po], po[0:ncs, 0:64], lhsT=hid[:, n_, ct * 128:ct * 128 + ncs], rhs=cw2[kd][:, n_, :], start=(n_ == 0), stop=(n_ == 1))
                        if kd == "k":
                            op(ACT, "activation", [po], [kcf[ct]], out=kcf[ct][0:ncs, g, :], in_=po[0:ncs, 0:64], func=AF.Copy)
                        else:
                            op(ACT, "activation", [po], [VcA], out=VcA[0:ncs, ct, g, 0:64], in_=po[0:ncs, 0:64], func=AF.Copy)
            for ct in range(2):
                norm_rope(kcf[ct], kcb[ct], 2, qkw, coscT[:, ct:ct + 1, :], sincT[:, ct:ct + 1, :], [coscT, sincT], qn2, st2, qw2, t12b, t34b, wo=8)
                op(PE, "transpose", [kcb[ct], identb], [psQ], out=psQ[:, ct, :], in_=kcb[ct][:].rearrange("p h d -> p (h d)"), identity=identb[:])
            op(ACT, "activation", [psQ], [kcT], out=kcT[:, 0:255], in_=psQ[:].rearrange("p c t -> p (c t)")[:, 0:255], func=AF.Copy)
            fw.barrier()
        tokst.close()
        if stop == 2:
            att.close()
            return nc

        qT = fw.sb("qT", [128, 4, T], BF16, att)
        KA = [fw.sb("KA%d" % g_, [128, T], BF16, att) for g_ in range(2)]
        dma(SP, KA[0], [], [KA[0]], out=KA[0][64:128, :], in_=efull_d[:, :])
        dma(SP, KA[1], [], [KA[1]], out=KA[1][0:64, :], in_=efull_d[:, :])
        kwT = fw.sb("kwT", [128, T], BF16, att)
        Vs = fw.sb("Vs", [128, NT, 2, 65], BF16, att)
        Vw = fw.sb("Vw", [128, NT, 2, 65], BF16, att)
        gates = fw.sb("gates", [128, NT, 24], F32, att)
        op(POOL, "memset", [], [Vs], Vs[:, :, :, 64:65], 1.0)
        op(POOL, "memset", [], [Vw], Vw[:, :, :, 64:65], 1.0)
        with ExitStack() as p1:
            W = nt_work(p1)
            WinB = fw.sb("WinB", [128, 8, 1048], BF16, p1)
            for k in range(8):
                dma(POOL, WinB, [], [WinB], out=WinB[:, k, 0:512], in_=win_d[k * 128:(k + 1) * 128, 1024:1536])
                dma(POOL, WinB, [], [WinB], out=WinB[:, k, 512:1048], in_=win_d[k * 128:(k + 1) * 128, 1792:2328])
            psM = Ring([fw.ps("psM", [128, 512], F32, p1) for _ in range(4)])
            psQ = Ring([fw.ps("psQ", [128, 6, 128], BF16, p1) for _ in range(2)])
            qkf = Ring([fw.sb("qkf", [128, 12, 64], F32, p1) for _ in range(2)])
            qn = fw.sb("qn", [128, 12, 64], F32, p1)
            qkb = Ring([fw.sb("qkb", [128, 12, 64], BF16, p1) for _ in range(2)])
            st12 = Ring([fw.sb("st12", [128, 12], F32, p1) for _ in range(4)])
            qw16 = fw.sb("qw16", [128, 12, 16], F32, p1)
            t12 = fw.sb("t12", [128, 12, 16], F32, p1)
            t34 = fw.sb("t34", [128, 12, 16], F32, p1)
            for tb in range(8):
                hT = W["hT"].get()
                norm_transpose_block(tb, x_d, anw, W, hT)
                for j in range(4):
                    tt = tb * 4 + j
                    qf = qkf.get()
                    pA = psM.get()
                    for k in range(8):
                        op(PE, "matmul", [hT, WinB], [pA], pA[:, 0:512], lhsT=hT[:, k, j * 128:(j + 1) * 128], rhs=WinB[:, k, 0:512], start=(k == 0), stop=(k == 7))
                    op(ACT, "activation", [pA], [qf], out=qf[:, 0:8, :].rearrange("p (i g) d -> p i g d", g=2),
                       in_=pA[:, 0:512].rearrange("p (g i d) -> p i g d", g=2, i=4), func=AF.Copy)
                    pB = psM.get()
                    for k in range(8):
                        op(PE, "matmul", [hT, WinB], [pB], pB[:, 0:256], lhsT=hT[:, k, j * 128:(j + 1) * 128], rhs=WinB[:, k, 512:768], start=(k == 0), stop=(k == 7))
                    op(ACT, "activation", [pB], [qf], out=qf[:, 8:10, :].rearrange("p h d -> p (h d)"), in_=pB[:, 0:128], func=AF.Copy)
                    op(ACT, "activation", [pB], [Vs], out=Vs[:, tt, :, 0:64], in_=pB[:, 128:256].rearrange("p (g d) -> p g d", g=2), func=AF.Copy)
                    pC = psM.get()
                    for k in range(8):
                        op(PE, "matmul", [hT, WinB], [pC], pC[:, 0:280], lhsT=hT[:, k, j * 128:(j + 1) * 128], rhs=WinB[:, k, 768:1048], start=(k == 0), stop=(k == 7))
                    op(ACT, "activation", [pC], [qf], out=qf[:, 10:12, :].rearrange("p h d -> p (h d)"), in_=pC[:, 0:128], func=AF.Copy)
                    op(ACT, "activation", [pC], [Vw], out=Vw[:, tt, :, 0:64], in_=pC[:, 128:256].rearrange("p (g d) -> p g d", g=2), func=AF.Copy)
                    op(ACT, "activation", [pC], [gates], out=gates[:, tt, :], in_=pC[:, 256:280], func=AF.Sigmoid)
                    qb = qkb.get()
                    norm_rope(qf, qb, 12, qkw, cosT[:, tt:tt + 1, :], sinT[:, tt:tt + 1, :], [cosT, sinT], qn, st12, qw16, t12, t34)
                    pQ = psQ.get()
                    for i in range(6):
                        op(PE, "transpose", [qb, identb], [pQ], out=pQ[:, i, :], in_=qb[:, 2 * i:2 * i + 2, :].rearrange("p h d -> p (h d)"), identity=identb[:])
                    op(ACT, "activation", [pQ], [qT], out=qT[:, :, tt * 128:(tt + 1) * 128], in_=pQ[:, 0:4, :], func=AF.Copy)
                    op(ACT, "activation", [pQ], [KA[0]], out=KA[0][0:64, tt * 128:(tt + 1) * 128], in_=pQ[0:64, 4, :], func=AF.Copy)
                    op(ACT, "activation", [pQ], [KA[1]], out=KA[1][64:128, tt * 128:(tt + 1) * 128], in_=pQ[64:128, 4, :], func=AF.Copy)
                    op(ACT, "activation", [pQ], [kwT], out=kwT[:, tt * 128:(tt + 1) * 128], in_=pQ[:, 5, :], func=AF.Copy)
            fw.barrier()

        if stop == "B":
            att.close()
            return nc
        with ExitStack() as p3:
            tri4 = fw.sb("tri4", [128, 4, 128], BF16, p3)
            wlow4 = fw.sb("wlow4", [128, 4, 128], BF16, p3)
            cmask = fw.sb("cmask", [128, 33, 128], BF16, p3)
            vmbs = fw.sb("vmbs", [128, 2, 128], F32, p3)
            wout = fw.sb("wout", [128, 8, D], BF16, p3)
            aonw = fw.sb("aonw", [128, 512], F32, p3)
            dma(SP, tri4, [], [tri4], out=tri4[:].rearrange("p h t -> p (h t)"), in_=tri4_d[:, :])
            dma(SP, wlow4, [], [wlow4], out=wlow4[:].rearrange("p h t -> p (h t)"), in_=wlow4_d[:, :])
            dma(SP, cmask, [], [cmask], out=cmask[:].rearrange("p m t -> p (m t)"), in_=cmask_d[:, :])
            dma(SP, vmbs, [], [vmbs], out=vmbs[:].rearrange("p a t -> p (a t)"), in_=vmbs_d[:, :])
            dma(SP, aonw, [], [aonw], out=aonw[:], in_=aonw_d[:, :])
            for k in range(8):
                dma(POOL, wout, [], [wout], out=wout[:, k, :], in_=wout_d[k * 128:(k + 1) * 128, :])
            psS = Ring([fw.ps("psS3", [128, 4, 128], F32, p3) for _ in range(3)])
            psC = fw.ps("psC", [128, 4, 128], F32, p3)
            psSel = fw.ps("psSel", [128, 4, 128], F32, p3)
            psWin = fw.ps("psWin", [128, 4, 128], F32, p3)
            psX = Ring([fw.ps("psX", [128, 512], F32, p3) for _ in range(1)])
            psTb = fw.ps("psTb", [128, 1024], BF16, p3)
            Er = Ring([fw.sb("E", [128, 4, 128], BF16, p3) for _ in range(4)])
            xr3 = Ring([fw.sb("xt3", [128, D], F32, p3) for _ in range(2)])
            ocmp = [fw.sb("ocmp", [128, 4, 64], F32, p3) for _ in range(2)]
            RA = [[fw.sb("RA", [128, 4, 128], BF16, p3) for _ in range(2)] for _ in range(2)]
            negm2 = [fw.sb("negm2", [128, 128], BF16, p3) for _ in range(2)]
            sm = Ring([fw.sb("sm", [128, 16], F32, p3) for _ in range(12)])
            imp = fw.sb("imp", [128, 64], F32, p3)
            score = fw.sb("score", [128, 64], F32, p3)
            score2 = fw.sb("score2", [128, 64], F32, p3)
            negm = fw.sb("negm", [128, 64], BF16, p3)
            yatts = [fw.sb("yatt", [128, 8, 64], F32, p3) for _ in range(2)]
            ytmp = fw.sb("ytmp", [128, 4, 64], F32, p3)
            yb = fw.sb("yb", [128, 512], BF16, p3)
            yjunk = fw.sb("yjunk", [128, 512], BF16, p3)
            yattT = fw.sb("yattT", [128, 4, 128], BF16, p3)

            for g in range(2):
                op(POOL, "memset", [], [negm2[g]], negm2[g][:], 0.0)

            def qslice(g, qt):
                return qT[64 * g:64 * g + 64, :, qt * 128:(qt + 1) * 128]

            ytr = Ring([fw.sb("ytmp", [128, 4, 64], F32, p3) for _ in range(2)])
            xts3 = {}

            def gview(qt, g):
                return gates[:, qt, g * 12:(g + 1) * 12].rearrange("p (h b) -> p b h", b=3)

            deferred = []

            def topk(qt, g):
                gv = gview(qt, g)
                yatt = yatts[qt % 2]
                sums = sm.get()
                op(DVE, "tensor_reduce", [psC], [sums], out=sums[:, 0:4], in_=psC[:, :, 64:128], axis=AX.X, op=ALU.add)
                op(DVE, "tensor_scalar", [sums], [sums], out=sums[:, 4:8], in0=sums[:, 0:4], scalar1=1e-30, scalar2=None, op0=ALU.max)
                op(DVE, "reciprocal", [sums], [sums], out=sums[:, 8:12], in_=sums[:, 4:8])
                op(DVE, "tensor_tensor", [sums, gates], [sums], out=sums[:, 12:16], in0=sums[:, 8:12], in1=gv[:, 0, :], op=ALU.mult)
                op(DVE, "tensor_tensor", [psC, sums], [yatt], out=yatt[:, g * 4:(g + 1) * 4, :], in0=psC[:, :, 0:64], in1=sums[:, 12:16].unsqueeze(2).broadcast_to([128, 4, 64]), op=ALU.mult)
                op(DVE, "tensor_scalar", [psC, sums], [imp], out=imp[:], in0=psC[:, 0, 64:128], scalar1=sums[:, 8:9], scalar2=None, op0=ALU.mult)
                for h in range(1, 4):
                    op(DVE, "scalar_tensor_tensor", [psC, sums, imp], [imp], out=imp[:], in0=psC[:, h, 64:128], scalar=sums[:, 8 + h:9 + h], in1=imp[:], op0=ALU.mult, op1=ALU.add)
                lo = 64 - 2 * qt
                op(DVE, "tensor_tensor", [imp, vmbs], [score], out=score[:], in0=imp[:], in1=vmbs[:, 0, lo:lo + 64], op=ALU.mult)
                op(DVE, "tensor_tensor", [score, vmbs], [score], out=score[:], in0=score[:], in1=vmbs[:, 1, lo:lo + 64], op=ALU.add)
                op(DVE, "memset", [], [score], score[:, 0:1], 1.0e4)
                m8 = sm.get()
                op(DVE, "max", [score], [m8], out=m8[:, 0:8], in_=score[:])
                op(DVE, "match_replace", [m8, score], [score2], out=score2[:], in_to_replace=m8[:, 0:8], in_values=score[:], imm_value=-3.0e38)
                op(DVE, "max", [score2], [m8], out=m8[:, 8:16], in_=score2[:])
                c0 = 64 if g == 0 else 0
                nm = negm2[g]
                op(DVE, "tensor_scalar", [score, m8], [nm], out=nm[:, c0:c0 + 64], in0=score[:], scalar1=m8[:, 15:16], scalar2=NEG, op0=ALU.is_lt, op1=ALU.mult)
                _nw = 2 * min(qt + 1, 5)
                _allow = ((1 if qt < 16 else 2) + _nw - 2) if g == 0 else (_nw + qt + 1 - 2)
                _dt = max(1, min(int(_os.environ.get('KTOPDEF', 5)), _allow))
                if _dt == 0:
                    topk_b(qt, g)
                else:
                    deferred.append([_dt, lambda: topk_b(qt, g)])

            def topk_b(qt, g):
                nm = negm2[g]
                pNb = psTb[:, 512:1024]
                op(PE, "transpose", [nm, identb], [psTb], out=pNb[:, 0:128], in_=nm[:], identity=identb[:])
                ra = RA[g][qt % 2]
                r0 = 64 if g == 0 else 0
                q0 = 0 if g == 0 else 64
                op(DVE, "tensor_copy", [psTb], [ra], out=ra[r0:r0 + 64, 0, :], in_=pNb[r0:r0 + 64, 0:128])
                op(POOL, "tensor_copy", [ra], [ra], out=ra[r0:r0 + 64, 1:4, :], in_=ra[r0:r0 + 64, 0:1, :].broadcast_to([64, 3, 128]))
                op(POOL, "tensor_copy", [qT], [ra], out=ra[q0:q0 + 64, :, :], in_=qT[q0:q0 + 64, :, qt * 128:(qt + 1) * 128])

            def evac(qt, g, pO, b):
                gv = gview(qt, g)
                yatt = yatts[qt % 2]
                cf = sm.get()
                op(DVE, "tensor_scalar", [pO], [cf], out=cf[:, 0:4], in0=pO[:, :, 64], scalar1=1e-30, scalar2=None, op0=ALU.max)
                op(DVE, "reciprocal", [cf], [cf], out=cf[:, 4:8], in_=cf[:, 0:4])
                op(DVE, "tensor_tensor", [cf, gates], [cf], out=cf[:, 8:12], in0=cf[:, 4:8], in1=gv[:, b, :], op=ALU.mult)
                yt = ytr.get()
                yg = yatt[:, g * 4:(g + 1) * 4, :]
                op(DVE, "tensor_tensor", [pO, cf], [yt], out=yt[:], in0=pO[:, :, 0:64], in1=cf[:, 8:12].unsqueeze(2).broadcast_to([128, 4, 64]), op=ALU.mult)
                op(POOL, "tensor_tensor", [yatt, yt], [yatt], out=yg, in0=yg, in1=yt[:], op=ALU.add)

            def finish_a(qt):
                yatt = yatts[qt % 2]
                yf = yatt[:].rearrange("p h d -> p (h d)")
                st = sm.get()
                op(DVE, "tensor_tensor", [yatt], [yjunk], out=yjunk[:], in0=yf, in1=yf, op=ALU.mult)
                op(DVE, "tensor_reduce", [yjunk], [st], out=st[:, 0:1], in_=yjunk[:], axis=AX.X, op=ALU.add)
                rsqrt_small(st[:, 0:1], [st], st, st[:, 2:3], 1, 1.0 / 512, sm.get())
                op(DVE, "scalar_tensor_tensor", [yatt, st, aonw], [yb], out=yb[:], in0=yf, scalar=st[:, 2:3], in1=aonw[:], op0=ALU.mult, op1=ALU.mult)

            def finish_b(qt):
                pYb = psTb[:, 0:512]
                for c in range(4):
                    op(PE, "transpose", [yb, identb], [psTb], out=pYb[:, c * 128:(c + 1) * 128], in_=yb[:, c * 128:(c + 1) * 128], identity=identb[:])
                op(DVE, "tensor_copy", [psTb], [yattT], out=yattT[:].rearrange("p c t -> p (c t)"), in_=pYb[:, 0:512])

            def finish_c(qt):
                xt = xts3.pop(qt)
                for half in range(2):
                    pO_ = psX.get()
                    for c in range(4):
                        op(PE, "matmul", [yrnnT, wout], [pO_], pO_[:], lhsT=yrnnT[:, c, qt * 128:(qt + 1) * 128], rhs=wout[:, c, half * 512:(half + 1) * 512], start=(c == 0), stop=False)
                    for c in range(4):
                        op(PE, "matmul", [yattT, wout], [pO_], pO_[:], lhsT=yattT[:, c, :], rhs=wout[:, 4 + c, half * 512:(half + 1) * 512], start=False, stop=(c == 3))
                    op(DVE, "tensor_tensor", [xt, pO_], [xt], out=xt[:, half * 512:(half + 1) * 512], in0=xt[:, half * 512:(half + 1) * 512], in1=pO_[:], op=ALU.add)
                dma(SP, xt, [xt], [ydr[qt]], out=y_d[qt * 128:(qt + 1) * 128, :], in_=xt[:])

            def finish(qt):
                finish_a(qt)
                _d = int(_os.environ.get('KDEF', 3))
                if _d == 0:
                    finish_b(qt)
                    finish_c(qt)
                else:
                    deferred.append([_d, lambda: finish_b(qt)])
                    deferred.append([2 * _d, lambda: finish_c(qt)])

            def load_x(qt):
                xt = xr3.get()
                xts3[qt] = xt
                dma(SP, xt, [], [xt], out=xt[:], in_=x_d[qt * 128:(qt + 1) * 128, :])

            import os as _os
            _nqt = int(_os.environ.get('KQT', NT))
            jobs = []

            def cmp_jobs(qt):
                for g in range(2):
                    cts = [0] if qt < 16 else [0, 1]
                    for ct in cts:
                        ncs = 128 if ct == 0 else 127
                        mi = (qt if qt <= 16 else None) if ct == 0 else 17 + qt - 16
                        mm = [(kcT[64 * g:64 * g + 64, ct * 128:ct * 128 + ncs], qslice(g, qt), [kcT, qT], None)]
                        if mi is not None:
                            for h in range(4):
                                mm.append((identb[0:ncs, 0:ncs], cmask[0:ncs, mi, :], [identb, cmask], h))
                        jobs.append(dict(qt=qt, ncs=ncs, mm=mm, pO=psC, V=VcA[0:ncs, ct, g, :], Vb=VcA, ncol=128, first=(ct == cts[0]), before=None,
                                         after=((lambda qt=qt, g=g: topk(qt, g)) if ct == cts[-1] else None)))

            def main_jobs(qt):
                first = True
                for g in range(2):
                    kts = list(range(max(0, qt - 4), qt + 1))
                    for kt in kts:
                        mm = [(kwT[64 * g:64 * g + 64, kt * 128:(kt + 1) * 128], qslice(g, qt), [kwT, qT], None)]
                        if kt == qt:
                            mm.append((identb[:], tri4[:], [identb, tri4], None))
                        if kt == qt - 4:
                            mm.append((identb[:], wlow4[:], [identb, wlow4], None))
                        jobs.append(dict(qt=qt, ncs=128, mm=mm, pO=psWin, V=Vw[:, kt, g, :], Vb=Vw, ncol=65, first=(kt == kts[0]), before=(load_x if first else None),
                                         after=((lambda qt=qt, g=g: evac(qt, g, psWin, 2)) if kt == kts[-1] else None)))
                        first = False
                for g in range(2):
                    kts = list(range(qt + 1))
                    for kt in kts:
                        mm = [(KA[g][:, kt * 128:(kt + 1) * 128], RA[g][qt % 2][:], [KA[g], RA[g][qt % 2]], None)]
                        if kt == qt:
                            mm.append((identb[:], tri4[:], [identb, tri4], None))
                        if kt == kts[-1]:
                            if g == 0:
                                aft = (lambda qt=qt, g=g: evac(qt, g, psSel, 1))
                            else:
                                aft = (lambda qt=qt, g=g: (evac(qt, g, psSel, 1), finish(qt)))
                        else:
                            aft = None
                        jobs.append(dict(qt=qt, ncs=128, mm=mm, pO=psSel, V=Vs[:, kt, g, :], Vb=Vs, ncol=65, first=(kt == kts[0]), before=None, after=aft))

            if _os.environ.get('KAHEAD', '0') == '1':
                for it in range(_nqt + 1):
                    if it < _nqt:
                        cmp_jobs(it)
                    if it >= 1:
                        main_jobs(it - 1)
            else:
                for it in range(_nqt):
                    cmp_jobs(it)
                    main_jobs(it)

            def emit_score(job):
                if job["before"]:
                    job["before"](job["qt"])
                pS = psS.get()
                ncs = job["ncs"]
                n = len(job["mm"])
                for i, (l_, r_, bufs_, h) in enumerate(job["mm"]):
                    o_ = pS[0:ncs, :, :] if h is None else pS[0:ncs, h, :]
                    op(PE, "matmul", bufs_, [pS], o_, lhsT=l_, rhs=r_, start=(i == 0), stop=(i == n - 1), skip_group_check=True)
                return pS

            def emit_rest(job, pS):
                ncs = job["ncs"]
                E = Er.get()
                op(ACT, "activation", [pS], [E], out=E[0:ncs], in_=pS[0:ncs], func=AF.Exp, scale=0.125)
                pO = job["pO"]
                for h in range(4):
                    op(PE, "matmul", [E, job["Vb"]], [pO], pO[:, h, 0:job["ncol"]], lhsT=E[0:ncs, h, :], rhs=job["V"], start=(job["first"] and h == 0), stop=True, skip_group_check=True)
                if job["after"]:
                    job["after"]()
                for d_ in list(deferred):
                    d_[0] -= 1
                    if d_[0] <= 0:
                        deferred.remove(d_)
                        d_[1]()

            LOOK = 2
            pend = []
            for job in jobs:
                pend.append((job, emit_score(job)))
                if len(pend) > LOOK:
                    emit_rest(*pend.pop(0))
            while pend:
                emit_rest(*pend.pop(0))
            for d_ in list(deferred):
                d_[1]()
            fw.barrier()
        att.close()
        if stop == 3:
            return nc

        with ExitStack() as p4:
            Wg = fw.sb("Wg", [128, 8, DFF], BF16, p4)
            Wu = fw.sb("Wu", [128, 8, DFF], BF16, p4)
            Wd = fw.sb("Wd", [128, NFF, D], BF16, p4)
            fnw = fw.sb("fnw", [128, D], F32, p4)
            dma(SP, fnw, [], [fnw], out=fnw[:], in_=fnw_d[:, :])
            for k in range(8):
                dma(POOL, Wg, [], [Wg], out=Wg[:, k, :], in_=wg_d[k * 128:(k + 1) * 128, :])
                dma(POOL, Wu, [], [Wu], out=Wu[:, k, :], in_=wu_d[k * 128:(k + 1) * 128, :])
            for j in range(NFF):
                dma(POOL, Wd, [], [Wd], out=Wd[:, j, :], in_=wd_d[j * 128:(j + 1) * 128, :])
            W = nt_work(p4, nx=5, nh=1)
            psG = Ring([fw.ps("psG", [128, 512], F32, p4) for _ in range(2)])
            psU = Ring([fw.ps("psU", [128, 512], F32, p4) for _ in range(2)])
            psD = Ring([fw.ps("psD", [128, 512], F32, p4) for _ in range(2)])
            sg = Ring([fw.sb("sg", [128, 512], F32, p4) for _ in range(2)])
            aT = fw.sb("aT", [128, NFF, 512], BF16, p4)
            for tb in range(8):
                hT = W["hT"].get()
                xts = norm_transpose_block(tb, y_d, fnw, W, hT, srcbufs=ydr)
                for j in range(NFF):
                    pg, pu = psG.get(), psU.get()
                    for k in range(8):
                        op(PE, "matmul", [Wg, hT], [pg], pg[:], lhsT=Wg[:, k, j * 128:(j + 1) * 128], rhs=hT[:, k, :], start=(k == 0), stop=(k == 7))
                    for k in range(8):
                        op(PE, "matmul", [Wu, hT], [pu], pu[:], lhsT=Wu[:, k, j * 128:(j + 1) * 128], rhs=hT[:, k, :], start=(k == 0), stop=(k == 7))
                    sg_ = sg.get()
                    op(ACT, "activation", [pg], [sg_], out=sg_[:], in_=pg[:], func=AF.Silu)
                    op(DVE, "tensor_tensor", [sg_, pu], [aT], out=aT[:, j, :], in0=sg_[:], in1=pu[:], op=ALU.mult)
                for i in range(4):
                    tt = tb * 4 + i
                    xt = xts[i]
                    for half in range(2):
                        pd = psD.get()
                        for j in range(NFF):
                            op(PE, "matmul", [aT, Wd], [pd], pd[:], lhsT=aT[:, j, i * 128:(i + 1) * 128], rhs=Wd[:, j, half * 512:(half + 1) * 512], start=(j == 0), stop=(j == NFF - 1))
                        op(DVE, "tensor_tensor", [xt, pd], [xt], out=xt[:, half * 512:(half + 1) * 512], in0=xt[:, half * 512:(half + 1) * 512], in1=pd[:], op=ALU.add)
                    dma(SP, xt, [xt], [ydr[tt]], out=y_d[tt * 128:(tt + 1) * 128, :], in_=xt[:])
            fw.barrier()
    return nc


def host_inputs(inp, b):
    f32 = np.float32
    m = {}
    m["x"] = np.ascontiguousarray(inp["x"][b], dtype=f32)
    pos = np.asarray(inp["positions"][b], dtype=np.int32)
    m["pos"] = np.ascontiguousarray(pos.reshape(NT, 128).T)
    pc = np.zeros(256, np.int32)
    pc[:255] = pos[np.arange(255) * 16 + 31]
    m["posc"] = np.ascontiguousarray(pc.reshape(2, 128).T)
    invf = (500000.0 ** (-np.arange(8, dtype=np.float32) * 2.0 / 16)).astype(f32)
    m["invf"] = np.ascontiguousarray(np.broadcast_to(invf, (128, 8)), dtype=f32)
    m["w_in"] = np.ascontiguousarray(inp["w_in"][0], dtype=f32)
    m["anw"] = np.ascontiguousarray(np.broadcast_to(inp["attn_norm_w"][0], (128, D)), dtype=f32)
    qkw = np.concatenate([np.tile(inp["q_norm_w"][0], 8), np.tile(inp["k_norm_w"][0], 4)])
    m["qkw"] = np.ascontiguousarray(np.broadcast_to(qkw, (128, 768)), dtype=f32)
    cw = inp["conv_w"][0].reshape(4, 4, 128)
    m["convw"] = np.ascontiguousarray(cw.transpose(2, 1, 0).reshape(128, 16), dtype=f32)
    rp = np.stack([inp["conv_b"][0], inp["gate_a_b"][0], inp["gate_x_b"][0], inp["lru_lambda"][0], inp["rnn_out_norm_w"][0]], 0)
    m["rnnp"] = np.ascontiguousarray(rp.reshape(5, 4, 128).transpose(2, 1, 0).reshape(128, 20), dtype=f32)
    for nm, key in (("gaw", "gate_a_w"), ("gxw", "gate_x_w")):
        w = inp[key][0]
        bd = np.zeros((128, 4, 128), f32)
        for c in range(4):
            for i in range(2):
                bd[64 * i:64 * i + 64, c, 64 * i:64 * i + 64] = w[2 * c + i]
        m[nm] = bd.reshape(128, 512)
    m["identf"] = np.eye(128, dtype=f32)
    for kd in ("k", "v"):
        w1 = inp["cmp_%s_w1" % kd][0].reshape(32, 64, 256).transpose(1, 0, 2).reshape(64, 32 * 256)
        m["cw1" + kd] = np.ascontiguousarray(np.concatenate([w1, w1], 0), dtype=f32)
        w2 = inp["cmp_%s_w2" % kd][0].reshape(2, 128, 64).transpose(1, 0, 2).reshape(128, 128)
        m["cw2" + kd] = np.ascontiguousarray(w2, dtype=f32)
    cp = inp["cmp_pos"][0].T
    m["cposT"] = np.ascontiguousarray(np.concatenate([cp, cp], 0), dtype=f32)
    m.update(CONSTS)
    m["w_out"] = np.ascontiguousarray(inp["w_out"][0], dtype=f32)
    m["aonw"] = np.ascontiguousarray(np.broadcast_to(inp["attn_out_norm_w"][0], (128, 512)), dtype=f32)
    m["fnw"] = np.ascontiguousarray(np.broadcast_to(inp["ffn_norm_w"][0], (128, D)), dtype=f32)
    m["w_gate"] = np.ascontiguousarray(inp["w_gate"][0], dtype=f32)
    m["w_up"] = np.ascontiguousarray(inp["w_up"][0], dtype=f32)
    m["w_down"] = np.ascontiguousarray(inp["w_down"][0], dtype=f32)
    return m


def _consts():
    bf = ml_dtypes.bfloat16
    f32 = np.float32
    c = {}
    kl = np.arange(128)[:, None]
    ql = np.arange(128)[None, :]
    tri = np.where(kl <= ql, 0.0, NEG).astype(f32)
    c["tri4"] = np.ascontiguousarray(np.tile(tri, (1, 4))).astype(bf)
    wl = np.where(kl > ql, 0.0, NEG).astype(f32)
    c["wlow4"] = np.ascontiguousarray(np.tile(wl, (1, 4))).astype(bf)
    cm = np.zeros((128, 33, 128), f32)
    for mi in range(33):
        ct, qt = (0, mi) if mi <= 16 else (1, 16 + mi - 17)
        dv = 128 * qt - 2048 * ct - 31
        cm[:, mi, :] = np.where(16 * kl - ql <= dv, 0.0, NEG)
    c["cmask"] = np.ascontiguousarray(cm.reshape(128, 33 * 128)).astype(bf)
    ef = (np.arange(T)[None, :] // 64 == np.arange(64)[:, None]).astype(f32)
    c["efull"] = np.ascontiguousarray(ef).astype(bf)
    cs = np.arange(255)[:, None] * 16
    bs = np.arange(64)[None, :] * 64
    cov = np.clip(np.minimum(cs + 32, bs + 64) - np.maximum(cs, bs), 0, None).astype(f32) / 32.0
    covp = np.zeros((256, 64), f32)
    covp[:255] = cov
    c["cover"] = np.ascontiguousarray(covp.reshape(2, 128, 64).transpose(1, 0, 2).reshape(128, 128))
    tl = np.arange(128)[:, None]
    jp = np.arange(128)[None, :] - 64
    cc = (tl >= 64).astype(np.int64)
    forced = (jp == cc) | (jp == cc - 1)
    invalid = jp > cc
    vm = np.where(forced | invalid, 0.0, 1.0).astype(f32)
    bsb = np.where(forced, 1.0e4, np.where(invalid, -1.0e30, 0.0)).astype(f32)
    c["vmbs"] = np.ascontiguousarray(np.concatenate([vm, bsb], 1))
    return c


CONSTS = _consts()


def kernel(**inputs):
    inp = {k: np.asarray(v) for k, v in inputs.items()}
    nc = build()
    in_maps = [host_inputs(inp, b) for b in range(8)]
    res = run_bass_kernel_spmd(nc, in_maps, core_ids=list(range(8)))
    return np.stack([r["y"] for r in res.results], 0).astype(np.float32)
```

```python
import numpy as np
import ml_dtypes
from contextlib import ExitStack
import concourse.bass as bass
import concourse.mybir as mybir
from concourse.bass_utils import run_bass_kernel_spmd

F32, BF16, I32 = mybir.dt.float32, mybir.dt.bfloat16, mybir.dt.int32
AF = mybir.ActivationFunctionType
ALU = mybir.AluOpType
AX = mybir.AxisListType

T = 4096
NT = 32
D = 1024
DIN = 2328
DFF = 2816
NFF = 22
EPS = 1e-6
NEG = -30000.0
TWO_PI = 6.283185307179586
PI = 3.141592653589793


class Buf:
    def __init__(self, fw, t, name):
        self.fw, self.t, self.name = fw, t, name
        self.w = None
        self.r = {}
        self.dsem = None
        self.dkey = None
        self.dcnt = 0

    def __getitem__(self, k):
        return self.t[k]


class Eng:
    def __init__(self, fw, name, eng, sem, key, selfwait):
        self.fw, self.name, self.eng, self.sem, self.key = fw, name, eng, sem, key
        self.selfwait = selfwait
        self.cnt = 0
        self.waited = {}

    def sync(self, reads, writes):
        waits = {}

        def need(ev, raw):
            if ev is None:
                return
            key, sem, val = ev
            if key == self.key and not (self.selfwait and raw):
                return
            if self.waited.get(key, 0) >= val:
                return
            if key not in waits or waits[key][1] < val:
                waits[key] = (sem, val)

        for b in reads:
            need(b.w, True)
        for b in writes:
            need(b.w, False)
            for ev in b.r.values():
                need(ev, False)
        for key, (sem, val) in waits.items():
            self.eng.wait_ge(sem, val)
            self.waited[key] = val

    def wait_ev(self, ev):
        key, sem, val = ev
        if self.waited.get(key, 0) >= val:
            return
        self.eng.wait_ge(sem, val)
        self.waited[key] = val


class FW:
    def __init__(self, nc, es):
        self.nc, self.es = nc, es
        self.nkey = 0
        self.engs = {}
        for name, eng, sw in (("PE", nc.tensor, False), ("ACT", nc.scalar, True),
                              ("DVE", nc.vector, True), ("POOL", nc.gpsimd, True),
                              ("SP", nc.sync, False)):
            sem = es.enter_context(nc.semaphore("sem_" + name))
            self.engs[name] = Eng(self, name, eng, sem, self.newkey(), sw)
        self.PE, self.ACT, self.DVE, self.POOL, self.SP = (self.engs[n] for n in ("PE", "ACT", "DVE", "POOL", "SP"))
        self.carriers = []
        self.nbuf = 0

    def newkey(self):
        self.nkey += 1
        return self.nkey

    def sb(self, name, shape, dt, es=None):
        es = es or self.es
        self.nbuf += 1
        t = es.enter_context(self.nc.sbuf_tensor("%s_%d" % (name, self.nbuf), list(shape), dt))
        return Buf(self, t, name)

    def ps(self, name, shape, dt, es=None):
        es = es or self.es
        self.nbuf += 1
        t = es.enter_context(self.nc.psum_tensor("%s_%d" % (name, self.nbuf), list(shape), dt))
        return Buf(self, t, name)

    def dram(self, ap, name):
        return Buf(self, ap, name)

    def op(self, E, meth, reads, writes, *a, **kw):
        E.sync(reads, writes)
        ins = getattr(E.eng, meth)(*a, **kw)
        E.cnt += 1
        ins.then_inc(E.sem, 1)
        ev = (E.key, E.sem, E.cnt)
        for b in reads:
            b.r[E.key] = ev
        for b in writes:
            b.w = ev
            b.r = {}
        return ins

    def dma(self, Q, carrier, reads, writes, out, in_, **kw):
        Q.sync(reads, writes)
        if carrier.dsem is None:
            carrier.dsem = self.es.enter_context(self.nc.semaphore("dsem_%s_%d" % (carrier.name, len(self.carriers))))
            carrier.dkey = self.newkey()
            self.carriers.append(carrier)
        ins = Q.eng.dma_start(out=out, in_=in_, **kw)
        carrier.dcnt += 16
        ins.then_inc(carrier.dsem, 16)
        ev = (carrier.dkey, carrier.dsem, carrier.dcnt)
        for b in reads:
            b.r[carrier.dkey] = ev
        for b in writes:
            b.w = ev
            b.r = {}
        return ins

    def barrier(self):
        SP = self.SP
        for E in self.engs.values():
            if E is not SP and E.cnt > 0:
                SP.wait_ev((E.key, E.sem, E.cnt))
        for c in self.carriers:
            if c.dcnt > 0:
                SP.wait_ev((c.dkey, c.dsem, c.dcnt))
        ins = SP.eng.nop()
        SP.cnt += 1
        ins.then_inc(SP.sem, 1)
        ev = (SP.key, SP.sem, SP.cnt)
        for E in self.engs.values():
            if E is not SP:
                E.wait_ev(ev)


class Ring:
    def __init__(self, bufs):
        self.bufs, self.i = bufs, 0

    def get(self):
        b = self.bufs[self.i % len(self.bufs)]
        self.i += 1
        return b


def build(dbg=None, stop=None):
    nc = bass.Bass("TRN2", target_bir_lowering=False)

    def din(name, shape, dt=F32):
        return nc.dram_tensor(name, list(shape), dt, kind="ExternalInput").ap()

    x_d = din("x", [T, D])
    pos_d = din("pos", [128, NT], I32)
    posc_d = din("posc", [128, 2], I32)
    invf_d = din("invf", [128, 8])
    win_d = din("w_in", [D, DIN])
    anw_d = din("anw", [128, D])
    qkw_d = din("qkw", [128, 12 * 64])
    convw_d = din("convw", [128, 16])
    rnnp_d = din("rnnp", [128, 20])
    gaw_d = din("gaw", [128, 512])
    gxw_d = din("gxw", [128, 512])
    identf_d = din("identf", [128, 128])
    cw1_d = {"k": din("cw1k", [128, 32 * 256]), "v": din("cw1v", [128, 32 * 256])}
    cw2_d = {"k": din("cw2k", [128, 128]), "v": din("cw2v", [128, 128])}
    cposT_d = din("cposT", [128, 32])
    cover_d = din("cover", [128, 128])
    tri4_d = din("tri4", [128, 512], BF16)
    wlow4_d = din("wlow4", [128, 512], BF16)
    cmask_d = din("cmask", [128, 33 * 128], BF16)
    efull_d = din("efull", [64, T], BF16)
    vmbs_d = din("vmbs", [128, 256])
    wout_d = din("w_out", [D, D])
    aonw_d = din("aonw", [128, 512])
    fnw_d = din("fnw", [128, D])
    wg_d = din("w_gate", [D, DFF])
    wu_d = din("w_up", [D, DFF])
    wd_d = din("w_down", [DFF, D])
    y_d = nc.dram_tensor("y", [T, D], F32, kind="ExternalOutput").ap()
    dbg_d = {}
    if dbg:
        for nm, shp, dt in dbg:
            dbg_d[nm] = nc.dram_tensor("dbg_" + nm, list(shp), dt, kind="ExternalOutput").ap()

    with ExitStack() as es:
        fw = FW(nc, es)
        PE, ACT, DVE, POOL, SP = fw.PE, fw.ACT, fw.DVE, fw.POOL, fw.SP
        op, dma = fw.op, fw.dma
        att = ExitStack()
        tokst = ExitStack()

        identf = fw.sb("identf", [128, 128], F32)
        identb = fw.sb("identb", [128, 128], BF16)
        dma(SP, identf, [], [identf], out=identf[:], in_=identf_d[:, :])
        op(DVE, "tensor_copy", [identf], [identb], out=identb[:], in_=identf[:])
        ones_col = fw.sb("ones_col", [128, 1], F32)
        ones_row = fw.sb("ones_row", [1, 128], F32)
        neghalf = fw.sb("neghalf", [128, 512], F32)
        op(DVE, "memset", [], [ones_col], ones_col[:], 1.0)
        op(DVE, "memset", [], [ones_row], ones_row[:], 1.0)
        op(DVE, "memset", [], [neghalf], neghalf[:], -0.5)

        anw = fw.sb("anw", [128, D], F32, att)
        dma(SP, anw, [], [anw], out=anw[:], in_=anw_d[:, :])
        qkw = fw.sb("qkw", [128, 12, 64], F32, att)
        dma(SP, qkw, [], [qkw], out=qkw[:].rearrange("p h d -> p (h d)"), in_=qkw_d[:, :])
        convw = fw.sb("convw", [128, 4, 4], F32, att)
        dma(SP, convw, [], [convw], out=convw[:].rearrange("p c k -> p (c k)"), in_=convw_d[:, :])
        rnnp = fw.sb("rnnp", [128, 4, 5], F32, att)
        dma(SP, rnnp, [], [rnnp], out=rnnp[:].rearrange("p c k -> p (c k)"), in_=rnnp_d[:, :])
        gaw = fw.sb("gaw", [128, 4, 128], BF16, att)
        gxw = fw.sb("gxw", [128, 4, 128], BF16, att)
        dma(POOL, gaw, [], [gaw], out=gaw[:].rearrange("p c k -> p (c k)"), in_=gaw_d[:, :])
        dma(POOL, gxw, [], [gxw], out=gxw[:].rearrange("p c k -> p (c k)"), in_=gxw_d[:, :])

        def rope_tables(pos_ap, n, name):
            cosT_ = fw.sb(name + "cos", [128, n, 16], F32, att)
            sinT_ = fw.sb(name + "sin", [128, n, 16], F32, att)
            with ExitStack() as tmp:
                posi = fw.sb(name + "posi", [128, n], I32, tmp)
                dma(SP, posi, [], [posi], out=posi[:], in_=pos_ap)
                invf = fw.sb(name + "invf", [128, 8], F32, tmp)
                dma(SP, invf, [], [invf], out=invf[:], in_=invf_d[:, :])
                posf = fw.sb(name + "posf", [128, n], F32, tmp)
                op(DVE, "tensor_copy", [posi], [posf], out=posf[:], in_=posi[:])
                ang = fw.sb(name + "ang", [128, n, 8], F32, tmp)
                op(DVE, "tensor_tensor", [posf, invf], [ang], out=ang[:],
                   in0=posf[:].unsqueeze(2).broadcast_to([128, n, 8]),
                   in1=invf[:].unsqueeze(1).broadcast_to([128, n, 8]), op=ALU.mult)
                a2 = fw.sb(name + "a2", [128, n * 8], F32, tmp)
                ki = fw.sb(name + "ki", [128, n * 8], I32, tmp)
                kf = fw.sb(name + "kf", [128, n * 8], F32, tmp)
                m = fw.sb(name + "m", [128, n * 8], F32, tmp)
                for tab, shift in ((cosT_, PI / 2), (sinT_, 0.0)):
                    angf = ang[:].rearrange("p n f -> p (n f)")
                    op(DVE, "tensor_scalar", [ang], [a2], out=a2[:], in0=angf, scalar1=shift, scalar2=None, op0=ALU.add)
                    op(DVE, "tensor_scalar", [a2], [kf], out=kf[:], in0=a2[:], scalar1=1.0 / TWO_PI, scalar2=None, op0=ALU.mult)
                    op(DVE, "tensor_copy", [kf], [ki], out=ki[:], in_=kf[:])
                    op(DVE, "tensor_copy", [ki], [kf], out=kf[:], in_=ki[:])
                    op(DVE, "scalar_tensor_tensor", [kf, a2], [a2], out=a2[:], in0=kf[:], scalar=-TWO_PI, in1=a2[:], op0=ALU.mult, op1=ALU.add)
                    op(DVE, "tensor_scalar", [a2], [m], out=m[:], in0=a2[:], scalar1=PI, scalar2=None, op0=ALU.is_gt)
                    op(DVE, "scalar_tensor_tensor", [m, a2], [a2], out=a2[:], in0=m[:], scalar=-TWO_PI, in1=a2[:], op0=ALU.mult, op1=ALU.add)
                    op(DVE, "tensor_scalar", [a2], [m], out=m[:], in0=a2[:], scalar1=-PI, scalar2=None, op0=ALU.is_lt)
                    op(DVE, "scalar_tensor_tensor", [m, a2], [a2], out=a2[:], in0=m[:], scalar=TWO_PI, in1=a2[:], op0=ALU.mult, op1=ALU.add)
                    op(DVE, "tensor_scalar", [a2], [a2], out=a2[:], in0=a2[:], scalar1=PI, scalar2=-PI, op0=ALU.min, op1=ALU.max)
                    a3 = a2[:].rearrange("p (n f) -> p n f", f=8)
                    op(ACT, "activation", [a2], [tab], out=tab[:, :, 0:8], in_=a3, func=AF.Sin)
                    op(ACT, "activation", [a2], [tab], out=tab[:, :, 8:16], in_=a3, func=AF.Sin)
                fw.barrier()
            return cosT_, sinT_

        cosT, sinT = rope_tables(pos_d[:, :], NT, "rp")
        coscT, sincT = rope_tables(posc_d[:, :], 2, "rc")

        def rsqrt_small(src_ap, srcbufs, dst, dst_ap, n, scale, tmp):
            op(DVE, "tensor_scalar", srcbufs, [tmp], out=tmp[:, 0:n], in0=src_ap, scalar1=scale, scalar2=EPS, op0=ALU.mult, op1=ALU.add)
            op(POOL, "tensor_tensor", [tmp, neghalf], [dst], out=dst_ap, in0=tmp[:, 0:n], in1=neghalf[:, 0:n], op=ALU.pow)

        def norm_rope(qf, qb, nh, wt, cos_ap, sin_ap, csbufs, qn, st12, qw16, t12, t34, wo=0):
            op(DVE, "tensor_tensor", [qf], [qn], out=qn[:, 0:nh, :], in0=qf[:, 0:nh, :], in1=qf[:, 0:nh, :], op=ALU.mult)
            s12 = st12.get()
            op(DVE, "tensor_reduce", [qn], [s12], out=s12[:, 0:nh], in_=qn[:, 0:nh, :], axis=AX.X, op=ALU.add)
            r12 = st12.get()
            rsqrt_small(s12[:, 0:nh], [s12], r12, r12[:, 0:nh], nh, 1.0 / 64, st12.get())
            op(DVE, "tensor_tensor", [qf, r12], [qn], out=qn[:, 0:nh, :], in0=qf[:, 0:nh, :], in1=r12[:, 0:nh].unsqueeze(2).broadcast_to([128, nh, 64]), op=ALU.mult)
            op(DVE, "tensor_tensor", [qn, wt], [qb], out=qb[:, 0:nh, :], in0=qn[:, 0:nh, :], in1=wt[:, wo:wo + nh, :], op=ALU.mult)
            op(DVE, "tensor_tensor", [qn, wt], [qw16], out=qw16[:, 0:nh, :], in0=qn[:, 0:nh, 0:16], in1=wt[:, wo:wo + nh, 0:16], op=ALU.mult)
            op(DVE, "tensor_tensor", [qw16] + csbufs, [t12], out=t12[:, 0:nh, :], in0=qw16[:, 0:nh, :], in1=cos_ap.broadcast_to([128, nh, 16]), op=ALU.mult)
            op(DVE, "tensor_tensor", [qw16] + csbufs, [t34], out=t34[:, 0:nh, :], in0=qw16[:, 0:nh, :], in1=sin_ap.broadcast_to([128, nh, 16]), op=ALU.mult)
            op(DVE, "tensor_tensor", [t12, t34], [qb], out=qb[:, 0:nh, 0:8], in0=t12[:, 0:nh, 0:8], in1=t34[:, 0:nh, 8:16], op=ALU.subtract)
            op(DVE, "tensor_tensor", [t12, t34], [qb], out=qb[:, 0:nh, 8:16], in0=t12[:, 0:nh, 8:16], in1=t34[:, 0:nh, 0:8], op=ALU.add)

        def norm_transpose_block(tb, src_d, wbc, W, hT, srcbufs=None, js=(0, 1, 2, 3)):
            xts = []
            for j in js:
                tt = tb * 4 + j
                xt = W["x"].get()
                xts.append(xt)
                dma(SP, xt, [srcbufs[tt]] if srcbufs else [], [xt], out=xt[:], in_=src_d[tt * 128:(tt + 1) * 128, :])
                st = W["stat"].get()
                hb = W["hb"].get()
                op(ACT, "activation", [xt], [hb, st], out=hb[:], in_=xt[:], func=AF.Square, accum_out=st[:, 0:1])
                rsqrt_small(st[:, 0:1], [st], st, st[:, 2:3], 1, 1.0 / D, W["stmp"].get())
                op(DVE, "scalar_tensor_tensor", [xt, st, wbc], [hb], out=hb[:], in0=xt[:], scalar=st[:, 2:3], in1=wbc[:], op0=ALU.mult, op1=ALU.mult)
                pT = W["psT"].get()
                for k in range(8):
                    op(PE, "transpose", [hb, identb], [pT], out=pT[:, k * 128:(k + 1) * 128], in_=hb[:, k * 128:(k + 1) * 128], identity=identb[:])
                op(ACT, "activation", [pT], [hT], out=hT[:, :, j * 128:(j + 1) * 128], in_=pT[:].rearrange("p (k t) -> p k t", k=8), func=AF.Copy)
            return xts

        def nt_work(es_, nx=2, nh=2):
            return {
                "x": Ring([fw.sb("xt", [128, D], F32, es_) for _ in range(nx)]),
                "stat": Ring([fw.sb("stat", [128, 4], F32, es_) for _ in range(4)]),
                "stmp": Ring([fw.sb("stmp", [128, 12], F32, es_) for _ in range(4)]),
                "hb": Ring([fw.sb("hb", [128, D], BF16, es_) for _ in range(2)]),
                "psT": Ring([fw.ps("psT", [128, D], BF16, es_) for _ in range(2)]),
                "hT": Ring([fw.sb("hT", [128, 8, 512], BF16, es_) for _ in range(nh)]),
            }

        yrnnT = fw.sb("yrnnT", [128, 4, T], BF16, att)
        kcT = fw.sb("kcT", [128, 256], BF16, att)
        VcA = fw.sb("VcA", [128, 2, 2, 128], BF16, att)
        kcTok = fw.sb("kcTok", [128, T], BF16, tokst)
        vcTok = fw.sb("vcTok", [128, T], BF16, tokst)
        ydr = [fw.dram(None, "ydr%d" % i) for i in range(NT)]

        with ExitStack() as p1:
            W = nt_work(p1)
            WinA = fw.sb("WinA", [128, 8, 1280], BF16, p1)
            for k in range(8):
                dma(POOL, WinA, [], [WinA], out=WinA[:, k, 0:1024], in_=win_d[k * 128:(k + 1) * 128, 0:1024])
                dma(POOL, WinA, [], [WinA], out=WinA[:, k, 1024:1280], in_=win_d[k * 128:(k + 1) * 128, 1536:1792])
            psM = Ring([fw.ps("psM", [128, 512], F32, p1) for _ in range(4)])
            psS = fw.ps("psS", [128, 512], F32, p1)
            xpad = [fw.sb("xpad", [128, 515], F32, p1) for _ in range(4)]
            gel = Ring([fw.sb("gel", [128, 512], F32, p1) for _ in range(2)])
            xc = Ring([fw.sb("xc", [128, 512], F32, p1) for _ in range(2)])
            xcb = Ring([fw.sb("xcb", [128, 512], BF16, p1) for _ in range(2)])
            rr = Ring([fw.sb("rr", [128, 512], F32, p1) for _ in range(2)])
            ii = Ring([fw.sb("ii", [128, 512], F32, p1) for _ in range(2)])
            aa = Ring([fw.sb("aa", [128, 512], F32, p1) for _ in range(2)])
            hring = Ring([fw.sb("hh", [128, 512], F32, p1) for _ in range(2)])
            hlast = fw.sb("hlast", [128, 4], F32, p1)
            y4 = fw.sb("y4", [128, 4, 512], F32, p1)
            ysq = Ring([fw.sb("ysq", [128, 512], F32, p1) for _ in range(2)])
            ssrow = fw.sb("ssrow", [1, 512], F32, p1)
            rrow = fw.sb("rrow", [1, 512], F32, p1)
            c1 = fw.sb("c1", [128, 4, 2], F32, p1)
            c1t = fw.sb("c1t", [128, 4], F32, p1)

            for c in range(4):
                op(DVE, "memset", [], [xpad[c]], xpad[c][:], 0.0)
            op(DVE, "memset", [], [hlast], hlast[:], 0.0)
            op(ACT, "activation", [rnnp], [c1t], out=c1t[:], in_=rnnp[:, :, 3], func=AF.Exp, scale=-1.0)
            op(ACT, "activation", [c1t], [c1t], out=c1t[:], in_=c1t[:], func=AF.Ln, bias=1.0)
            op(DVE, "tensor_scalar", [c1t], [c1], out=c1[:, :, 0], in0=c1t[:], scalar1=-8.0, scalar2=None, op0=ALU.mult)
            op(DVE, "tensor_scalar", [c1t], [c1], out=c1[:, :, 1], in0=c1t[:], scalar1=-16.0, scalar2=None, op0=ALU.mult)

            hT_next = W["hT"].get()
            norm_transpose_block(0, x_d, anw, W, hT_next)
            for tb in range(8):
                hT = hT_next
                if tb + 1 < 8:
                    hT_next = W["hT"].get()
                for dst, c0 in ((kcTok, 1024), (vcTok, 1152)):
                    pF = psM.get()
                    for k in range(8):
                        op(PE, "matmul", [hT, WinA], [pF], pF[:], lhsT=WinA[:, k, c0:c0 + 128], rhs=hT[:, k, :], start=(k == 0), stop=(k == 7))
                    op(ACT, "activation", [pF], [dst], out=dst[:, tb * 512:(tb + 1) * 512], in_=pF[:], func=AF.Copy)
                for c in range(4):
                    pX = psM.get()
                    for k in range(8):
                        op(PE, "matmul", [hT, WinA], [pX], pX[:], lhsT=WinA[:, k, c * 128:(c + 1) * 128], rhs=hT[:, k, :], start=(k == 0), stop=(k == 7))
                    xp = xpad[c]
                    if tb > 0:
                        op(DVE, "tensor_copy", [xp], [xp], out=xp[:, 0:3], in_=xp[:, 512:515])
                    op(ACT, "activation", [pX], [xp], out=xp[:, 3:515], in_=pX[:], func=AF.Copy)
                    pG = psM.get()
                    for k in range(8):
                        op(PE, "matmul", [hT, WinA], [pG], pG[:], lhsT=WinA[:, k, 512 + c * 128:512 + (c + 1) * 128], rhs=hT[:, k, :], start=(k == 0), stop=(k == 7))
                    g_ = gel.get()
                    op(ACT, "activation", [pG], [g_], out=g_[:], in_=pG[:], func=AF.Gelu_apprx_tanh)
                    xc_ = xc.get()
                    op(DVE, "tensor_scalar", [xp, convw, rnnp], [xc_], out=xc_[:], in0=xp[:, 0:512], scalar1=convw[:, c, 0:1], scalar2=rnnp[:, c, 0:1], op0=ALU.mult, op1=ALU.add)
                    for k in range(1, 4):
                        op(DVE, "scalar_tensor_tensor", [xp, convw, xc_], [xc_], out=xc_[:], in0=xp[:, k:k + 512], scalar=convw[:, c, k:k + 1], in1=xc_[:], op0=ALU.mult, op1=ALU.add)
                    xcb_ = xcb.get()
                    op(POOL, "tensor_copy", [xc_], [xcb_], out=xcb_[:], in_=xc_[:])
                    pa = psM.get()
                    op(PE, "matmul", [gaw, xcb_], [pa], pa[:], lhsT=gaw[:, c, :], rhs=xcb_[:], start=True, stop=True)
                    px = psM.get()
                    op(PE, "matmul", [gxw, xcb_], [px], px[:], lhsT=gxw[:, c, :], rhs=xcb_[:], start=True, stop=True)
                    r_, i_, a_ = rr.get(), ii.get(), aa.get()
                    op(ACT, "activation", [pa, rnnp], [r_], out=r_[:], in_=pa[:], func=AF.Sigmoid, bias=rnnp[:, c, 1:2])
                    op(ACT, "activation", [px, rnnp], [i_], out=i_[:], in_=px[:], func=AF.Sigmoid, bias=rnnp[:, c, 2:3])
                    op(ACT, "activation", [r_, c1], [a_], out=a_[:], in_=r_[:], func=AF.Exp, scale=c1[:, c, 0:1])
                    op(ACT, "activation", [r_, c1], [r_], out=r_[:], in_=r_[:], func=AF.Exp, scale=c1[:, c, 1:2])
                    op(ACT, "activation", [r_], [r_], out=r_[:], in_=r_[:], func=AF.Sqrt, scale=-1.0, bias=1.0)
                    op(DVE, "tensor_tensor", [i_, xc_], [i_], out=i_[:], in0=i_[:], in1=xc_[:], op=ALU.mult)
                    op(DVE, "tensor_tensor", [i_, r_], [i_], out=i_[:], in0=i_[:], in1=r_[:], op=ALU.mult)
                    h_ = hring.get()
                    op(DVE, "tensor_tensor_scan", [a_, i_, hlast], [h_], out=h_[:], data0=a_[:], data1=i_[:], initial=hlast[:, c:c + 1], op0=ALU.mult, op1=ALU.add)
                    op(DVE, "tensor_copy", [h_], [hlast], out=hlast[:, c:c + 1], in_=h_[:, 511:512])
                    op(DVE, "tensor_tensor", [g_, h_], [y4], out=y4[:, c, :], in0=g_[:], in1=h_[:], op=ALU.mult)
                    ys_ = ysq.get()
                    op(POOL, "tensor_tensor", [y4], [ys_], out=ys_[:], in0=y4[:, c, :], in1=y4[:, c, :], op=ALU.mult)
                    op(PE, "matmul", [ones_col, ys_], [psS], psS[0:1, :], lhsT=ones_col[:, 0:1], rhs=ys_[:], start=(c == 0), stop=(c == 3))
                    if tb + 1 < 8:
                        norm_transpose_block(tb + 1, x_d, anw, W, hT_next, js=(c,))
                op(DVE, "tensor_scalar", [psS], [ssrow], out=ssrow[:], in0=psS[0:1, :], scalar1=1.0 / 512, scalar2=EPS, op0=ALU.mult, op1=ALU.add)
                op(ACT, "activation", [ssrow], [ssrow], out=ssrow[:], in_=ssrow[:], func=AF.Sqrt)
                op(DVE, "reciprocal", [ssrow], [rrow], out=rrow[:], in_=ssrow[:])
                pb_ = psM.get()
                op(PE, "matmul", [ones_row, rrow], [pb_], pb_[:], lhsT=ones_row[:], rhs=rrow[:], start=True, stop=True)
                for c in range(4):
                    op(DVE, "scalar_tensor_tensor", [y4, rnnp, pb_], [yrnnT], out=yrnnT[:, c, tb * 512:(tb + 1) * 512], in0=y4[:, c, :], scalar=rnnp[:, c, 4:5], in1=pb_[:], op0=ALU.mult, op1=ALU.mult)
            fw.barrier()

        with ExitStack() as p2:
            cw1 = {}
            cw2 = {}
            for kd in ("k", "v"):
                cw1[kd] = fw.sb("cw1" + kd, [128, 32, 256], BF16, p2)
                for l0 in range(0, 32, 8):
                    dma(POOL, cw1[kd], [], [cw1[kd]], out=cw1[kd][:, l0:l0 + 8, :].rearrange("p l n -> p (l n)"), in_=cw1_d[kd][:, l0 * 256:(l0 + 8) * 256])
                cw2[kd] = fw.sb("cw2" + kd, [128, 2, 64], BF16, p2)
                dma(POOL, cw2[kd], [], [cw2[kd]], out=cw2[kd][:].rearrange("p c d -> p (c d)"), in_=cw2_d[kd][:, :])
            cposT = fw.sb("cposT", [128, 32], BF16, p2)
            dma(POOL, cposT, [], [cposT], out=cposT[:], in_=cposT_d[:, :])
            covf = fw.sb("covf", [128, 2, 64], F32, p2)
            dma(SP, covf, [], [covf], out=covf[:].rearrange("p c j -> p (c j)"), in_=cover_d[:, :])
            for g in range(2):
                op(DVE, "tensor_copy", [covf], [VcA], out=VcA[:, :, g, 64:128], in_=covf[:])
            psM = Ring([fw.ps("psM2", [128, 512], F32, p2) for _ in range(4)])
            psQ = fw.ps("psQ2", [128, 2, 128], BF16, p2)
            hidT = Ring([fw.sb("hidT", [128, 2, 256], BF16, p2) for _ in range(2)])
            cbias = fw.sb("cbias", [128, 4], F32, p2)
            kcf = [fw.sb("kcf", [128, 2, 64], F32, p2) for _ in range(2)]
            kcb = [fw.sb("kcb", [128, 2, 64], BF16, p2) for _ in range(2)]
            qn2 = fw.sb("qn2", [128, 2, 64], F32, p2)
            st2 = Ring([fw.sb("st2", [128, 12], F32, p2) for _ in range(6)])
            qw2 = fw.sb("qw2", [128, 2, 16], F32, p2)
            t12b = fw.sb("t12b", [128, 2, 16], F32, p2)
            t34b = fw.sb("t34b", [128, 2, 16], F32, p2)
            for ct in range(2):
                op(DVE, "memset", [], [kcf[ct]], kcf[ct][:], 0.0)
            op(DVE, "memset", [], [kcT], kcT[:], 0.0)
            pbias = psM.get()
            for ki, kd in enumerate(("k", "v")):
                for n_ in range(2):
                    col = ki * 2 + n_
                    for l in range(32):
                        op(PE, "matmul", [cw1[kd], cposT], [pbias], pbias[:, col:col + 1], lhsT=cw1[kd][0:64, l, n_ * 128:(n_ + 1) * 128], rhs=cposT[0:64, l:l + 1], start=(l == 0), stop=(l == 31))
            op(DVE, "tensor_copy", [pbias], [cbias], out=cbias[:], in_=pbias[:, 0:4])
            for ki, (kd, tok) in enumerate((("k", kcTok), ("v", vcTok))):
                for g in range(2):
                    hid = hidT.get()
                    for n_ in range(2):
                        ph = psM.get()
                        for l in range(32):
                            op(PE, "matmul", [cw1[kd], tok], [ph], ph[:, 0:255], lhsT=cw1[kd][64 * g:64 * g + 64, l, n_ * 128:(n_ + 1) * 128],
                               rhs=tok[64 * g:64 * g + 64, l:l + 16 * 254 + 1:16], start=(l == 0), stop=(l == 31))
                        op(ACT, "activation", [ph, cbias], [hid], out=hid[:, n_, 0:255], in_=ph[:, 0:255], func=AF.Gelu_apprx_tanh, bias=cbias[:, ki * 2 + n_:ki * 2 + n_ + 1])
                    for ct in range(2):
                        ncs = 128 if ct == 0 else 127
                        po = psM.get()
                        for n_ in range(2):
                            op(PE, "matmul", [hid, cw2[kd]], [po], po[0:ncs, 0:64], lhsT=hid[:, n_, ct * 128:ct * 128 + ncs], rhs=cw2[kd][:, n_, :], start=(n_ == 0), stop=(n_ == 1))
                        if kd == "k":
                            op(ACT, "activation", [po], [kcf[ct]], out=kcf[ct][0:ncs, g, :], in_=po[0:ncs, 0:64], func=AF.Copy)
                        else:
                            op(ACT, "activation", [po], [VcA], out=VcA[0:ncs, ct, g, 0:64], in_=po[0:ncs, 0:64], func=AF.Copy)
            for ct in range(2):
                norm_rope(kcf[ct], kcb[ct], 2, qkw, coscT[:, ct:ct + 1, :], sincT[:, ct:ct + 1, :], [coscT, sincT], qn2, st2, qw2, t12b, t34b, wo=8)
                op(PE, "transpose", [kcb[ct], identb], [psQ], out=psQ[:, ct, :], in_=kcb[ct][:].rearrange("p h d -> p (h d)"), identity=identb[:])
            op(ACT, "activation", [psQ], [kcT], out=kcT[:, 0:255], in_=psQ[:].rearrange("p c t -> p (c t)")[:, 0:255], func=AF.Copy)
            fw.barrier()
        tokst.close()
        if stop == 2:
            att.close()
            return nc

        qT = fw.sb("qT", [128, 4, T], BF16, att)
        KA = [fw.sb("KA%d" % g_, [128, T], BF16, att) for g_ in range(2)]
        dma(SP, KA[0], [], [KA[0]], out=KA[0][64:128, :], in_=efull_d[:, :])
        dma(SP, KA[1], [], [KA[1]], out=KA[1][0:64, :], in_=efull_d[:, :])
        kwT = fw.sb("kwT", [128, T], BF16, att)
        Vs = fw.sb("Vs", [128, NT, 2, 65], BF16, att)
        Vw = fw.sb("Vw", [128, NT, 2, 65], BF16, att)
        gates = fw.sb("gates", [128, NT, 24], F32, att)
        op(POOL, "memset", [], [Vs], Vs[:, :, :, 64:65], 1.0)
        op(POOL, "memset", [], [Vw], Vw[:, :, :, 64:65], 1.0)
        with ExitStack() as p1:
            W = nt_work(p1)
            WinB = fw.sb("WinB", [128, 8, 1048], BF16, p1)
            for k in range(8):
                dma(POOL, WinB, [], [WinB], out=WinB[:, k, 0:512], in_=win_d[k * 128:(k + 1) * 128, 1024:1536])
                dma(POOL, WinB, [], [WinB], out=WinB[:, k, 512:1048], in_=win_d[k * 128:(k + 1) * 128, 1792:2328])
            psM = Ring([fw.ps("psM", [128, 512], F32, p1) for _ in range(4)])
            psQ = Ring([fw.ps("psQ", [128, 6, 128], BF16, p1) for _ in range(2)])
            qkf = Ring([fw.sb("qkf", [128, 12, 64], F32, p1) for _ in range(2)])
            qn = fw.sb("qn", [128, 12, 64], F32, p1)
            qkb = Ring([fw.sb("qkb", [128, 12, 64], BF16, p1) for _ in range(2)])
            st12 = Ring([fw.sb("st12", [128, 12], F32, p1) for _ in range(4)])
            qw16 = fw.sb("qw16", [128, 12, 16], F32, p1)
            t12 = fw.sb("t12", [128, 12, 16], F32, p1)
            t34 = fw.sb("t34", [128, 12, 16], F32, p1)
            hT_next = W["hT"].get()
            norm_transpose_block(0, x_d, anw, W, hT_next)
            ptail = []
            for tb in range(8):
                hT = hT_next
                if tb + 1 < 8:
                    hT_next = W["hT"].get()
                for j in range(4):
                    tt = tb * 4 + j
                    qf = qkf.get()
                    pA = psM.get()
                    for k in range(8):
                        op(PE, "matmul", [hT, WinB], [pA], pA[:, 0:512], lhsT=hT[:, k, j * 128:(j + 1) * 128], rhs=WinB[:, k, 0:512], start=(k == 0), stop=(k == 7))
                    op(ACT, "activation", [pA], [qf], out=qf[:, 0:8, :].rearrange("p (i g) d -> p i g d", g=2),
                       in_=pA[:, 0:512].rearrange("p (g i d) -> p i g d", g=2, i=4), func=AF.Copy)
                    pB = psM.get()
                    for k in range(8):
                        op(PE, "matmul", [hT, WinB], [pB], pB[:, 0:256], lhsT=hT[:, k, j * 128:(j + 1) * 128], rhs=WinB[:, k, 512:768], start=(k == 0), stop=(k == 7))
                    op(ACT, "activation", [pB], [qf], out=qf[:, 8:10, :].rearrange("p h d -> p (h d)"), in_=pB[:, 0:128], func=AF.Copy)
                    op(ACT, "activation", [pB], [Vs], out=Vs[:, tt, :, 0:64], in_=pB[:, 128:256].rearrange("p (g d) -> p g d", g=2), func=AF.Copy)
                    pC = psM.get()
                    for k in range(8):
                        op(PE, "matmul", [hT, WinB], [pC], pC[:, 0:280], lhsT=hT[:, k, j * 128:(j + 1) * 128], rhs=WinB[:, k, 768:1048], start=(k == 0), stop=(k == 7))
                    op(ACT, "activation", [pC], [qf], out=qf[:, 10:12, :].rearrange("p h d -> p (h d)"), in_=pC[:, 0:128], func=AF.Copy)
                    op(ACT, "activation", [pC], [Vw], out=Vw[:, tt, :, 0:64], in_=pC[:, 128:256].rearrange("p (g d) -> p g d", g=2), func=AF.Copy)
                    op(ACT, "activation", [pC], [gates], out=gates[:, tt, :], in_=pC[:, 256:280], func=AF.Sigmoid)
                    qb = qkb.get()
                    norm_rope(qf, qb, 12, qkw, cosT[:, tt:tt + 1, :], sinT[:, tt:tt + 1, :], [cosT, sinT], qn, st12, qw16, t12, t34)

                    def tail(tt=tt, qb=qb):
                        pQ = psQ.get()
                        for i in range(6):
                            op(PE, "transpose", [qb, identb], [pQ], out=pQ[:, i, :], in_=qb[:, 2 * i:2 * i + 2, :].rearrange("p h d -> p (h d)"), identity=identb[:])
                        op(ACT, "activation", [pQ], [qT], out=qT[:, :, tt * 128:(tt + 1) * 128], in_=pQ[:, 0:4, :], func=AF.Copy)
                        op(ACT, "activation", [pQ], [KA[0]], out=KA[0][0:64, tt * 128:(tt + 1) * 128], in_=pQ[0:64, 4, :], func=AF.Copy)
                        op(ACT, "activation", [pQ], [KA[1]], out=KA[1][64:128, tt * 128:(tt + 1) * 128], in_=pQ[64:128, 4, :], func=AF.Copy)
                        op(ACT, "activation", [pQ], [kwT], out=kwT[:, tt * 128:(tt + 1) * 128], in_=pQ[:, 5, :], func=AF.Copy)

                    if tb + 1 < 8:
                        norm_transpose_block(tb + 1, x_d, anw, W, hT_next, js=(j,))
                    while ptail:
                        ptail.pop(0)()
                    ptail.append(tail)
            while ptail:
                ptail.pop(0)()
            fw.barrier()

        if stop == "B":
            att.close()
            return nc
        with ExitStack() as p3:
            tri4 = fw.sb("tri4", [128, 4, 128], BF16, p3)
            wlow4 = fw.sb("wlow4", [128, 4, 128], BF16, p3)
            cmask = fw.sb("cmask", [128, 33, 128], BF16, p3)
            vmbs = fw.sb("vmbs", [128, 2, 128], F32, p3)
            wout = fw.sb("wout", [128, 8, D], BF16, p3)
            aonw = fw.sb("aonw", [128, 512], F32, p3)
            dma(SP, tri4, [], [tri4], out=tri4[:].rearrange("p h t -> p (h t)"), in_=tri4_d[:, :])
            dma(SP, wlow4, [], [wlow4], out=wlow4[:].rearrange("p h t -> p (h t)"), in_=wlow4_d[:, :])
            dma(SP, cmask, [], [cmask], out=cmask[:].rearrange("p m t -> p (m t)"), in_=cmask_d[:, :])
            dma(SP, vmbs, [], [vmbs], out=vmbs[:].rearrange("p a t -> p (a t)"), in_=vmbs_d[:, :])
            dma(SP, aonw, [], [aonw], out=aonw[:], in_=aonw_d[:, :])
            for k in range(8):
                dma(POOL, wout, [], [wout], out=wout[:, k, :], in_=wout_d[k * 128:(k + 1) * 128, :])
            psS = Ring([fw.ps("psS3", [128, 4, 128], F32, p3) for _ in range(3)])
            psC = fw.ps("psC", [128, 4, 128], F32, p3)
            psSel = fw.ps("psSel", [128, 4, 128], F32, p3)
            psWin = fw.ps("psWin", [128, 4, 128], F32, p3)
            psX = Ring([fw.ps("psX", [128, 512], F32, p3) for _ in range(1)])
            psTb = fw.ps("psTb", [128, 1024], BF16, p3)
            Er = Ring([fw.sb("E", [128, 4, 128], BF16, p3) for _ in range(4)])
            xr3 = Ring([fw.sb("xt3", [128, D], F32, p3) for _ in range(2)])
            ocmp = [fw.sb("ocmp", [128, 4, 64], F32, p3) for _ in range(2)]
            RA = [[fw.sb("RA", [128, 4, 128], BF16, p3) for _ in range(2)] for _ in range(2)]
            negm2 = [fw.sb("negm2", [128, 128], BF16, p3) for _ in range(2)]
            sm = Ring([fw.sb("sm", [128, 16], F32, p3) for _ in range(12)])
            imp = fw.sb("imp", [128, 64], F32, p3)
            score = fw.sb("score", [128, 64], F32, p3)
            score2 = fw.sb("score2", [128, 64], F32, p3)
            negm = fw.sb("negm", [128, 64], BF16, p3)
            yatts = [fw.sb("yatt", [128, 8, 64], F32, p3) for _ in range(2)]
            ytmp = fw.sb("ytmp", [128, 4, 64], F32, p3)
            yb = fw.sb("yb", [128, 512], BF16, p3)
            yjunk = fw.sb("yjunk", [128, 512], BF16, p3)
            yattT = fw.sb("yattT", [128, 4, 128], BF16, p3)

            for g in range(2):
                op(POOL, "memset", [], [negm2[g]], negm2[g][:], 0.0)

            def qslice(g, qt):
                return qT[64 * g:64 * g + 64, :, qt * 128:(qt + 1) * 128]

            ytr = Ring([fw.sb("ytmp", [128, 4, 64], F32, p3) for _ in range(2)])
            xts3 = {}

            def gview(qt, g):
                return gates[:, qt, g * 12:(g + 1) * 12].rearrange("p (h b) -> p b h", b=3)

            deferred = []

            def topk(qt, g):
                gv = gview(qt, g)
                yatt = yatts[qt % 2]
                sums = sm.get()
                op(DVE, "tensor_reduce", [psC], [sums], out=sums[:, 0:4], in_=psC[:, :, 64:128], axis=AX.X, op=ALU.add)
                op(DVE, "tensor_scalar", [sums], [sums], out=sums[:, 4:8], in0=sums[:, 0:4], scalar1=1e-30, scalar2=None, op0=ALU.max)
                op(DVE, "reciprocal", [sums], [sums], out=sums[:, 8:12], in_=sums[:, 4:8])
                op(DVE, "tensor_tensor", [sums, gates], [sums], out=sums[:, 12:16], in0=sums[:, 8:12], in1=gv[:, 0, :], op=ALU.mult)
                op(DVE, "tensor_tensor", [psC, sums], [yatt], out=yatt[:, g * 4:(g + 1) * 4, :], in0=psC[:, :, 0:64], in1=sums[:, 12:16].unsqueeze(2).broadcast_to([128, 4, 64]), op=ALU.mult)
                op(DVE, "tensor_scalar", [psC, sums], [imp], out=imp[:], in0=psC[:, 0, 64:128], scalar1=sums[:, 8:9], scalar2=None, op0=ALU.mult)
                for h in range(1, 4):
                    op(DVE, "scalar_tensor_tensor", [psC, sums, imp], [imp], out=imp[:], in0=psC[:, h, 64:128], scalar=sums[:, 8 + h:9 + h], in1=imp[:], op0=ALU.mult, op1=ALU.add)
                lo = 64 - 2 * qt
                op(DVE, "tensor_tensor", [imp, vmbs], [score], out=score[:], in0=imp[:], in1=vmbs[:, 0, lo:lo + 64], op=ALU.mult)
                op(DVE, "tensor_tensor", [score, vmbs], [score], out=score[:], in0=score[:], in1=vmbs[:, 1, lo:lo + 64], op=ALU.add)
                op(DVE, "memset", [], [score], score[:, 0:1], 1.0e4)
                m8 = sm.get()
                op(DVE, "max", [score], [m8], out=m8[:, 0:8], in_=score[:])
                op(DVE, "match_replace", [m8, score], [score2], out=score2[:], in_to_replace=m8[:, 0:8], in_values=score[:], imm_value=-3.0e38)
                op(DVE, "max", [score2], [m8], out=m8[:, 8:16], in_=score2[:])
                c0 = 64 if g == 0 else 0
                nm = negm2[g]
                op(DVE, "tensor_scalar", [score, m8], [nm], out=nm[:, c0:c0 + 64], in0=score[:], scalar1=m8[:, 15:16], scalar2=NEG, op0=ALU.is_lt, op1=ALU.mult)
                _nw = 2 * min(qt + 1, 5)
                _allow = ((1 if qt < 16 else 2) + _nw - 2) if g == 0 else (_nw + qt + 1 - 2)
                _dt = max(1, min(int(_os.environ.get('KTOPDEF', 5)), _allow))
                if _dt == 0:
                    topk_b(qt, g)
                else:
                    deferred.append([_dt, lambda: topk_b(qt, g)])

            def topk_b(qt, g):
                nm = negm2[g]
                pNb = psTb[:, 512:1024]
                op(PE, "transpose", [nm, identb], [psTb], out=pNb[:, 0:128], in_=nm[:], identity=identb[:])
                ra = RA[g][qt % 2]
                r0 = 64 if g == 0 else 0
                q0 = 0 if g == 0 else 64
                op(DVE, "tensor_copy", [psTb], [ra], out=ra[r0:r0 + 64, 0, :], in_=pNb[r0:r0 + 64, 0:128])
                op(POOL, "tensor_copy", [ra], [ra], out=ra[r0:r0 + 64, 1:4, :], in_=ra[r0:r0 + 64, 0:1, :].broadcast_to([64, 3, 128]))
                op(POOL, "tensor_copy", [qT], [ra], out=ra[q0:q0 + 64, :, :], in_=qT[q0:q0 + 64, :, qt * 128:(qt + 1) * 128])

            def evac(qt, g, pO, b):
                gv = gview(qt, g)
                yatt = yatts[qt % 2]
                cf = sm.get()
                op(DVE, "tensor_scalar", [pO], [cf], out=cf[:, 0:4], in0=pO[:, :, 64], scalar1=1e-30, scalar2=None, op0=ALU.max)
                op(DVE, "reciprocal", [cf], [cf], out=cf[:, 4:8], in_=cf[:, 0:4])
                op(DVE, "tensor_tensor", [cf, gates], [cf], out=cf[:, 8:12], in0=cf[:, 4:8], in1=gv[:, b, :], op=ALU.mult)
                yt = ytr.get()
                yg = yatt[:, g * 4:(g + 1) * 4, :]
                op(DVE, "tensor_tensor", [pO, cf], [yt], out=yt[:], in0=pO[:, :, 0:64], in1=cf[:, 8:12].unsqueeze(2).broadcast_to([128, 4, 64]), op=ALU.mult)
                op(POOL, "tensor_tensor", [yatt, yt], [yatt], out=yg, in0=yg, in1=yt[:], op=ALU.add)

            def finish_a(qt):
                yatt = yatts[qt % 2]
                yf = yatt[:].rearrange("p h d -> p (h d)")
                st = sm.get()
                op(DVE, "tensor_tensor", [yatt], [yjunk], out=yjunk[:], in0=yf, in1=yf, op=ALU.mult)
                op(DVE, "tensor_reduce", [yjunk], [st], out=st[:, 0:1], in_=yjunk[:], axis=AX.X, op=ALU.add)
                rsqrt_small(st[:, 0:1], [st], st, st[:, 2:3], 1, 1.0 / 512, sm.get())
                op(DVE, "scalar_tensor_tensor", [yatt, st, aonw], [yb], out=yb[:], in0=yf, scalar=st[:, 2:3], in1=aonw[:], op0=ALU.mult, op1=ALU.mult)

            def finish_b(qt):
                pYb = psTb[:, 0:512]
                for c in range(4):
                    op(PE, "transpose", [yb, identb], [psTb], out=pYb[:, c * 128:(c + 1) * 128], in_=yb[:, c * 128:(c + 1) * 128], identity=identb[:])
                op(DVE, "tensor_copy", [psTb], [yattT], out=yattT[:].rearrange("p c t -> p (c t)"), in_=pYb[:, 0:512])

            def finish_c(qt):
                xt = xts3.pop(qt)
                for half in range(2):
                    pO_ = psX.get()
                    for c in range(4):
                        op(PE, "matmul", [yrnnT, wout], [pO_], pO_[:], lhsT=yrnnT[:, c, qt * 128:(qt + 1) * 128], rhs=wout[:, c, half * 512:(half + 1) * 512], start=(c == 0), stop=False)
                    for c in range(4):
                        op(PE, "matmul", [yattT, wout], [pO_], pO_[:], lhsT=yattT[:, c, :], rhs=wout[:, 4 + c, half * 512:(half + 1) * 512], start=False, stop=(c == 3))
                    op(DVE, "tensor_tensor", [xt, pO_], [xt], out=xt[:, half * 512:(half + 1) * 512], in0=xt[:, half * 512:(half + 1) * 512], in1=pO_[:], op=ALU.add)
                dma(SP, xt, [xt], [ydr[qt]], out=y_d[qt * 128:(qt + 1) * 128, :], in_=xt[:])

            def finish(qt):
                finish_a(qt)
                _d = int(_os.environ.get('KDEF', 3))
                if _d == 0:
                    finish_b(qt)
                    finish_c(qt)
                else:
                    deferred.append([_d, lambda: finish_b(qt)])
                    deferred.append([2 * _d, lambda: finish_c(qt)])

            def load_x(qt):
                xt = xr3.get()
                xts3[qt] = xt
                dma(SP, xt, [], [xt], out=xt[:], in_=x_d[qt * 128:(qt + 1) * 128, :])

            import os as _os
            _nqt = int(_os.environ.get('KQT', NT))
            jobs = []

            def cmp_jobs(qt):
                for g in range(2):
                    cts = [0] if qt < 16 else [0, 1]
                    for ct in cts:
                        ncs = 128 if ct == 0 else 127
                        mi = (qt if qt <= 16 else None) if ct == 0 else 17 + qt - 16
                        mm = [(kcT[64 * g:64 * g + 64, ct * 128:ct * 128 + ncs], qslice(g, qt), [kcT, qT], None)]
                        if mi is not None:
                            for h in range(4):
                                mm.append((identb[0:ncs, 0:ncs], cmask[0:ncs, mi, :], [identb, cmask], h))
                        jobs.append(dict(qt=qt, ncs=ncs, mm=mm, pO=psC, V=VcA[0:ncs, ct, g, :], Vb=VcA, ncol=128, first=(ct == cts[0]), before=None,
                                         after=((lambda qt=qt, g=g: topk(qt, g)) if ct == cts[-1] else None)))

            def main_jobs(qt):
                first = True
                for g in range(2):
                    kts = list(range(max(0, qt - 4), qt + 1))
                    for kt in kts:
                        mm = [(kwT[64 * g:64 * g + 64, kt * 128:(kt + 1) * 128], qslice(g, qt), [kwT, qT], None)]
                        if kt == qt:
                            mm.append((identb[:], tri4[:], [identb, tri4], None))
                        if kt == qt - 4:
                            mm.append((identb[:], wlow4[:], [identb, wlow4], None))
                        jobs.append(dict(qt=qt, ncs=128, mm=mm, pO=psWin, V=Vw[:, kt, g, :], Vb=Vw, ncol=65, first=(kt == kts[0]), before=(load_x if first else None),
                                         after=((lambda qt=qt, g=g: evac(qt, g, psWin, 2)) if kt == kts[-1] else None)))
                        first = False
                for g in range(2):
                    kts = list(range(qt + 1))
                    for kt in kts:
                        mm = [(KA[g][:, kt * 128:(kt + 1) * 128], RA[g][qt % 2][:], [KA[g], RA[g][qt % 2]], None)]
                        if kt == qt:
                            mm.append((identb[:], tri4[:], [identb, tri4], None))
                        if kt == kts[-1]:
                            if g == 0:
                                aft = (lambda qt=qt, g=g: evac(qt, g, psSel, 1))
                            else:
                                aft = (lambda qt=qt, g=g: (evac(qt, g, psSel, 1), finish(qt)))
                        else:
                            aft = None
                        jobs.append(dict(qt=qt, ncs=128, mm=mm, pO=psSel, V=Vs[:, kt, g, :], Vb=Vs, ncol=65, first=(kt == kts[0]), before=None, after=aft))

            if _os.environ.get('KAHEAD', '0') == '1':
                for it in range(_nqt + 1):
                    if it < _nqt:
                        cmp_jobs(it)
                    if it >= 1:
                        main_jobs(it - 1)
            else:
                for it in range(_nqt):
                    cmp_jobs(it)
                    main_jobs(it)

            def emit_score(job):
                if job["before"]:
                    job["before"](job["qt"])
                pS = psS.get()
                ncs = job["ncs"]
                n = len(job["mm"])
                for i, (l_, r_, bufs_, h) in enumerate(job["mm"]):
                    o_ = pS[0:ncs, :, :] if h is None else pS[0:ncs, h, :]
                    op(PE, "matmul", bufs_, [pS], o_, lhsT=l_, rhs=r_, start=(i == 0), stop=(i == n - 1), skip_group_check=True)
                return pS

            def emit_rest(job, pS):
                ncs = job["ncs"]
                E = Er.get()
                op(ACT, "activation", [pS], [E], out=E[0:ncs], in_=pS[0:ncs], func=AF.Exp, scale=0.125)
                pO = job["pO"]
                for h in range(4):
                    op(PE, "matmul", [E, job["Vb"]], [pO], pO[:, h, 0:job["ncol"]], lhsT=E[0:ncs, h, :], rhs=job["V"], start=(job["first"] and h == 0), stop=True, skip_group_check=True)
                if job["after"]:
                    job["after"]()
                for d_ in list(deferred):
                    d_[0] -= 1
                    if d_[0] <= 0:
                        deferred.remove(d_)
                        d_[1]()

            LOOK = int(_os.environ.get('KLOOK', 2))
            pend = []
            for job in jobs:
                pend.append((job, emit_score(job)))
                if len(pend) > LOOK:
                    emit_rest(*pend.pop(0))
            while pend:
                emit_rest(*pend.pop(0))
            for d_ in list(deferred):
                d_[1]()
            fw.barrier()
        att.close()
        if stop == 3:
            return nc

        with ExitStack() as p4:
            Wg = fw.sb("Wg", [128, 8, DFF], BF16, p4)
            Wu = fw.sb("Wu", [128, 8, DFF], BF16, p4)
            Wd = fw.sb("Wd", [128, NFF, D], BF16, p4)
            fnw = fw.sb("fnw", [128, D], F32, p4)
            dma(SP, fnw, [], [fnw], out=fnw[:], in_=fnw_d[:, :])
            for k in range(8):
                dma(POOL, Wg, [], [Wg], out=Wg[:, k, :], in_=wg_d[k * 128:(k + 1) * 128, :])
                dma(POOL, Wu, [], [Wu], out=Wu[:, k, :], in_=wu_d[k * 128:(k + 1) * 128, :])
            for j in range(NFF):
                dma(POOL, Wd, [], [Wd], out=Wd[:, j, :], in_=wd_d[j * 128:(j + 1) * 128, :])
            W = nt_work(p4, nx=5, nh=1)
            psG = Ring([fw.ps("psG", [128, 512], F32, p4) for _ in range(2)])
            psU = Ring([fw.ps("psU", [128, 512], F32, p4) for _ in range(2)])
            psD = Ring([fw.ps("psD", [128, 512], F32, p4) for _ in range(2)])
            sg = Ring([fw.sb("sg", [128, 512], F32, p4) for _ in range(2)])
            aT = fw.sb("aT", [128, NFF, 512], BF16, p4)
            for tb in range(8):
                hT = W["hT"].get()
                xts = norm_transpose_block(tb, y_d, fnw, W, hT, srcbufs=ydr)
                for j in range(NFF):
                    pg, pu = psG.get(), psU.get()
                    for k in range(8):
                        op(PE, "matmul", [Wg, hT], [pg], pg[:], lhsT=Wg[:, k, j * 128:(j + 1) * 128], rhs=hT[:, k, :], start=(k == 0), stop=(k == 7))
                    for k in range(8):
                        op(PE, "matmul", [Wu, hT], [pu], pu[:], lhsT=Wu[:, k, j * 128:(j + 1) * 128], rhs=hT[:, k, :], start=(k == 0), stop=(k == 7))
                    sg_ = sg.get()
                    op(ACT, "activation", [pg], [sg_], out=sg_[:], in_=pg[:], func=AF.Silu)
                    op(DVE, "tensor_tensor", [sg_, pu], [aT], out=aT[:, j, :], in0=sg_[:], in1=pu[:], op=ALU.mult)
                for i in range(4):
                    tt = tb * 4 + i
                    xt = xts[i]
                    for half in range(2):
                        pd = psD.get()
                        for j in range(NFF):
                            op(PE, "matmul", [aT, Wd], [pd], pd[:], lhsT=aT[:, j, i * 128:(i + 1) * 128], rhs=Wd[:, j, half * 512:(half + 1) * 512], start=(j == 0), stop=(j == NFF - 1))
                        op(DVE, "tensor_tensor", [xt, pd], [xt], out=xt[:, half * 512:(half + 1) * 512], in0=xt[:, half * 512:(half + 1) * 512], in1=pd[:], op=ALU.add)
                    dma(SP, xt, [xt], [ydr[tt]], out=y_d[tt * 128:(tt + 1) * 128, :], in_=xt[:])
            fw.barrier()
    return nc


def host_inputs(inp, b):
    f32 = np.float32
    m = {}
    m["x"] = np.ascontiguousarray(inp["x"][b], dtype=f32)
    pos = np.asarray(inp["positions"][b], dtype=np.int32)
    m["pos"] = np.ascontiguousarray(pos.reshape(NT, 128).T)
    pc = np.zeros(256, np.int32)
    pc[:255] = pos[np.arange(255) * 16 + 31]
    m["posc"] = np.ascontiguousarray(pc.reshape(2, 128).T)
    invf = (500000.0 ** (-np.arange(8, dtype=np.float32) * 2.0 / 16)).astype(f32)
    m["invf"] = np.ascontiguousarray(np.broadcast_to(invf, (128, 8)), dtype=f32)
    m["w_in"] = np.ascontiguousarray(inp["w_in"][0], dtype=f32)
    m["anw"] = np.ascontiguousarray(np.broadcast_to(inp["attn_norm_w"][0], (128, D)), dtype=f32)
    qkw = np.concatenate([np.tile(inp["q_norm_w"][0], 8), np.tile(inp["k_norm_w"][0], 4)])
    m["qkw"] = np.ascontiguousarray(np.broadcast_to(qkw, (128, 768)), dtype=f32)
    cw = inp["conv_w"][0].reshape(4, 4, 128)
    m["convw"] = np.ascontiguousarray(cw.transpose(2, 1, 0).reshape(128, 16), dtype=f32)
    rp = np.stack([inp["conv_b"][0], inp["gate_a_b"][0], inp["gate_x_b"][0], inp["lru_lambda"][0], inp["rnn_out_norm_w"][0]], 0)
    m["rnnp"] = np.ascontiguousarray(rp.reshape(5, 4, 128).transpose(2, 1, 0).reshape(128, 20), dtype=f32)
    for nm, key in (("gaw", "gate_a_w"), ("gxw", "gate_x_w")):
        w = inp[key][0]
        bd = np.zeros((128, 4, 128), f32)
        for c in range(4):
            for i in range(2):
                bd[64 * i:64 * i + 64, c, 64 * i:64 * i + 64] = w[2 * c + i]
        m[nm] = bd.reshape(128, 512)
    m["identf"] = np.eye(128, dtype=f32)
    for kd in ("k", "v"):
        w1 = inp["cmp_%s_w1" % kd][0].reshape(32, 64, 256).transpose(1, 0, 2).reshape(64, 32 * 256)
        m["cw1" + kd] = np.ascontiguousarray(np.concatenate([w1, w1], 0), dtype=f32)
        w2 = inp["cmp_%s_w2" % kd][0].reshape(2, 128, 64).transpose(1, 0, 2).reshape(128, 128)
        m["cw2" + kd] = np.ascontiguousarray(w2, dtype=f32)
    cp = inp["cmp_pos"][0].T
    m["cposT"] = np.ascontiguousarray(np.concatenate([cp, cp], 0), dtype=f32)
    m.update(CONSTS)
    m["w_out"] = np.ascontiguousarray(inp["w_out"][0], dtype=f32)
    m["aonw"] = np.ascontiguousarray(np.broadcast_to(inp["attn_out_norm_w"][0], (128, 512)), dtype=f32)
    m["fnw"] = np.ascontiguousarray(np.broadcast_to(inp["ffn_norm_w"][0], (128, D)), dtype=f32)
    m["w_gate"] = np.ascontiguousarray(inp["w_gate"][0], dtype=f32)
    m["w_up"] = np.ascontiguousarray(inp["w_up"][0], dtype=f32)
    m["w_down"] = np.ascontiguousarray(inp["w_down"][0], dtype=f32)
    return m


def _consts():
    bf = ml_dtypes.bfloat16
    f32 = np.float32
    c = {}
    kl = np.arange(128)[:, None]
    ql = np.arange(128)[None, :]
    tri = np.where(kl <= ql, 0.0, NEG).astype(f32)
    c["tri4"] = np.ascontiguousarray(np.tile(tri, (1, 4))).astype(bf)
    wl = np.where(kl > ql, 0.0, NEG).astype(f32)
    c["wlow4"] = np.ascontiguousarray(np.tile(wl, (1, 4))).astype(bf)
    cm = np.zeros((128, 33, 128), f32)
    for mi in range(33):
        ct, qt = (0, mi) if mi <= 16 else (1, 16 + mi - 17)
        dv = 128 * qt - 2048 * ct - 31
        cm[:, mi, :] = np.where(16 * kl - ql <= dv, 0.0, NEG)
    c["cmask"] = np.ascontiguousarray(cm.reshape(128, 33 * 128)).astype(bf)
    ef = (np.arange(T)[None, :] // 64 == np.arange(64)[:, None]).astype(f32)
    c["efull"] = np.ascontiguousarray(ef).astype(bf)
    cs = np.arange(255)[:, None] * 16
    bs = np.arange(64)[None, :] * 64
    cov = np.clip(np.minimum(cs + 32, bs + 64) - np.maximum(cs, bs), 0, None).astype(f32) / 32.0
    covp = np.zeros((256, 64), f32)
    covp[:255] = cov
    c["cover"] = np.ascontiguousarray(covp.reshape(2, 128, 64).transpose(1, 0, 2).reshape(128, 128))
    tl = np.arange(128)[:, None]
    jp = np.arange(128)[None, :] - 64
    cc = (tl >= 64).astype(np.int64)
    forced = (jp == cc) | (jp == cc - 1)
    invalid = jp > cc
    vm = np.where(forced | invalid, 0.0, 1.0).astype(f32)
    bsb = np.where(forced, 1.0e4, np.where(invalid, -1.0e30, 0.0)).astype(f32)
    c["vmbs"] = np.ascontiguousarray(np.concatenate([vm, bsb], 1))
    return c


CONSTS = _consts()


def kernel(**inputs):
    inp = {k: np.asarray(v) for k, v in inputs.items()}
    nc = build()
    in_maps = [host_inputs(inp, b) for b in range(8)]
    res = run_bass_kernel_spmd(nc, in_maps, core_ids=list(range(8)))
    return np.stack([r["y"] for r in res.results], 0).astype(np.float32)
```

```python
import numpy as np
import ml_dtypes
from contextlib import ExitStack
import concourse.bass as bass
import concourse.mybir as mybir
from concourse.bass_utils import run_bass_kernel_spmd

F32, BF16, I32 = mybir.dt.float32, mybir.dt.bfloat16, mybir.dt.int32
AF = mybir.ActivationFunctionType
ALU = mybir.AluOpType
AX = mybir.AxisListType

T = 4096
NT = 32
D = 1024
DIN = 2328
DFF = 2816
NFF = 22
EPS = 1e-6
NEG = -30000.0
TWO_PI = 6.283185307179586
PI = 3.141592653589793


class Buf:
    def __init__(self, fw, t, name):
        self.fw, self.t, self.name = fw, t, name
        self.w = None
        self.r = {}
        self.dsem = None
        self.dkey = None
        self.dcnt = 0

    def __getitem__(self, k):
        return self.t[k]


class Eng:
    def __init__(self, fw, name, eng, sem, key, selfwait):
        self.fw, self.name, self.eng, self.sem, self.key = fw, name, eng, sem, key
        self.selfwait = selfwait
        self.cnt = 0
        self.waited = {}

    def sync(self, reads, writes):
        waits = {}

        def need(ev, raw):
            if ev is None:
                return
            key, sem, val = ev
            if key == self.key and not self.selfwait:
                return
            if self.waited.get(key, 0) >= val:
                return
            if key not in waits or waits[key][1] < val:
                waits[key] = (sem, val)

        for b in reads:
            need(b.w, True)
        for b in writes:
            need(b.w, False)
            for ev in b.r.values():
                need(ev, False)
        for key, (sem, val) in waits.items():
            self.eng.wait_ge(sem, val)
            self.waited[key] = val

    def wait_ev(self, ev):
        key, sem, val = ev
        if self.waited.get(key, 0) >= val:
            return
        self.eng.wait_ge(sem, val)
        self.waited[key] = val


class FW:
    def __init__(self, nc, es):
        self.nc, self.es = nc, es
        self.nkey = 0
        self.engs = {}
        for name, eng, sw in (("PE", nc.tensor, False), ("ACT", nc.scalar, True),
                              ("DVE", nc.vector, True), ("POOL", nc.gpsimd, True),
                              ("SP", nc.sync, False)):
            sem = es.enter_context(nc.semaphore("sem_" + name))
            self.engs[name] = Eng(self, name, eng, sem, self.newkey(), sw)
        self.PE, self.ACT, self.DVE, self.POOL, self.SP = (self.engs[n] for n in ("PE", "ACT", "DVE", "POOL", "SP"))
        self.carriers = []
        self.nbuf = 0

    def newkey(self):
        self.nkey += 1
        return self.nkey

    def sb(self, name, shape, dt, es=None):
        es = es or self.es
        self.nbuf += 1
        t = es.enter_context(self.nc.sbuf_tensor("%s_%d" % (name, self.nbuf), list(shape), dt))
        return Buf(self, t, name)

    def ps(self, name, shape, dt, es=None):
        es = es or self.es
        self.nbuf += 1
        t = es.enter_context(self.nc.psum_tensor("%s_%d" % (name, self.nbuf), list(shape), dt))
        return Buf(self, t, name)

    def dram(self, ap, name):
        return Buf(self, ap, name)

    def op(self, E, meth, reads, writes, *a, **kw):
        E.sync(reads, writes)
        ins = getattr(E.eng, meth)(*a, **kw)
        E.cnt += 1
        ins.then_inc(E.sem, 1)
        ev = (E.key, E.sem, E.cnt)
        for b in reads:
            b.r[E.key] = ev
        for b in writes:
            b.w = ev
            b.r = {}
        return ins

    def dma(self, Q, carrier, reads, writes, out, in_, **kw):
        Q.sync(reads, writes)
        if carrier.dsem is None:
            carrier.dsem = self.es.enter_context(self.nc.semaphore("dsem_%s_%d" % (carrier.name, len(self.carriers))))
            carrier.dkey = self.newkey()
            self.carriers.append(carrier)
        ins = Q.eng.dma_start(out=out, in_=in_, **kw)
        carrier.dcnt += 16
        ins.then_inc(carrier.dsem, 16)
        ev = (carrier.dkey, carrier.dsem, carrier.dcnt)
        for b in reads:
            b.r[carrier.dkey] = ev
        for b in writes:
            b.w = ev
            b.r = {}
        return ins

    def barrier(self):
        SP = self.SP
        for E in self.engs.values():
            if E is not SP and E.cnt > 0:
                SP.wait_ev((E.key, E.sem, E.cnt))
        for c in self.carriers:
            if c.dcnt > 0:
                SP.wait_ev((c.dkey, c.dsem, c.dcnt))
        ins = SP.eng.nop()
        SP.cnt += 1
        ins.then_inc(SP.sem, 1)
        ev = (SP.key, SP.sem, SP.cnt)
        for E in self.engs.values():
            if E is not SP:
                E.wait_ev(ev)


class Ring:
    def __init__(self, bufs):
        self.bufs, self.i = bufs, 0

    def get(self):
        b = self.bufs[self.i % len(self.bufs)]
        self.i += 1
        return b


def build(dbg=None, stop=None):
    nc = bass.Bass("TRN2", target_bir_lowering=False)

    def din(name, shape, dt=F32):
        return nc.dram_tensor(name, list(shape), dt, kind="ExternalInput").ap()

    x_d = din("x", [T, D])
    pos_d = din("pos", [128, NT], I32)
    posc_d = din("posc", [128, 2], I32)
    invf_d = din("invf", [128, 8])
    win_d = din("w_in", [D, DIN])
    anw_d = din("anw", [128, D])
    qkw_d = din("qkw", [128, 12 * 64])
    convw_d = din("convw", [128, 16])
    rnnp_d = din("rnnp", [128, 20])
    gaw_d = din("gaw", [128, 512])
    gxw_d = din("gxw", [128, 512])
    identf_d = din("identf", [128, 128])
    cw1_d = {"k": din("cw1k", [128, 32 * 256]), "v": din("cw1v", [128, 32 * 256])}
    cw2_d = {"k": din("cw2k", [128, 128]), "v": din("cw2v", [128, 128])}
    cposT_d = din("cposT", [128, 32])
    cover_d = din("cover", [128, 128])
    tri4_d = din("tri4", [128, 512], BF16)
    wlow4_d = din("wlow4", [128, 512], BF16)
    cmask_d = din("cmask", [128, 33 * 128], BF16)
    efull_d = din("efull", [64, T], BF16)
    vmbs_d = din("vmbs", [128, 256])
    wout_d = din("w_out", [D, D])
    aonw_d = din("aonw", [128, 512])
    fnw_d = din("fnw", [128, D])
    wg_d = din("w_gate", [D, DFF])
    wu_d = din("w_up", [D, DFF])
    wd_d = din("w_down", [DFF, D])
    y_d = nc.dram_tensor("y", [T, D], F32, kind="ExternalOutput").ap()
    dbg_d = {}
    if dbg:
        for nm, shp, dt in dbg:
            dbg_d[nm] = nc.dram_tensor("dbg_" + nm, list(shp), dt, kind="ExternalOutput").ap()

    with ExitStack() as es:
        fw = FW(nc, es)
        PE, ACT, DVE, POOL, SP = fw.PE, fw.ACT, fw.DVE, fw.POOL, fw.SP
        op, dma = fw.op, fw.dma
        att = ExitStack()
        tokst = ExitStack()

        identf = fw.sb("identf", [128, 128], F32)
        identb = fw.sb("identb", [128, 128], BF16)
        dma(SP, identf, [], [identf], out=identf[:], in_=identf_d[:, :])
        op(DVE, "tensor_copy", [identf], [identb], out=identb[:], in_=identf[:])
        ones_col = fw.sb("ones_col", [128, 1], F32)
        ones_row = fw.sb("ones_row", [1, 128], F32)
        neghalf = fw.sb("neghalf", [128, 512], F32)
        op(DVE, "memset", [], [ones_col], ones_col[:], 1.0)
        op(DVE, "memset", [], [ones_row], ones_row[:], 1.0)
        op(DVE, "memset", [], [neghalf], neghalf[:], -0.5)

        anw = fw.sb("anw", [128, D], F32, att)
        dma(SP, anw, [], [anw], out=anw[:], in_=anw_d[:, :])
        qkw = fw.sb("qkw", [128, 12, 64], F32, att)
        dma(SP, qkw, [], [qkw], out=qkw[:].rearrange("p h d -> p (h d)"), in_=qkw_d[:, :])
        convw = fw.sb("convw", [128, 4, 4], F32, att)
        dma(SP, convw, [], [convw], out=convw[:].rearrange("p c k -> p (c k)"), in_=convw_d[:, :])
        rnnp = fw.sb("rnnp", [128, 4, 5], F32, att)
        dma(SP, rnnp, [], [rnnp], out=rnnp[:].rearrange("p c k -> p (c k)"), in_=rnnp_d[:, :])
        gaw = fw.sb("gaw", [128, 4, 128], BF16, att)
        gxw = fw.sb("gxw", [128, 4, 128], BF16, att)
        dma(POOL, gaw, [], [gaw], out=gaw[:].rearrange("p c k -> p (c k)"), in_=gaw_d[:, :])
        dma(POOL, gxw, [], [gxw], out=gxw[:].rearrange("p c k -> p (c k)"), in_=gxw_d[:, :])

        def rope_tables(pos_ap, n, name):
            cosT_ = fw.sb(name + "cos", [128, n, 16], F32, att)
            sinT_ = fw.sb(name + "sin", [128, n, 16], F32, att)
            with ExitStack() as tmp:
                posi = fw.sb(name + "posi", [128, n], I32, tmp)
                dma(SP, posi, [], [posi], out=posi[:], in_=pos_ap)
                invf = fw.sb(name + "invf", [128, 8], F32, tmp)
                dma(SP, invf, [], [invf], out=invf[:], in_=invf_d[:, :])
                posf = fw.sb(name + "posf", [128, n], F32, tmp)
                op(DVE, "tensor_copy", [posi], [posf], out=posf[:], in_=posi[:])
                ang = fw.sb(name + "ang", [128, n, 8], F32, tmp)
                op(DVE, "tensor_tensor", [posf, invf], [ang], out=ang[:],
                   in0=posf[:].unsqueeze(2).broadcast_to([128, n, 8]),
                   in1=invf[:].unsqueeze(1).broadcast_to([128, n, 8]), op=ALU.mult)
                a2 = fw.sb(name + "a2", [128, n * 8], F32, tmp)
                ki = fw.sb(name + "ki", [128, n * 8], I32, tmp)
                kf = fw.sb(name + "kf", [128, n * 8], F32, tmp)
                m = fw.sb(name + "m", [128, n * 8], F32, tmp)
                for tab, shift in ((cosT_, PI / 2), (sinT_, 0.0)):
                    angf = ang[:].rearrange("p n f -> p (n f)")
                    op(DVE, "tensor_scalar", [ang], [a2], out=a2[:], in0=angf, scalar1=shift, scalar2=None, op0=ALU.add)
                    op(DVE, "tensor_scalar", [a2], [kf], out=kf[:], in0=a2[:], scalar1=1.0 / TWO_PI, scalar2=None, op0=ALU.mult)
                    op(DVE, "tensor_copy", [kf], [ki], out=ki[:], in_=kf[:])
                    op(DVE, "tensor_copy", [ki], [kf], out=kf[:], in_=ki[:])
                    op(DVE, "scalar_tensor_tensor", [kf, a2], [a2], out=a2[:], in0=kf[:], scalar=-TWO_PI, in1=a2[:], op0=ALU.mult, op1=ALU.add)
                    op(DVE, "tensor_scalar", [a2], [m], out=m[:], in0=a2[:], scalar1=PI, scalar2=None, op0=ALU.is_gt)
                    op(DVE, "scalar_tensor_tensor", [m, a2], [a2], out=a2[:], in0=m[:], scalar=-TWO_PI, in1=a2[:], op0=ALU.mult, op1=ALU.add)
                    op(DVE, "tensor_scalar", [a2], [m], out=m[:], in0=a2[:], scalar1=-PI, scalar2=None, op0=ALU.is_lt)
                    op(DVE, "scalar_tensor_tensor", [m, a2], [a2], out=a2[:], in0=m[:], scalar=TWO_PI, in1=a2[:], op0=ALU.mult, op1=ALU.add)
                    op(DVE, "tensor_scalar", [a2], [a2], out=a2[:], in0=a2[:], scalar1=PI, scalar2=-PI, op0=ALU.min, op1=ALU.max)
                    a3 = a2[:].rearrange("p (n f) -> p n f", f=8)
                    op(ACT, "activation", [a2], [tab], out=tab[:, :, 0:8], in_=a3, func=AF.Sin)
                    op(ACT, "activation", [a2], [tab], out=tab[:, :, 8:16], in_=a3, func=AF.Sin)
                fw.barrier()
            return cosT_, sinT_

        cosT, sinT = rope_tables(pos_d[:, :], NT, "rp")
        coscT, sincT = rope_tables(posc_d[:, :], 2, "rc")

        def rsqrt_small(src_ap, srcbufs, dst, dst_ap, n, scale, tmp):
            op(DVE, "tensor_scalar", srcbufs, [tmp], out=tmp[:, 0:n], in0=src_ap, scalar1=scale, scalar2=EPS, op0=ALU.mult, op1=ALU.add)
            op(POOL, "tensor_tensor", [tmp, neghalf], [dst], out=dst_ap, in0=tmp[:, 0:n], in1=neghalf[:, 0:n], op=ALU.pow)

        def norm_rope(qf, qb, nh, wt, cos_ap, sin_ap, csbufs, qn, st12, qw16, t12, t34, wo=0):
            op(DVE, "tensor_tensor", [qf], [qn], out=qn[:, 0:nh, :], in0=qf[:, 0:nh, :], in1=qf[:, 0:nh, :], op=ALU.mult)
            s12 = st12.get()
            op(DVE, "tensor_reduce", [qn], [s12], out=s12[:, 0:nh], in_=qn[:, 0:nh, :], axis=AX.X, op=ALU.add)
            r12 = st12.get()
            rsqrt_small(s12[:, 0:nh], [s12], r12, r12[:, 0:nh], nh, 1.0 / 64, st12.get())
            op(DVE, "tensor_tensor", [qf, r12], [qn], out=qn[:, 0:nh, :], in0=qf[:, 0:nh, :], in1=r12[:, 0:nh].unsqueeze(2).broadcast_to([128, nh, 64]), op=ALU.mult)
            op(DVE, "tensor_tensor", [qn, wt], [qb], out=qb[:, 0:nh, :], in0=qn[:, 0:nh, :], in1=wt[:, wo:wo + nh, :], op=ALU.mult)
            op(DVE, "tensor_tensor", [qn, wt], [qw16], out=qw16[:, 0:nh, :], in0=qn[:, 0:nh, 0:16], in1=wt[:, wo:wo + nh, 0:16], op=ALU.mult)
            op(DVE, "tensor_tensor", [qw16] + csbufs, [t12], out=t12[:, 0:nh, :], in0=qw16[:, 0:nh, :], in1=cos_ap.broadcast_to([128, nh, 16]), op=ALU.mult)
            op(DVE, "tensor_tensor", [qw16] + csbufs, [t34], out=t34[:, 0:nh, :], in0=qw16[:, 0:nh, :], in1=sin_ap.broadcast_to([128, nh, 16]), op=ALU.mult)
            op(DVE, "tensor_tensor", [t12, t34], [qb], out=qb[:, 0:nh, 0:8], in0=t12[:, 0:nh, 0:8], in1=t34[:, 0:nh, 8:16], op=ALU.subtract)
            op(DVE, "tensor_tensor", [t12, t34], [qb], out=qb[:, 0:nh, 8:16], in0=t12[:, 0:nh, 8:16], in1=t34[:, 0:nh, 0:8], op=ALU.add)

        def norm_transpose_block(tb, src_d, wbc, W, hT, srcbufs=None, js=(0, 1, 2, 3)):
            xts = []
            for j in js:
                tt = tb * 4 + j
                xt = W["x"].get()
                xts.append(xt)
                dma(SP, xt, [srcbufs[tt]] if srcbufs else [], [xt], out=xt[:], in_=src_d[tt * 128:(tt + 1) * 128, :])
                st = W["stat"].get()
                hb = W["hb"].get()
                op(ACT, "activation", [xt], [hb, st], out=hb[:], in_=xt[:], func=AF.Square, accum_out=st[:, 0:1])
                rsqrt_small(st[:, 0:1], [st], st, st[:, 2:3], 1, 1.0 / D, W["stmp"].get())
                op(DVE, "scalar_tensor_tensor", [xt, st, wbc], [hb], out=hb[:], in0=xt[:], scalar=st[:, 2:3], in1=wbc[:], op0=ALU.mult, op1=ALU.mult)
                pT = W["psT"].get()
                for k in range(8):
                    op(PE, "transpose", [hb, identb], [pT], out=pT[:, k * 128:(k + 1) * 128], in_=hb[:, k * 128:(k + 1) * 128], identity=identb[:])
                op(ACT, "activation", [pT], [hT], out=hT[:, :, j * 128:(j + 1) * 128], in_=pT[:].rearrange("p (k t) -> p k t", k=8), func=AF.Copy)
            return xts

        def nt_work(es_, nx=2, nh=2):
            return {
                "x": Ring([fw.sb("xt", [128, D], F32, es_) for _ in range(nx)]),
                "stat": Ring([fw.sb("stat", [128, 4], F32, es_) for _ in range(4)]),
                "stmp": Ring([fw.sb("stmp", [128, 12], F32, es_) for _ in range(4)]),
                "hb": Ring([fw.sb("hb", [128, D], BF16, es_) for _ in range(2)]),
                "psT": Ring([fw.ps("psT", [128, D], BF16, es_) for _ in range(2)]),
                "hT": Ring([fw.sb("hT", [128, 8, 512], BF16, es_) for _ in range(nh)]),
            }

        yrnnT = fw.sb("yrnnT", [128, 4, T], BF16, att)
        kcT = fw.sb("kcT", [128, 256], BF16, att)
        VcA = fw.sb("VcA", [128, 2, 2, 128], BF16, att)
        kcTok = fw.sb("kcTok", [128, T], BF16, tokst)
        vcTok = fw.sb("vcTok", [128, T], BF16, tokst)
        ydr = [fw.dram(None, "ydr%d" % i) for i in range(NT)]

        p2w = ExitStack()
        cw1 = {}
        cw2 = {}
        for kd in ("k", "v"):
            cw1[kd] = fw.sb("cw1" + kd, [128, 32, 256], BF16, p2w)
            for l0 in range(0, 32, 8):
                dma(POOL, cw1[kd], [], [cw1[kd]], out=cw1[kd][:, l0:l0 + 8, :].rearrange("p l n -> p (l n)"), in_=cw1_d[kd][:, l0 * 256:(l0 + 8) * 256])
            cw2[kd] = fw.sb("cw2" + kd, [128, 2, 64], BF16, p2w)
            dma(POOL, cw2[kd], [], [cw2[kd]], out=cw2[kd][:].rearrange("p c d -> p (c d)"), in_=cw2_d[kd][:, :])
        cposT = fw.sb("cposT", [128, 32], BF16, p2w)
        dma(POOL, cposT, [], [cposT], out=cposT[:], in_=cposT_d[:, :])
        covf = fw.sb("covf", [128, 2, 64], F32, p2w)
        dma(SP, covf, [], [covf], out=covf[:].rearrange("p c j -> p (c j)"), in_=cover_d[:, :])

        with ExitStack() as p1:
            W = nt_work(p1)
            WinA = fw.sb("WinA", [128, 8, 1280], BF16, p1)
            for k in range(8):
                dma(POOL, WinA, [], [WinA], out=WinA[:, k, 0:1024], in_=win_d[k * 128:(k + 1) * 128, 0:1024])
                dma(POOL, WinA, [], [WinA], out=WinA[:, k, 1024:1280], in_=win_d[k * 128:(k + 1) * 128, 1536:1792])
            psM = Ring([fw.ps("psM", [128, 512], F32, p1) for _ in range(4)])
            psS = fw.ps("psS", [128, 512], F32, p1)
            xpad = [fw.sb("xpad", [128, 515], F32, p1) for _ in range(4)]
            gel = Ring([fw.sb("gel", [128, 512], F32, p1) for _ in range(2)])
            xc = Ring([fw.sb("xc", [128, 512], F32, p1) for _ in range(2)])
            xcb = Ring([fw.sb("xcb", [128, 512], BF16, p1) for _ in range(2)])
            rr = Ring([fw.sb("rr", [128, 512], F32, p1) for _ in range(2)])
            ii = Ring([fw.sb("ii", [128, 512], F32, p1) for _ in range(2)])
            aa = Ring([fw.sb("aa", [128, 512], F32, p1) for _ in range(2)])
            hring = Ring([fw.sb("hh", [128, 512], F32, p1) for _ in range(2)])
            hlast = fw.sb("hlast", [128, 4], F32, p1)
            y4 = fw.sb("y4", [128, 4, 512], F32, p1)
            ysq = Ring([fw.sb("ysq", [128, 512], F32, p1) for _ in range(2)])
            ssrow = fw.sb("ssrow", [1, 512], F32, p1)
            rrow = fw.sb("rrow", [1, 512], F32, p1)
            c1 = fw.sb("c1", [128, 4, 2], F32, p1)
            c1t = fw.sb("c1t", [128, 4], F32, p1)

            for c in range(4):
                op(DVE, "memset", [], [xpad[c]], xpad[c][:], 0.0)
            op(DVE, "memset", [], [hlast], hlast[:], 0.0)
            op(ACT, "activation", [rnnp], [c1t], out=c1t[:], in_=rnnp[:, :, 3], func=AF.Exp, scale=-1.0)
            op(ACT, "activation", [c1t], [c1t], out=c1t[:], in_=c1t[:], func=AF.Ln, bias=1.0)
            op(DVE, "tensor_scalar", [c1t], [c1], out=c1[:, :, 0], in0=c1t[:], scalar1=-8.0, scalar2=None, op0=ALU.mult)
            op(DVE, "tensor_scalar", [c1t], [c1], out=c1[:, :, 1], in0=c1t[:], scalar1=-16.0, scalar2=None, op0=ALU.mult)

            hT_next = W["hT"].get()
            norm_transpose_block(0, x_d, anw, W, hT_next)
            for tb in range(8):
                hT = hT_next
                if tb + 1 < 8:
                    hT_next = W["hT"].get()
                for dst, c0 in ((kcTok, 1024), (vcTok, 1152)):
                    pF = psM.get()
                    for k in range(8):
                        op(PE, "matmul", [hT, WinA], [pF], pF[:], lhsT=WinA[:, k, c0:c0 + 128], rhs=hT[:, k, :], start=(k == 0), stop=(k == 7))
                    op(ACT, "activation", [pF], [dst], out=dst[:, tb * 512:(tb + 1) * 512], in_=pF[:], func=AF.Copy)
                for c in range(4):
                    pX = psM.get()
                    for k in range(8):
                        op(PE, "matmul", [hT, WinA], [pX], pX[:], lhsT=WinA[:, k, c * 128:(c + 1) * 128], rhs=hT[:, k, :], start=(k == 0), stop=(k == 7))
                    xp = xpad[c]
                    if tb > 0:
                        op(DVE, "tensor_copy", [xp], [xp], out=xp[:, 0:3], in_=xp[:, 512:515])
                    op(ACT, "activation", [pX], [xp], out=xp[:, 3:515], in_=pX[:], func=AF.Copy)
                    pG = psM.get()
                    for k in range(8):
                        op(PE, "matmul", [hT, WinA], [pG], pG[:], lhsT=WinA[:, k, 512 + c * 128:512 + (c + 1) * 128], rhs=hT[:, k, :], start=(k == 0), stop=(k == 7))
                    g_ = gel.get()
                    op(ACT, "activation", [pG], [g_], out=g_[:], in_=pG[:], func=AF.Gelu_apprx_tanh)
                    xc_ = xc.get()
                    op(DVE, "tensor_scalar", [xp, convw, rnnp], [xc_], out=xc_[:], in0=xp[:, 0:512], scalar1=convw[:, c, 0:1], scalar2=rnnp[:, c, 0:1], op0=ALU.mult, op1=ALU.add)
                    for k in range(1, 4):
                        op(DVE, "scalar_tensor_tensor", [xp, convw, xc_], [xc_], out=xc_[:], in0=xp[:, k:k + 512], scalar=convw[:, c, k:k + 1], in1=xc_[:], op0=ALU.mult, op1=ALU.add)
                    xcb_ = xcb.get()
                    op(POOL, "tensor_copy", [xc_], [xcb_], out=xcb_[:], in_=xc_[:])
                    pa = psM.get()
                    op(PE, "matmul", [gaw, xcb_], [pa], pa[:], lhsT=gaw[:, c, :], rhs=xcb_[:], start=True, stop=True)
                    px = psM.get()
                    op(PE, "matmul", [gxw, xcb_], [px], px[:], lhsT=gxw[:, c, :], rhs=xcb_[:], start=True, stop=True)
                    r_, i_, a_ = rr.get(), ii.get(), aa.get()
                    op(ACT, "activation", [pa, rnnp], [r_], out=r_[:], in_=pa[:], func=AF.Sigmoid, bias=rnnp[:, c, 1:2])
                    op(ACT, "activation", [px, rnnp], [i_], out=i_[:], in_=px[:], func=AF.Sigmoid, bias=rnnp[:, c, 2:3])
                    op(ACT, "activation", [r_, c1], [a_], out=a_[:], in_=r_[:], func=AF.Exp, scale=c1[:, c, 0:1])
                    op(ACT, "activation", [r_, c1], [r_], out=r_[:], in_=r_[:], func=AF.Exp, scale=c1[:, c, 1:2])
                    op(ACT, "activation", [r_], [r_], out=r_[:], in_=r_[:], func=AF.Sqrt, scale=-1.0, bias=1.0)
                    op(DVE, "tensor_tensor", [i_, xc_], [i_], out=i_[:], in0=i_[:], in1=xc_[:], op=ALU.mult)
                    op(DVE, "tensor_tensor", [i_, r_], [i_], out=i_[:], in0=i_[:], in1=r_[:], op=ALU.mult)
                    h_ = hring.get()
                    op(DVE, "tensor_tensor_scan", [a_, i_, hlast], [h_], out=h_[:], data0=a_[:], data1=i_[:], initial=hlast[:, c:c + 1], op0=ALU.mult, op1=ALU.add)
                    op(DVE, "tensor_copy", [h_], [hlast], out=hlast[:, c:c + 1], in_=h_[:, 511:512])
                    op(DVE, "tensor_tensor", [g_, h_], [y4], out=y4[:, c, :], in0=g_[:], in1=h_[:], op=ALU.mult)
                    ys_ = ysq.get()
                    op(POOL, "tensor_tensor", [y4], [ys_], out=ys_[:], in0=y4[:, c, :], in1=y4[:, c, :], op=ALU.mult)
                    op(PE, "matmul", [ones_col, ys_], [psS], psS[0:1, :], lhsT=ones_col[:, 0:1], rhs=ys_[:], start=(c == 0), stop=(c == 3))
                    if tb + 1 < 8:
                        norm_transpose_block(tb + 1, x_d, anw, W, hT_next, js=(c,))
                op(DVE, "tensor_scalar", [psS], [ssrow], out=ssrow[:], in0=psS[0:1, :], scalar1=1.0 / 512, scalar2=EPS, op0=ALU.mult, op1=ALU.add)
                op(ACT, "activation", [ssrow], [ssrow], out=ssrow[:], in_=ssrow[:], func=AF.Sqrt)
                op(DVE, "reciprocal", [ssrow], [rrow], out=rrow[:], in_=ssrow[:])
                pb_ = psM.get()
                op(PE, "matmul", [ones_row, rrow], [pb_], pb_[:], lhsT=ones_row[:], rhs=rrow[:], start=True, stop=True)
                for c in range(4):
                    op(DVE, "scalar_tensor_tensor", [y4, rnnp, pb_], [yrnnT], out=yrnnT[:, c, tb * 512:(tb + 1) * 512], in0=y4[:, c, :], scalar=rnnp[:, c, 4:5], in1=pb_[:], op0=ALU.mult, op1=ALU.mult)
            fw.barrier()

        with ExitStack() as p2:
            for g in range(2):
                op(DVE, "tensor_copy", [covf], [VcA], out=VcA[:, :, g, 64:128], in_=covf[:])
            psM = Ring([fw.ps("psM2", [128, 512], F32, p2) for _ in range(4)])
            psQ = fw.ps("psQ2", [128, 2, 128], BF16, p2)
            hidT = Ring([fw.sb("hidT", [128, 2, 256], BF16, p2) for _ in range(2)])
            cbias = fw.sb("cbias", [128, 4], F32, p2)
            kcf = [fw.sb("kcf", [128, 2, 64], F32, p2) for _ in range(2)]
            kcb = [fw.sb("kcb", [128, 2, 64], BF16, p2) for _ in range(2)]
            qn2 = fw.sb("qn2", [128, 2, 64], F32, p2)
            st2 = Ring([fw.sb("st2", [128, 12], F32, p2) for _ in range(6)])
            qw2 = fw.sb("qw2", [128, 2, 16], F32, p2)
            t12b = fw.sb("t12b", [128, 2, 16], F32, p2)
            t34b = fw.sb("t34b", [128, 2, 16], F32, p2)
            for ct in range(2):
                op(DVE, "memset", [], [kcf[ct]], kcf[ct][:], 0.0)
            op(DVE, "memset", [], [kcT], kcT[:], 0.0)
            pbias = psM.get()
            for ki, kd in enumerate(("k", "v")):
                for n_ in range(2):
                    col = ki * 2 + n_
                    for l in range(32):
                        op(PE, "matmul", [cw1[kd], cposT], [pbias], pbias[:, col:col + 1], lhsT=cw1[kd][0:64, l, n_ * 128:(n_ + 1) * 128], rhs=cposT[0:64, l:l + 1], start=(l == 0), stop=(l == 31))
            op(DVE, "tensor_copy", [pbias], [cbias], out=cbias[:], in_=pbias[:, 0:4])
            for ki, (kd, tok) in enumerate((("k", kcTok), ("v", vcTok))):
                for g in range(2):
                    hid = hidT.get()
                    for n_ in range(2):
                        ph = psM.get()
                        for l in range(32):
                            op(PE, "matmul", [cw1[kd], tok], [ph], ph[:, 0:255], lhsT=cw1[kd][64 * g:64 * g + 64, l, n_ * 128:(n_ + 1) * 128],
                               rhs=tok[64 * g:64 * g + 64, l:l + 16 * 254 + 1:16], start=(l == 0), stop=(l == 31))
                        op(ACT, "activation", [ph, cbias], [hid], out=hid[:, n_, 0:255], in_=ph[:, 0:255], func=AF.Gelu_apprx_tanh, bias=cbias[:, ki * 2 + n_:ki * 2 + n_ + 1])
                    for ct in range(2):
                        ncs = 128 if ct == 0 else 127
                        po = psM.get()
                        for n_ in range(2):
                            op(PE, "matmul", [hid, cw2[kd]], [po], po[0:ncs, 0:64], lhsT=hid[:, n_, ct * 128:ct * 128 + ncs], rhs=cw2[kd][:, n_, :], start=(n_ == 0), stop=(n_ == 1))
                        if kd == "k":
                            op(ACT, "activation", [po], [kcf[ct]], out=kcf[ct][0:ncs, g, :], in_=po[0:ncs, 0:64], func=AF.Copy)
                        else:
                            op(ACT, "activation", [po], [VcA], out=VcA[0:ncs, ct, g, 0:64], in_=po[0:ncs, 0:64], func=AF.Copy)
            for ct in range(2):
                norm_rope(kcf[ct], kcb[ct], 2, qkw, coscT[:, ct:ct + 1, :], sincT[:, ct:ct + 1, :], [coscT, sincT], qn2, st2, qw2, t12b, t34b, wo=8)
                op(PE, "transpose", [kcb[ct], identb], [psQ], out=psQ[:, ct, :], in_=kcb[ct][:].rearrange("p h d -> p (h d)"), identity=identb[:])
            op(ACT, "activation", [psQ], [kcT], out=kcT[:, 0:255], in_=psQ[:].rearrange("p c t -> p (c t)")[:, 0:255], func=AF.Copy)
            fw.barrier()
        p2w.close()
        tokst.close()
        if stop == 2:
            att.close()
            return nc

        qT = fw.sb("qT", [128, 4, T], BF16, att)
        KA = [fw.sb("KA%d" % g_, [128, T], BF16, att) for g_ in range(2)]
        dma(SP, KA[0], [], [KA[0]], out=KA[0][64:128, :], in_=efull_d[:, :])
        dma(SP, KA[1], [], [KA[1]], out=KA[1][0:64, :], in_=efull_d[:, :])
        kwT = fw.sb("kwT", [128, T], BF16, att)
        Vs = fw.sb("Vs", [128, NT, 2, 65], BF16, att)
        Vw = fw.sb("Vw", [128, NT, 2, 65], BF16, att)
        gates = fw.sb("gates", [128, NT, 24], F32, att)
        op(POOL, "memset", [], [Vs], Vs[:, :, :, 64:65], 1.0)
        op(POOL, "memset", [], [Vw], Vw[:, :, :, 64:65], 1.0)
        with ExitStack() as p1:
            W = nt_work(p1)
            WinB = fw.sb("WinB", [128, 8, 1048], BF16, p1)
            for k in range(8):
                dma(POOL, WinB, [], [WinB], out=WinB[:, k, 0:512], in_=win_d[k * 128:(k + 1) * 128, 1024:1536])
                dma(POOL, WinB, [], [WinB], out=WinB[:, k, 512:1048], in_=win_d[k * 128:(k + 1) * 128, 1792:2328])
            psM = Ring([fw.ps("psM", [128, 512], F32, p1) for _ in range(4)])
            psQ = Ring([fw.ps("psQ", [128, 6, 128], BF16, p1) for _ in range(2)])
            qkf = Ring([fw.sb("qkf", [128, 12, 64], F32, p1) for _ in range(2)])
            qn = fw.sb("qn", [128, 12, 64], F32, p1)
            qkb = Ring([fw.sb("qkb", [128, 12, 64], BF16, p1) for _ in range(2)])
            st12 = Ring([fw.sb("st12", [128, 12], F32, p1) for _ in range(4)])
            qw16 = fw.sb("qw16", [128, 12, 16], F32, p1)
            t12 = fw.sb("t12", [128, 12, 16], F32, p1)
            t34 = fw.sb("t34", [128, 12, 16], F32, p1)
            hT_next = W["hT"].get()
            norm_transpose_block(0, x_d, anw, W, hT_next)
            ptail = []
            for tb in range(8):
                hT = hT_next
                if tb + 1 < 8:
                    hT_next = W["hT"].get()
                for j in range(4):
                    tt = tb * 4 + j
                    qf = qkf.get()
                    pA = psM.get()
                    for k in range(8):
                        op(PE, "matmul", [hT, WinB], [pA], pA[:, 0:512], lhsT=hT[:, k, j * 128:(j + 1) * 128], rhs=WinB[:, k, 0:512], start=(k == 0), stop=(k == 7))
                    op(ACT, "activation", [pA], [qf], out=qf[:, 0:8, :].rearrange("p (i g) d -> p i g d", g=2),
                       in_=pA[:, 0:512].rearrange("p (g i d) -> p i g d", g=2, i=4), func=AF.Copy)
                    pB = psM.get()
                    for k in range(8):
                        op(PE, "matmul", [hT, WinB], [pB], pB[:, 0:256], lhsT=hT[:, k, j * 128:(j + 1) * 128], rhs=WinB[:, k, 512:768], start=(k == 0), stop=(k == 7))
                    op(ACT, "activation", [pB], [qf], out=qf[:, 8:10, :].rearrange("p h d -> p (h d)"), in_=pB[:, 0:128], func=AF.Copy)
                    op(ACT, "activation", [pB], [Vs], out=Vs[:, tt, :, 0:64], in_=pB[:, 128:256].rearrange("p (g d) -> p g d", g=2), func=AF.Copy)
                    pC = psM.get()
                    for k in range(8):
                        op(PE, "matmul", [hT, WinB], [pC], pC[:, 0:280], lhsT=hT[:, k, j * 128:(j + 1) * 128], rhs=WinB[:, k, 768:1048], start=(k == 0), stop=(k == 7))
                    op(ACT, "activation", [pC], [qf], out=qf[:, 10:12, :].rearrange("p h d -> p (h d)"), in_=pC[:, 0:128], func=AF.Copy)
                    op(ACT, "activation", [pC], [Vw], out=Vw[:, tt, :, 0:64], in_=pC[:, 128:256].rearrange("p (g d) -> p g d", g=2), func=AF.Copy)
                    op(ACT, "activation", [pC], [gates], out=gates[:, tt, :], in_=pC[:, 256:280], func=AF.Sigmoid)
                    qb = qkb.get()
                    norm_rope(qf, qb, 12, qkw, cosT[:, tt:tt + 1, :], sinT[:, tt:tt + 1, :], [cosT, sinT], qn, st12, qw16, t12, t34)

                    def tail(tt=tt, qb=qb):
                        pQ = psQ.get()
                        for i in range(6):
                            op(PE, "transpose", [qb, identb], [pQ], out=pQ[:, i, :], in_=qb[:, 2 * i:2 * i + 2, :].rearrange("p h d -> p (h d)"), identity=identb[:])
                        op(ACT, "activation", [pQ], [qT], out=qT[:, :, tt * 128:(tt + 1) * 128], in_=pQ[:, 0:4, :], func=AF.Copy)
                        op(ACT, "activation", [pQ], [KA[0]], out=KA[0][0:64, tt * 128:(tt + 1) * 128], in_=pQ[0:64, 4, :], func=AF.Copy)
                        op(ACT, "activation", [pQ], [KA[1]], out=KA[1][64:128, tt * 128:(tt + 1) * 128], in_=pQ[64:128, 4, :], func=AF.Copy)
                        op(ACT, "activation", [pQ], [kwT], out=kwT[:, tt * 128:(tt + 1) * 128], in_=pQ[:, 5, :], func=AF.Copy)

                    if tb + 1 < 8:
                        norm_transpose_block(tb + 1, x_d, anw, W, hT_next, js=(j,))
                    while ptail:
                        ptail.pop(0)()
                    ptail.append(tail)
            while ptail:
                ptail.pop(0)()
            fw.barrier()

        if stop == "B":
            att.close()
            return nc
        with ExitStack() as p3:
            tri4 = fw.sb("tri4", [128, 4, 128], BF16, p3)
            wlow4 = fw.sb("wlow4", [128, 4, 128], BF16, p3)
            cmask = fw.sb("cmask", [128, 33, 128], BF16, p3)
            vmbs = fw.sb("vmbs", [128, 2, 128], F32, p3)
            wout = fw.sb("wout", [128, 8, D], BF16, p3)
            aonw = fw.sb("aonw", [128, 512], F32, p3)
            dma(SP, tri4, [], [tri4], out=tri4[:].rearrange("p h t -> p (h t)"), in_=tri4_d[:, :])
            dma(SP, wlow4, [], [wlow4], out=wlow4[:].rearrange("p h t -> p (h t)"), in_=wlow4_d[:, :])
            dma(SP, cmask, [], [cmask], out=cmask[:].rearrange("p m t -> p (m t)"), in_=cmask_d[:, :])
            dma(SP, vmbs, [], [vmbs], out=vmbs[:].rearrange("p a t -> p (a t)"), in_=vmbs_d[:, :])
            dma(SP, aonw, [], [aonw], out=aonw[:], in_=aonw_d[:, :])
            for k in range(8):
                dma(POOL, wout, [], [wout], out=wout[:, k, :], in_=wout_d[k * 128:(k + 1) * 128, :])
            psS = Ring([fw.ps("psS3", [128, 4, 128], F32, p3) for _ in range(3)])
            psC = fw.ps("psC", [128, 4, 128], F32, p3)
            psSel = fw.ps("psSel", [128, 4, 128], F32, p3)
            psWin = fw.ps("psWin", [128, 4, 128], F32, p3)
            psX = Ring([fw.ps("psX", [128, 512], F32, p3) for _ in range(1)])
            psTb = fw.ps("psTb", [128, 1024], BF16, p3)
            Er = Ring([fw.sb("E", [128, 4, 128], BF16, p3) for _ in range(4)])
            xr3 = Ring([fw.sb("xt3", [128, D], F32, p3) for _ in range(2)])
            ocmp = [fw.sb("ocmp", [128, 4, 64], F32, p3) for _ in range(2)]
            RA = [[fw.sb("RA", [128, 4, 128], BF16, p3) for _ in range(2)] for _ in range(2)]
            negm2 = [fw.sb("negm2", [128, 128], BF16, p3) for _ in range(2)]
            sm = Ring([fw.sb("sm", [128, 16], F32, p3) for _ in range(12)])
            imp = fw.sb("imp", [128, 64], F32, p3)
            score = fw.sb("score", [128, 64], F32, p3)
            score2 = fw.sb("score2", [128, 64], F32, p3)
            negm = fw.sb("negm", [128, 64], BF16, p3)
            yatts = [fw.sb("yatt", [128, 8, 64], F32, p3) for _ in range(2)]
            ytmp = fw.sb("ytmp", [128, 4, 64], F32, p3)
            yb = fw.sb("yb", [128, 512], BF16, p3)
            yjunk = fw.sb("yjunk", [128, 512], BF16, p3)
            yattT = fw.sb("yattT", [128, 4, 128], BF16, p3)

            for g in range(2):
                op(POOL, "memset", [], [negm2[g]], negm2[g][:], 0.0)

            def qslice(g, qt):
                return qT[64 * g:64 * g + 64, :, qt * 128:(qt + 1) * 128]

            ytr = Ring([fw.sb("ytmp", [128, 4, 64], F32, p3) for _ in range(2)])
            xts3 = {}

            def gview(qt, g):
                return gates[:, qt, g * 12:(g + 1) * 12].rearrange("p (h b) -> p b h", b=3)

            deferred = []

            def topk(qt, g):
                gv = gview(qt, g)
                yatt = yatts[qt % 2]
                sums = sm.get()
                op(DVE, "tensor_reduce", [psC], [sums], out=sums[:, 0:4], in_=psC[:, :, 64:128], axis=AX.X, op=ALU.add)
                op(DVE, "tensor_scalar", [sums], [sums], out=sums[:, 4:8], in0=sums[:, 0:4], scalar1=1e-30, scalar2=None, op0=ALU.max)
                op(DVE, "reciprocal", [sums], [sums], out=sums[:, 8:12], in_=sums[:, 4:8])
                op(DVE, "tensor_tensor", [sums, gates], [sums], out=sums[:, 12:16], in0=sums[:, 8:12], in1=gv[:, 0, :], op=ALU.mult)
                op(DVE, "tensor_tensor", [psC, sums], [yatt], out=yatt[:, g * 4:(g + 1) * 4, :], in0=psC[:, :, 0:64], in1=sums[:, 12:16].unsqueeze(2).broadcast_to([128, 4, 64]), op=ALU.mult)
                op(DVE, "tensor_scalar", [psC, sums], [imp], out=imp[:], in0=psC[:, 0, 64:128], scalar1=sums[:, 8:9], scalar2=None, op0=ALU.mult)
                for h in range(1, 4):
                    op(DVE, "scalar_tensor_tensor", [psC, sums, imp], [imp], out=imp[:], in0=psC[:, h, 64:128], scalar=sums[:, 8 + h:9 + h], in1=imp[:], op0=ALU.mult, op1=ALU.add)
                lo = 64 - 2 * qt
                op(DVE, "tensor_tensor", [imp, vmbs], [score], out=score[:], in0=imp[:], in1=vmbs[:, 0, lo:lo + 64], op=ALU.mult)
                op(DVE, "tensor_tensor", [score, vmbs], [score], out=score[:], in0=score[:], in1=vmbs[:, 1, lo:lo + 64], op=ALU.add)
                op(DVE, "memset", [], [score], score[:, 0:1], 1.0e4)
                m8 = sm.get()
                op(DVE, "max", [score], [m8], out=m8[:, 0:8], in_=score[:])
                op(DVE, "match_replace", [m8, score], [score2], out=score2[:], in_to_replace=m8[:, 0:8], in_values=score[:], imm_value=-3.0e38)
                op(DVE, "max", [score2], [m8], out=m8[:, 8:16], in_=score2[:])
                c0 = 64 if g == 0 else 0
                nm = negm2[g]
                op(DVE, "tensor_scalar", [score, m8], [nm], out=nm[:, c0:c0 + 64], in0=score[:], scalar1=m8[:, 15:16], scalar2=NEG, op0=ALU.is_lt, op1=ALU.mult)
                _nw = 2 * min(qt + 1, 5)
                _allow = ((1 if qt < 16 else 2) + _nw - 2) if g == 0 else (_nw + qt + 1 - 2)
                if _os.environ.get('KAHEAD', '1') == '1':
                    _allow = 4 if qt == 0 else 7
                _dt = max(1, min(int(_os.environ.get('KTOPDEF', 6)), _allow))
                if _dt == 0:
                    topk_b(qt, g)
                else:
                    deferred.append([_dt, lambda: topk_b(qt, g)])

            def topk_b(qt, g):
                nm = negm2[g]
                pNb = psTb[:, 512:1024]
                op(PE, "transpose", [nm, identb], [psTb], out=pNb[:, 0:128], in_=nm[:], identity=identb[:])
                ra = RA[g][qt % 2]
                r0 = 64 if g == 0 else 0
                q0 = 0 if g == 0 else 64
                op(DVE, "tensor_copy", [psTb], [ra], out=ra[r0:r0 + 64, 0, :], in_=pNb[r0:r0 + 64, 0:128])
                op(POOL, "tensor_copy", [ra], [ra], out=ra[r0:r0 + 64, 1:4, :], in_=ra[r0:r0 + 64, 0:1, :].broadcast_to([64, 3, 128]))
                op(POOL, "tensor_copy", [qT], [ra], out=ra[q0:q0 + 64, :, :], in_=qT[q0:q0 + 64, :, qt * 128:(qt + 1) * 128])

            def evac(qt, g, pO, b):
                gv = gview(qt, g)
                yatt = yatts[qt % 2]
                cf = sm.get()
                op(DVE, "tensor_scalar", [pO], [cf], out=cf[:, 0:4], in0=pO[:, :, 64], scalar1=1e-30, scalar2=None, op0=ALU.max)
                op(DVE, "reciprocal", [cf], [cf], out=cf[:, 4:8], in_=cf[:, 0:4])
                op(DVE, "tensor_tensor", [cf, gates], [cf], out=cf[:, 8:12], in0=cf[:, 4:8], in1=gv[:, b, :], op=ALU.mult)
                yt = ytr.get()
                yg = yatt[:, g * 4:(g + 1) * 4, :]
                op(DVE, "tensor_tensor", [pO, cf], [yt], out=yt[:], in0=pO[:, :, 0:64], in1=cf[:, 8:12].unsqueeze(2).broadcast_to([128, 4, 64]), op=ALU.mult)
                op(POOL, "tensor_tensor", [yatt, yt], [yatt], out=yg, in0=yg, in1=yt[:], op=ALU.add)

            def finish_a(qt):
                yatt = yatts[qt % 2]
                yf = yatt[:].rearrange("p h d -> p (h d)")
                st = sm.get()
                op(DVE, "tensor_tensor", [yatt], [yjunk], out=yjunk[:], in0=yf, in1=yf, op=ALU.mult)
                op(DVE, "tensor_reduce", [yjunk], [st], out=st[:, 0:1], in_=yjunk[:], axis=AX.X, op=ALU.add)
                rsqrt_small(st[:, 0:1], [st], st, st[:, 2:3], 1, 1.0 / 512, sm.get())
                op(DVE, "scalar_tensor_tensor", [yatt, st, aonw], [yb], out=yb[:], in0=yf, scalar=st[:, 2:3], in1=aonw[:], op0=ALU.mult, op1=ALU.mult)

            def finish_b(qt):
                pYb = psTb[:, 0:512]
                for c in range(4):
                    op(PE, "transpose", [yb, identb], [psTb], out=pYb[:, c * 128:(c + 1) * 128], in_=yb[:, c * 128:(c + 1) * 128], identity=identb[:])
                op(DVE, "tensor_copy", [psTb], [yattT], out=yattT[:].rearrange("p c t -> p (c t)"), in_=pYb[:, 0:512])

            def finish_c(qt):
                xt = xts3.pop(qt)
                for half in range(2):
                    pO_ = psX.get()
                    for c in range(4):
                        op(PE, "matmul", [yrnnT, wout], [pO_], pO_[:], lhsT=yrnnT[:, c, qt * 128:(qt + 1) * 128], rhs=wout[:, c, half * 512:(half + 1) * 512], start=(c == 0), stop=False)
                    for c in range(4):
                        op(PE, "matmul", [yattT, wout], [pO_], pO_[:], lhsT=yattT[:, c, :], rhs=wout[:, 4 + c, half * 512:(half + 1) * 512], start=False, stop=(c == 3))
                    op(DVE, "tensor_tensor", [xt, pO_], [xt], out=xt[:, half * 512:(half + 1) * 512], in0=xt[:, half * 512:(half + 1) * 512], in1=pO_[:], op=ALU.add)
                dma(SP, xt, [xt], [ydr[qt]], out=y_d[qt * 128:(qt + 1) * 128, :], in_=xt[:])

            def finish(qt):
                finish_a(qt)
                _d = int(_os.environ.get('KDEF', 1))
                if _d == 0:
                    finish_b(qt)
                    finish_c(qt)
                else:
                    deferred.append([_d, lambda: finish_b(qt)])
                    deferred.append([2 * _d, lambda: finish_c(qt)])

            def load_x(qt):
                xt = xr3.get()
                xts3[qt] = xt
                dma(SP, xt, [], [xt], out=xt[:], in_=x_d[qt * 128:(qt + 1) * 128, :])

            import os as _os
            _nqt = int(_os.environ.get('KQT', NT))
            jobs = []

            def cmp_jobs(qt):
                for g in range(2):
                    cts = [0] if qt < 16 else [0, 1]
                    for ct in cts:
                        ncs = 128 if ct == 0 else 127
                        mi = (qt if qt <= 16 else None) if ct == 0 else 17 + qt - 16
                        mm = [(kcT[64 * g:64 * g + 64, ct * 128:ct * 128 + ncs], qslice(g, qt), [kcT, qT], None)]
                        if mi is not None:
                            for h in range(4):
                                mm.append((identb[0:ncs, 0:ncs], cmask[0:ncs, mi, :], [identb, cmask], h))
                        jobs.append(dict(qt=qt, ncs=ncs, mm=mm, pO=psC, V=VcA[0:ncs, ct, g, :], Vb=VcA, ncol=128, first=(ct == cts[0]), before=None,
                                         after=((lambda qt=qt, g=g: topk(qt, g)) if ct == cts[-1] else None)))

            def main_jobs(qt):
                first = True
                for g in range(2):
                    kts = list(range(max(0, qt - 4), qt + 1))
                    for kt in kts:
                        mm = [(kwT[64 * g:64 * g + 64, kt * 128:(kt + 1) * 128], qslice(g, qt), [kwT, qT], None)]
                        if kt == qt:
                            mm.append((identb[:], tri4[:], [identb, tri4], None))
                        if kt == qt - 4:
                            mm.append((identb[:], wlow4[:], [identb, wlow4], None))
                        jobs.append(dict(qt=qt, ncs=128, mm=mm, pO=psWin, V=Vw[:, kt, g, :], Vb=Vw, ncol=65, first=(kt == kts[0]), before=(load_x if first else None),
                                         after=((lambda qt=qt, g=g: evac(qt, g, psWin, 2)) if kt == kts[-1] else None)))
                        first = False
                for g in range(2):
                    kts = list(range(qt + 1))
                    for kt in kts:
                        mm = [(KA[g][:, kt * 128:(kt + 1) * 128], RA[g][qt % 2][:], [KA[g], RA[g][qt % 2]], None)]
                        if kt == qt:
                            mm.append((identb[:], tri4[:], [identb, tri4], None))
                        if kt == kts[-1]:
                            if g == 0:
                                aft = (lambda qt=qt, g=g: evac(qt, g, psSel, 1))
                            else:
                                aft = (lambda qt=qt, g=g: (evac(qt, g, psSel, 1), finish(qt)))
                        else:
                            aft = None
                        jobs.append(dict(qt=qt, ncs=128, mm=mm, pO=psSel, V=Vs[:, kt, g, :], Vb=Vs, ncol=65, first=(kt == kts[0]), before=None, after=aft))

            if _os.environ.get('KAHEAD', '1') == '1':
                for it in range(_nqt + 1):
                    if it < _nqt:
                        cmp_jobs(it)
                    if it >= 1:
                        main_jobs(it - 1)
            else:
                for it in range(_nqt):
                    cmp_jobs(it)
                    main_jobs(it)

            def emit_score(job):
                if job["before"]:
                    job["before"](job["qt"])
                pS = psS.get()
                ncs = job["ncs"]
                n = len(job["mm"])
                for i, (l_, r_, bufs_, h) in enumerate(job["mm"]):
                    o_ = pS[0:ncs, :, :] if h is None else pS[0:ncs, h, :]
                    op(PE, "matmul", bufs_, [pS], o_, lhsT=l_, rhs=r_, start=(i == 0), stop=(i == n - 1), skip_group_check=True)
                return pS

            def emit_rest(job, pS):
                ncs = job["ncs"]
                E = Er.get()
                op(ACT, "activation", [pS], [E], out=E[0:ncs], in_=pS[0:ncs], func=AF.Exp, scale=0.125)
                pO = job["pO"]
                for h in range(4):
                    op(PE, "matmul", [E, job["Vb"]], [pO], pO[:, h, 0:job["ncol"]], lhsT=E[0:ncs, h, :], rhs=job["V"], start=(job["first"] and h == 0), stop=True, skip_group_check=True)
                if job["after"]:
                    job["after"]()
                for d_ in list(deferred):
                    d_[0] -= 1
                    if d_[0] <= 0:
                        deferred.remove(d_)
                        d_[1]()

            LOOK = int(_os.environ.get('KLOOK', 2))
            pend = []
            for job in jobs:
                pend.append((job, emit_score(job)))
                if len(pend) > LOOK:
                    emit_rest(*pend.pop(0))
            while pend:
                emit_rest(*pend.pop(0))
            for d_ in list(deferred):
                d_[1]()
            fw.barrier()
        att.close()
        if stop == 3:
            return nc

        with ExitStack() as p4:
            Wg = fw.sb("Wg", [128, 8, DFF], BF16, p4)
            Wu = fw.sb("Wu", [128, 8, DFF], BF16, p4)
            Wd = fw.sb("Wd", [128, NFF, D], BF16, p4)
            fnw = fw.sb("fnw", [128, D], F32, p4)
            dma(SP, fnw, [], [fnw], out=fnw[:], in_=fnw_d[:, :])
            for k in range(8):
                dma(POOL, Wg, [], [Wg], out=Wg[:, k, :], in_=wg_d[k * 128:(k + 1) * 128, :])
                dma(POOL, Wu, [], [Wu], out=Wu[:, k, :], in_=wu_d[k * 128:(k + 1) * 128, :])
            for j in range(NFF):
                dma(POOL, Wd, [], [Wd], out=Wd[:, j, :], in_=wd_d[j * 128:(j + 1) * 128, :])
            W = nt_work(p4, nx=5, nh=1)
            psG = Ring([fw.ps("psG", [128, 512], F32, p4) for _ in range(2)])
            psU = Ring([fw.ps("psU", [128, 512], F32, p4) for _ in range(2)])
            psD = Ring([fw.ps("psD", [128, 512], F32, p4) for _ in range(2)])
            sg = Ring([fw.sb("sg", [128, 512], F32, p4) for _ in range(2)])
            aT = fw.sb("aT", [128, NFF, 512], BF16, p4)
            for tb in range(8):
                hT = W["hT"].get()
                xts = norm_transpose_block(tb, y_d, fnw, W, hT, srcbufs=ydr)
                for j in range(NFF):
                    pg, pu = psG.get(), psU.get()
                    for k in range(8):
                        op(PE, "matmul", [Wg, hT], [pg], pg[:], lhsT=Wg[:, k, j * 128:(j + 1) * 128], rhs=hT[:, k, :], start=(k == 0), stop=(k == 7))
                    for k in range(8):
                        op(PE, "matmul", [Wu, hT], [pu], pu[:], lhsT=Wu[:, k, j * 128:(j + 1) * 128], rhs=hT[:, k, :], start=(k == 0), stop=(k == 7))
                    sg_ = sg.get()
                    op(ACT, "activation", [pg], [sg_], out=sg_[:], in_=pg[:], func=AF.Silu)
                    op(DVE, "tensor_tensor", [sg_, pu], [aT], out=aT[:, j, :], in0=sg_[:], in1=pu[:], op=ALU.mult)
                for i in range(4):
                    tt = tb * 4 + i
                    xt = xts[i]
                    for half in range(2):
                        pd = psD.get()
                        for j in range(NFF):
                            op(PE, "matmul", [aT, Wd], [pd], pd[:], lhsT=aT[:, j, i * 128:(i + 1) * 128], rhs=Wd[:, j, half * 512:(half + 1) * 512], start=(j == 0), stop=(j == NFF - 1))
                        op(DVE, "tensor_tensor", [xt, pd], [xt], out=xt[:, half * 512:(half + 1) * 512], in0=xt[:, half * 512:(half + 1) * 512], in1=pd[:], op=ALU.add)
                    dma(SP, xt, [xt], [ydr[tt]], out=y_d[tt * 128:(tt + 1) * 128, :], in_=xt[:])
            fw.barrier()
    return nc


def host_inputs(inp, b):
    f32 = np.float32
    m = {}
    m["x"] = np.ascontiguousarray(inp["x"][b], dtype=f32)
    pos = np.asarray(inp["positions"][b], dtype=np.int32)
    m["pos"] = np.ascontiguousarray(pos.reshape(NT, 128).T)
    pc = np.zeros(256, np.int32)
    pc[:255] = pos[np.arange(255) * 16 + 31]
    m["posc"] = np.ascontiguousarray(pc.reshape(2, 128).T)
    invf = (500000.0 ** (-np.arange(8, dtype=np.float32) * 2.0 / 16)).astype(f32)
    m["invf"] = np.ascontiguousarray(np.broadcast_to(invf, (128, 8)), dtype=f32)
    m["w_in"] = np.ascontiguousarray(inp["w_in"][0], dtype=f32)
    m["anw"] = np.ascontiguousarray(np.broadcast_to(inp["attn_norm_w"][0], (128, D)), dtype=f32)
    qkw = np.concatenate([np.tile(inp["q_norm_w"][0], 8), np.tile(inp["k_norm_w"][0], 4)])
    m["qkw"] = np.ascontiguousarray(np.broadcast_to(qkw, (128, 768)), dtype=f32)
    cw = inp["conv_w"][0].reshape(4, 4, 128)
    m["convw"] = np.ascontiguousarray(cw.transpose(2, 1, 0).reshape(128, 16), dtype=f32)
    rp = np.stack([inp["conv_b"][0], inp["gate_a_b"][0], inp["gate_x_b"][0], inp["lru_lambda"][0], inp["rnn_out_norm_w"][0]], 0)
    m["rnnp"] = np.ascontiguousarray(rp.reshape(5, 4, 128).transpose(2, 1, 0).reshape(128, 20), dtype=f32)
    for nm, key in (("gaw", "gate_a_w"), ("gxw", "gate_x_w")):
        w = inp[key][0]
        bd = np.zeros((128, 4, 128), f32)
        for c in range(4):
            for i in range(2):
                bd[64 * i:64 * i + 64, c, 64 * i:64 * i + 64] = w[2 * c + i]
        m[nm] = bd.reshape(128, 512)
    m["identf"] = np.eye(128, dtype=f32)
    cmpw = {"k": (inp["cmp_k_w1"], inp["cmp_k_w2"]), "v": (inp["cmp_v_w1"], inp["cmp_v_w2"])}
    for kd in ("k", "v"):
        w1 = cmpw[kd][0][0].reshape(32, 64, 256).transpose(1, 0, 2).reshape(64, 32 * 256)
        m["cw1" + kd] = np.ascontiguousarray(np.concatenate([w1, w1], 0), dtype=f32)
        w2 = cmpw[kd][1][0].reshape(2, 128, 64).transpose(1, 0, 2).reshape(128, 128)
        m["cw2" + kd] = np.ascontiguousarray(w2, dtype=f32)
    cp = inp["cmp_pos"][0].T
    m["cposT"] = np.ascontiguousarray(np.concatenate([cp, cp], 0), dtype=f32)
    m.update(CONSTS)
    m["w_out"] = np.ascontiguousarray(inp["w_out"][0], dtype=f32)
    m["aonw"] = np.ascontiguousarray(np.broadcast_to(inp["attn_out_norm_w"][0], (128, 512)), dtype=f32)
    m["fnw"] = np.ascontiguousarray(np.broadcast_to(inp["ffn_norm_w"][0], (128, D)), dtype=f32)
    m["w_gate"] = np.ascontiguousarray(inp["w_gate"][0], dtype=f32)
    m["w_up"] = np.ascontiguousarray(inp["w_up"][0], dtype=f32)
    m["w_down"] = np.ascontiguousarray(inp["w_down"][0], dtype=f32)
    return m


def _consts():
    bf = ml_dtypes.bfloat16
    f32 = np.float32
    c = {}
    kl = np.arange(128)[:, None]
    ql = np.arange(128)[None, :]
    tri = np.where(kl <= ql, 0.0, NEG).astype(f32)
    c["tri4"] = np.ascontiguousarray(np.tile(tri, (1, 4))).astype(bf)
    wl = np.where(kl > ql, 0.0, NEG).astype(f32)
    c["wlow4"] = np.ascontiguousarray(np.tile(wl, (1, 4))).astype(bf)
    cm = np.zeros((128, 33, 128), f32)
    for mi in range(33):
        ct, qt = (0, mi) if mi <= 16 else (1, 16 + mi - 17)
        dv = 128 * qt - 2048 * ct - 31
        cm[:, mi, :] = np.where(16 * kl - ql <= dv, 0.0, NEG)
    c["cmask"] = np.ascontiguousarray(cm.reshape(128, 33 * 128)).astype(bf)
    ef = (np.arange(T)[None, :] // 64 == np.arange(64)[:, None]).astype(f32)
    c["efull"] = np.ascontiguousarray(ef).astype(bf)
    cs = np.arange(255)[:, None] * 16
    bs = np.arange(64)[None, :] * 64
    cov = np.clip(np.minimum(cs + 32, bs + 64) - np.maximum(cs, bs), 0, None).astype(f32) / 32.0
    covp = np.zeros((256, 64), f32)
    covp[:255] = cov
    c["cover"] = np.ascontiguousarray(covp.reshape(2, 128, 64).transpose(1, 0, 2).reshape(128, 128))
    tl = np.arange(128)[:, None]
    jp = np.arange(128)[None, :] - 64
    cc = (tl >= 64).astype(np.int64)
    forced = (jp == cc) | (jp == cc - 1)
    invalid = jp > cc
    vm = np.where(forced | invalid, 0.0, 1.0).astype(f32)
    bsb = np.where(forced, 1.0e4, np.where(invalid, -1.0e30, 0.0)).astype(f32)
    c["vmbs"] = np.ascontiguousarray(np.concatenate([vm, bsb], 1))
    return c


CONSTS = _consts()


def kernel(**inputs):
    inp = {k: np.asarray(v) for k, v in inputs.items()}
    nc = build()
    in_maps = [host_inputs(inp, b) for b in range(8)]
    res = run_bass_kernel_spmd(nc, in_maps, core_ids=list(range(8)))
    return np.stack([r["y"] for r in res.results], 0).astype(np.float32)
```
